# Optimizing a Trainium2 kernel written in Bass

```python
import math
import jax, jax.numpy as jnp
from jax import lax
import numpy as np

D_MODEL = 4096
BATCH = 4
SEQ = 2048
DEPTH = 1

D_PLE = 256
ATTN_HEADS = 8
ATTN_QK_DIM = 128
ATTN_V_DIM = 2 * ATTN_QK_DIM
ATTN_WIDTH = ATTN_HEADS * ATTN_V_DIM
QK_COLS = ATTN_HEADS * 2 * ATTN_QK_DIM
CONV_CH = D_MODEL - ATTN_WIDTH
CONV_WIDTH = 31
MIX_WIDTH = ATTN_WIDTH + CONV_CH
IN_COLS = 2 * QK_COLS + ATTN_WIDTH + 2 * CONV_CH
Q_BLOCK = 128
PEER_HEADS = 8
PEER_HALF = 128
PEER_QUERY_DIM = 2 * PEER_HALF
N_KEYS = 128
N_EXPERTS = N_KEYS * N_KEYS
PEER_TOPK = 16
PEER_TOKEN_CHUNK = 64
EPS = 1e-6

kernel_name = 'hybrid_diffattn_conformer_peer_block'


def rms_norm(x, g):
    xf = x.astype(jnp.float32)
    y = xf * lax.rsqrt(jnp.mean(xf * xf, axis=-1, keepdims=True) + EPS)
    return (y * g.astype(jnp.float32)).astype(x.dtype)


def layer_norm(x, g, b):
    xf = x.astype(jnp.float32)
    mu = jnp.mean(xf, axis=-1, keepdims=True)
    xc = xf - mu
    y = xc * lax.rsqrt(jnp.mean(xc * xc, axis=-1, keepdims=True) + EPS)
    return (y * g.astype(jnp.float32) + b.astype(jnp.float32)).astype(x.dtype)


def diff_attention(q, k, v, lam, lam_init, g_q, g_k, g_sub):
    B, S = q.shape[0], q.shape[1]
    q = rms_norm(q, g_q)
    k = rms_norm(k, g_k)
    scale = ATTN_QK_DIM ** -0.5
    nblk = S // Q_BLOCK
    qb = q.reshape(B, nblk, Q_BLOCK, ATTN_HEADS, 2, ATTN_QK_DIM).transpose(1, 0, 2, 3, 4, 5)
    key_pos = jnp.arange(S)

    def block(args):
        i, qi = args
        s = jnp.einsum('bqhmd,bkhmd->bhmqk', qi, k).astype(jnp.float32) * scale
        q_pos = i * Q_BLOCK + jnp.arange(Q_BLOCK)
        causal = key_pos[None, :] <= q_pos[:, None]
        s = jnp.where(causal, s, -jnp.inf)
        pr = jax.nn.softmax(s, axis=-1)
        a = pr[:, :, 0] - lam * pr[:, :, 1]
        return jnp.einsum('bhqk,bkhd->bqhd', a.astype(v.dtype), v)

    o = lax.map(block, (jnp.arange(nblk), qb))
    o = o.transpose(1, 0, 2, 3, 4).reshape(B, S, ATTN_HEADS, ATTN_V_DIM)
    o = rms_norm(o, g_sub) * (1.0 - lam_init)
    return o.reshape(B, S, ATTN_WIDTH)


def conformer_conv(u, b_glu, w_dw, b_dw, ln_g, ln_b):
    u = u + b_glu
    a, gate = jnp.split(u, 2, axis=-1)
    y = a * jax.nn.sigmoid(gate)
    y = lax.conv_general_dilated(
        y, w_dw[:, None, :], window_strides=(1,), padding=[(CONV_WIDTH - 1, 0)],
        dimension_numbers=('NWC', 'WIO', 'NWC'), feature_group_count=CONV_CH) + b_dw
    y = layer_norm(y, ln_g, ln_b)
    return jax.nn.silu(y)


def peer(h, w_query, sub_keys, expert_u, expert_v):
    B, S, D = h.shape
    q = (h @ w_query).reshape(B, S, PEER_HEADS, 2, PEER_HALF)
    s = jnp.einsum('bshcd,hcnd->bshcn', q, sub_keys).astype(jnp.float32)
    top_s, top_i = lax.top_k(s, PEER_TOPK)
    cand_s = (top_s[..., 0, :, None] + top_s[..., 1, None, :]).reshape(B, S, PEER_HEADS, PEER_TOPK * PEER_TOPK)
    cand_i = (top_i[..., 0, :, None] * N_KEYS + top_i[..., 1, None, :]).reshape(B, S, PEER_HEADS, PEER_TOPK * PEER_TOPK)
    best_s, pos = lax.top_k(cand_s, PEER_TOPK)
    idx = jnp.take_along_axis(cand_i, pos, axis=-1)
    g = jax.nn.softmax(best_s, axis=-1).astype(h.dtype)
    n_chunks = (B * S) // PEER_TOKEN_CHUNK
    hc = h.reshape(n_chunks, PEER_TOKEN_CHUNK, D)
    ic = idx.reshape(n_chunks, PEER_TOKEN_CHUNK, PEER_HEADS, PEER_TOPK)
    gc = g.reshape(n_chunks, PEER_TOKEN_CHUNK, PEER_HEADS, PEER_TOPK)

    def chunk(args):
        hx, ix, gx = args
        a = jnp.einsum('cd,chkd->chk', hx, expert_u[ix])
        act = jax.nn.gelu(a, approximate=False) * gx
        return jnp.einsum('chk,chkd->cd', act, expert_v[ix])

    out = lax.map(chunk, (hc, ic, gc))
    return out.reshape(B, S, D)


def setup_inputs(seed: int = 0) -> dict:
    key = jax.random.key(seed)
    ks = jax.random.split(key, 24)
    f32 = jnp.float32
    nrm = lambda k, shape, sc: jax.random.normal(k, shape, f32) * sc
    L = DEPTH
    return {
        'x': nrm(ks[0], (BATCH, SEQ, D_MODEL), 1.0),
        'p': nrm(ks[1], (DEPTH, BATCH, SEQ, D_PLE), 1.0),
        'mix_norm_g': 1.0 + nrm(ks[2], (L, D_MODEL), 0.02),
        'w_in': nrm(ks[3], (L, D_MODEL, IN_COLS), D_MODEL ** -0.5),
        'q_norm_g': 1.0 + nrm(ks[4], (L, 2, ATTN_QK_DIM), 0.02),
        'k_norm_g': 1.0 + nrm(ks[5], (L, 2, ATTN_QK_DIM), 0.02),
        'lambda_q': nrm(ks[6], (L, 2, ATTN_QK_DIM), 0.1),
        'lambda_k': nrm(ks[7], (L, 2, ATTN_QK_DIM), 0.1),
        'subln_g': 1.0 + nrm(ks[8], (L, ATTN_V_DIM), 0.02),
        'glu_b': nrm(ks[9], (L, 2 * CONV_CH), 0.01),
        'dw_kernel': nrm(ks[10], (L, CONV_WIDTH, CONV_CH), CONV_WIDTH ** -0.5),
        'dw_b': nrm(ks[11], (L, CONV_CH), 0.01),
        'conv_ln_g': 1.0 + nrm(ks[12], (L, CONV_CH), 0.02),
        'conv_ln_b': nrm(ks[13], (L, CONV_CH), 0.01),
        'w_out': nrm(ks[14], (L, MIX_WIDTH, D_MODEL), MIX_WIDTH ** -0.5),
        'ffn_norm_g': 1.0 + nrm(ks[15], (L, D_MODEL), 0.02),
        'peer_w_query': nrm(ks[16], (L, D_MODEL, PEER_HEADS * PEER_QUERY_DIM), D_MODEL ** -0.5),
        'peer_sub_keys': nrm(ks[17], (L, PEER_HEADS, 2, N_KEYS, PEER_HALF), PEER_HALF ** -0.5),
        'peer_u': nrm(ks[18], (L, N_EXPERTS, D_MODEL), D_MODEL ** -0.5),
        'peer_v': nrm(ks[19], (L, N_EXPERTS, D_MODEL), PEER_HEADS ** -0.5),
        'ple_norm_g': 1.0 + nrm(ks[20], (L, D_MODEL), 0.02),
        'ple_gate_w': nrm(ks[21], (L, D_MODEL, D_MODEL), D_MODEL ** -0.5),
        'ple_proj_w': nrm(ks[22], (L, D_PLE, D_MODEL), D_PLE ** -0.5),
    }


def reference(x, p, mix_norm_g, w_in, q_norm_g, k_norm_g, lambda_q, lambda_k, subln_g,
              glu_b, dw_kernel, dw_b, conv_ln_g, conv_ln_b, w_out, ffn_norm_g,
              peer_w_query, peer_sub_keys, peer_u, peer_v, ple_norm_g, ple_gate_w, ple_proj_w):
    B, S = x.shape[0], x.shape[1]
    for i in range(DEPTH):
        h = rms_norm(x, mix_norm_g[i])
        z = h @ w_in[i]
        zq, zk, zv, zc = jnp.split(z, [QK_COLS, 2 * QK_COLS, 2 * QK_COLS + ATTN_WIDTH], axis=-1)
        lam_init = 0.8 - 0.6 * math.exp(-0.3 * i)
        lam = (jnp.exp(jnp.sum(lambda_q[i, 0].astype(jnp.float32) * lambda_k[i, 0].astype(jnp.float32)))
               - jnp.exp(jnp.sum(lambda_q[i, 1].astype(jnp.float32) * lambda_k[i, 1].astype(jnp.float32)))
               + lam_init)
        attn = diff_attention(
            zq.reshape(B, S, ATTN_HEADS, 2, ATTN_QK_DIM),
            zk.reshape(B, S, ATTN_HEADS, 2, ATTN_QK_DIM),
            zv.reshape(B, S, ATTN_HEADS, ATTN_V_DIM),
            lam, lam_init, q_norm_g[i], k_norm_g[i], subln_g[i])
        conv = conformer_conv(zc, glu_b[i], dw_kernel[i], dw_b[i], conv_ln_g[i], conv_ln_b[i])
        x = x + jnp.concatenate([attn, conv], axis=-1) @ w_out[i]
        x = x + peer(rms_norm(x, ffn_norm_g[i]), peer_w_query[i], peer_sub_keys[i], peer_u[i], peer_v[i])
        gate = jax.nn.sigmoid(rms_norm(x, ple_norm_g[i]) @ ple_gate_w[i])
        x = x + (p[i] @ ple_proj_w[i]) * gate
    return x
```

```python
import os
import numpy as np
import concourse.bass as bass
import concourse.mybir as mybir
from concourse.bass_utils import run_bass_kernel_spmd
from contextlib import ExitStack

F32 = mybir.dt.float32
BF16 = mybir.dt.bfloat16
AF = mybir.ActivationFunctionType
ALU = mybir.AluOpType

EPS = 1e-6
T = 1024
ENGS = ["sync", "scalar", "vector", "tensor", "gpsimd"]
NRING = 8

G_MIX, G_FFN, G_PLE, QKG, LAM, SUBG, GLUB, DWB, LNG, LNB, FLAGS, DWW = 0, 32, 64, 96, 100, 104, 106, 138, 154, 170, 186, 188
NCONST = DWW + 16 * 31
CM_ID, CM_ONES, CM_MASK = 0, 128, 256
NCM = 256 + 4 * 512


class _Op:
    __slots__ = ("eng", "fn", "deps", "dma", "sem", "val", "need", "guard")

    def __init__(self, eng, fn, dma):
        self.eng = eng
        self.fn = fn
        self.dma = dma
        self.deps = []
        self.sem = None
        self.val = 0
        self.need = dma
        self.guard = None


class Phase:
    def __init__(self, nc, pid, G):
        self.nc = nc
        self.pid = pid
        self.G = G
        self.es = ExitStack()
        self.ops = {e: [] for e in ENGS}
        self.state = {}
        self.n = 0
        self.pend = []

    def sb(self, name, shape, dtype):
        self.n += 1
        return self.es.enter_context(self.nc.sbuf_tensor(f"{name}_{self.pid}_{self.n}", shape, dtype))

    def ps(self, name, shape=(128, 512), dtype=F32):
        self.n += 1
        return self.es.enter_context(self.nc.psum_tensor(f"{name}_{self.pid}_{self.n}", list(shape), dtype))

    def _rec(self, op, reads, writes):
        for k in reads:
            st = self.state.setdefault(k, [None, []])
            if st[0] is not None:
                op.deps.append((st[0], True))
            st[1].append(op)
        for k in writes:
            st = self.state.setdefault(k, [None, []])
            if st[0] is not None and st[0] is not op:
                op.deps.append((st[0], False))
            for r in st[1]:
                if r is not op:
                    op.deps.append((r, False))
            st[0] = op
            st[1] = []
        self.ops[op.eng].append(op)
        return op

    def op(self, eng, fn, reads=(), writes=()):
        return self._rec(_Op(eng, fn, False), reads, writes)

    def dma(self, eng, out, in_, reads=(), writes=()):
        return self._rec(_Op(eng, lambda e: e.dma_start(out=out, in_=in_), True), reads, writes)

    def mm(self, out, lhsT, rhs, start, stop, reads, writes):
        return self.op("tensor", lambda e: e.matmul(out, lhsT, rhs, start=start, stop=stop), reads, writes)

    def act(self, out, in_, func, reads, writes, **kw):
        return self.op("scalar", lambda e: e.activation(out=out, in_=in_, func=func, **kw), reads, writes)

    def ts(self, out, in0, s1, s2, op0, op1, reads, writes, eng="vector"):
        if op1 is None:
            return self.op(eng, lambda e: e.tensor_scalar(out=out, in0=in0, scalar1=s1, scalar2=None, op0=op0), reads, writes)
        return self.op(eng, lambda e: e.tensor_scalar(out=out, in0=in0, scalar1=s1, scalar2=s2, op0=op0, op1=op1), reads, writes)

    def stt(self, out, in0, scalar, in1, op0, op1, reads, writes):
        return self.op("vector", lambda e: e.scalar_tensor_tensor(out=out, in0=in0, scalar=scalar, in1=in1, op0=op0, op1=op1), reads, writes)

    def tt(self, out, in0, in1, op, reads, writes, eng="vector"):
        return self.op(eng, lambda e: e.tensor_tensor(out=out, in0=in0, in1=in1, op=op), reads, writes)

    def step(self, deferred):
        old = self.pend
        self.pend = list(deferred)
        for f in old:
            f()

    def flush(self):
        self.step([])

    def emit(self):
        self.flush()
        nc = self.nc
        es = self.es
        G = self.G
        if "esem" not in G:
            kes = G["kes"]
            G["esem"] = {e: kes.enter_context(nc.semaphore(f"e{e}")) for e in ENGS}
            G["rings"] = {e: [kes.enter_context(nc.semaphore(f"d{e}{i}")) for i in range(NRING)]
                          for e in ("sync", "scalar", "gpsimd")}
            G["cnt"] = {e: 0 for e in ENGS}
            G["rcnt"] = {e: [0] * NRING for e in ENGS}
            G["ri"] = {e: 0 for e in ENGS}
        esem, rings = G["esem"], G["rings"]

        def needs_wait(op, dep, raw):
            if dep.dma or op.dma:
                return True
            if dep.eng != op.eng:
                return True
            if op.eng == "tensor":
                return False
            return raw

        for e in ENGS:
            for op in self.ops[e]:
                for dep, raw in op.deps:
                    if needs_wait(op, dep, raw):
                        dep.need = True
        final = {}
        for e in ENGS:
            cnt = G["cnt"][e]
            rcnt = G["rcnt"][e]
            ri = G["ri"][e]
            for op in self.ops[e]:
                if op.dma:
                    op.sem = rings[e][ri]
                    op.guard = rcnt[ri]
                    rcnt[ri] += 16
                    op.val = rcnt[ri]
                    ri = (ri + 1) % NRING
                elif op.need:
                    cnt += 1
                    op.sem = esem[e]
                    op.val = cnt
            final[e] = list(zip(rings.get(e, []), list(rcnt)))
            G["cnt"][e] = cnt
            G["ri"][e] = ri
        ops = self.ops

        def body(ename):
            def f(eng):
                waited = {}
                for op in ops[ename]:
                    ws = {}
                    for dep, raw in op.deps:
                        if needs_wait(op, dep, raw):
                            k = id(dep.sem)
                            if ws.get(k, (None, 0))[1] < dep.val:
                                ws[k] = (dep.sem, dep.val)
                    if op.dma and op.guard > 0:
                        k = id(op.sem)
                        if ws.get(k, (None, 0))[1] < op.guard:
                            ws[k] = (op.sem, op.guard)
                    for k, (sem, val) in ws.items():
                        if waited.get(k, 0) < val:
                            eng.wait_ge(sem, val)
                            waited[k] = val
                    ins = op.fn(eng)
                    if op.dma:
                        ins.then_inc(op.sem, 16)
                    elif op.need:
                        ins.then_inc(op.sem, 1)
                for sem, val in final[ename]:
                    if val > 0 and waited.get(id(sem), 0) < val:
                        eng.wait_ge(sem, val)
            return f

        with nc.Block() as block:
            for e in ENGS:
                getattr(block, e)(body(e))
        es.close()


def wstream(ph, tiles, bufs, compute):
    nb = len(bufs)

    def issue(i):
        buf, key = bufs[i % nb]
        for ent in tiles[i]:
            dst_fn, src, eng = ent[:3]
            ph.dma(eng, dst_fn(buf), src, writes=[ent[3] if len(ent) > 3 else key])

    for i in range(min(nb - 1, len(tiles))):
        issue(i)
    for i in range(len(tiles)):
        if i + nb - 1 < len(tiles):
            issue(i + nb - 1)
        compute(i, *bufs[i % nb])


def build_nc(nph=99, debug=False):
    nc = bass.Bass("TRN2", target_bir_lowering=False)

    def din(name, shape):
        return nc.dram_tensor(name, list(shape), F32, kind="ExternalInput").ap()

    def scratch(name, shape, dt):
        return nc.dram_tensor(name, list(shape), dt, kind="ExternalOutput" if debug else "Internal").ap()

    xo = din("xo", (4096, T))
    xc = din("xc", (4096, T))
    pT = din("pT", (256, T))
    consts_d = din("consts", (128, NCONST))
    cm_d = din("cm", (128, NCM))
    w_in = din("w_in", (4096, 10240))
    w_out = din("w_out", (4096, 4096))
    wq = din("wq", (4096, 2048))
    ksub = din("ksub", (16, 128, 128))
    uT = din("uT", (4096, 16384))
    vv = din("v", (16384, 4096))
    wg = din("wg", (4096, 4096))
    wp = din("wp", (256, 4096))
    outT = nc.dram_tensor("outT", [4096, T], F32, kind="ExternalOutput").ap()

    qT_s = scratch("qT_s", (2048, T), BF16)
    kT_s = scratch("kT_s", (2048, 2 * T), BF16)
    v_s = scratch("v_s", (2 * T, 2048), BF16)
    y_s = scratch("y_s", (2048, 128 + T), F32)
    x2T = scratch("x2T", (4096, T), F32)
    x3T = scratch("x3T", (4096, T), F32)
    bS = scratch("bS", (128, 8 * 8 * 128), F32)
    s2S = scratch("s2S", (128, 8 * 8 * 128), F32)
    ecS = scratch("ecS", (128, 64), F32)
    rS = scratch("rS", (128, T), F32)
    PT = scratch("PT", (16384, T), BF16)
    mix_dbg = scratch("mix_dbg", (4096, T), BF16) if debug else None

    kes = ExitStack()
    act = kes.enter_context(nc.sbuf_tensor("act", [128, 32, T], BF16))
    hes = ExitStack()
    halo = hes.enter_context(nc.sbuf_tensor("halo", [128, 32, 128], BF16))
    halo_r = hes.enter_context(nc.sbuf_tensor("halo_r", [128, 128], F32))
    pid = [0]
    G = {"kes": kes}

    def new_phase():
        pid[0] += 1
        return Phase(nc, pid[0], G)

    def chunked(ap):
        return ap.rearrange("(c p) n -> p c n", p=128)

    def load_consts(ph):
        c = ph.sb("consts", [128, NCONST], F32)
        ph.dma("sync", c[:], consts_d, writes=["consts"])
        return c

    def load_cm(ph, lo, hi, name):
        t = ph.sb(name, [128, hi - lo], BF16)
        ph.dma("gpsimd", t[:], cm_d[:, lo:hi], writes=[name])
        return t

    def norm_to_act(ph, src, gcol, consts, ones, two_pass=False, rstd_src=None):
        xv = chunked(src)
        xb = [ph.sb("xb", [128, T], F32) for _ in range(3)]
        if rstd_src is not None:
            rstd = ph.sb("rstd", [128, T], F32)
            ph.dma("sync", rstd[:], rstd_src, writes=["rstd0", "rstd1"])
            for c in range(32):
                x_, xk = xb[c % 3], f"xb{c % 3}"
                ph.dma("sync", x_[:], xv[:, c, :], writes=[xk])
                ph.stt(act[:, c, :], x_[:], consts[:, gcol + c:gcol + c + 1], rstd[:], ALU.mult, ALU.mult,
                       [xk, "rstd0", "rstd1", "consts"], [f"act{c}"])
            return rstd, None
        ss = [ph.ps("ss") for _ in range(2)]
        sq = [ph.sb("sq", [128, T], BF16) for _ in range(2)]
        for c in range(32):
            x_, xk = xb[c % 3], f"xb{c % 3}"
            s_, sk = sq[c % 2], f"sq{c % 2}"
            ph.dma("sync", x_[:], xv[:, c, :], writes=[xk])
            ph.tt(s_[:], x_[:], x_[:], ALU.mult, [xk], [sk], eng="gpsimd")
            if not two_pass:
                ph.ts(act[:, c, :], x_[:], consts[:, gcol + c:gcol + c + 1], None, ALU.mult, None, [xk, "consts"], [f"act{c}"])
            for hf in range(2):
                ph.mm(ss[hf][:], ones[:], s_[:, hf * 512:(hf + 1) * 512], c == 0, c == 31, [sk, "ones"], [f"ss{hf}"])
        rstd = ph.sb("rstd", [128, T], F32)
        tmp = ph.sb("tmpn", [128, T], F32)
        for hf in range(2):
            sl = slice(hf * 512, (hf + 1) * 512)
            ph.act(tmp[:, sl], ss[hf][:], AF.Ln, [f"ss{hf}"], [f"tmpn{hf}"], scale=1.0 / 4096, bias=EPS)
            ph.act(rstd[:, sl], tmp[:, sl], AF.Exp, [f"tmpn{hf}"], [f"rstd{hf}"], scale=-0.5)
        if two_pass:
            for c in range(32):
                x_, xk = xb[c % 3], f"xb{c % 3}"
                ph.dma("sync", x_[:], xv[:, c, :], writes=[xk])
                ph.stt(act[:, c, :], x_[:], consts[:, gcol + c:gcol + c + 1], rstd[:], ALU.mult, ALU.mult,
                       [xk, "rstd0", "rstd1", "consts"], [f"act{c}"])
        return rstd, ss

    def proj_phase(is_ctx):
        ph = new_phase()
        consts = load_consts(ph)
        ones = load_cm(ph, CM_ONES, CM_ONES + 128, "ones")
        rstd, ss = norm_to_act(ph, xc if is_ctx else xo, G_MIX, consts, ones)
        identf = ph.sb("identf", [128, 2], F32)
        ph.dma("sync", identf[:], cm_d[:, CM_ID:CM_ID + 2], writes=["identf"])
        for tt in range(8):
            ph.mm(ss[0][:, 2 * tt:2 * tt + 2], rstd[:, tt * 128:(tt + 1) * 128], identf[:], True, True,
                  ["rstd0", "rstd1", "identf"], ["ss0"])
        rcol = ph.sb("rcol", [128, 16], F32)
        ph.act(rcol[:], ss[0][:, 0:16], AF.Copy, ["ss0"], ["rcol"])
        if is_ctx:
            ph.op("gpsimd", lambda e: e.tensor_copy(out=halo[:], in_=act[:, :, 896:1024]), [f"act{c}" for c in range(32)], ["halo"])
            ph.op("gpsimd", lambda e: e.tensor_copy(out=halo_r[:], in_=rstd[:, 896:1024]), ["rstd1"], ["halo_r"])
        qkg = ph.sb("qkg", [128, 4], F32)
        ph.ts(qkg[:, 0:2], consts[:, QKG:QKG + 2], 128.0 ** -0.5, None, ALU.mult, None, ["consts"], ["qkg"])
        ph.ts(qkg[:, 2:4], consts[:, QKG + 2:QKG + 4], 1.0, None, ALU.mult, None, ["consts", "qkg"], ["qkg"])
        wv = chunked(w_in)
        tokbase = 0 if is_ctx else T
        pq = [ph.ps("pq") for _ in range(4)]
        ssp = [ph.ps("ssp") for _ in range(2)]
        zc = [ph.sb("zc", [128, 512], F32) for _ in range(3)]
        sqb = [ph.sb("sqb", [128, 512], BF16) for _ in range(3)]
        lnb_ = [ph.sb("lnt", [128, 512], F32) for _ in range(2)]
        rs_ = [ph.sb("rs", [128, 512], F32) for _ in range(2)]
        ob = [ph.sb("ob", [128, 512], BF16) for _ in range(3)]
        vb = [ph.sb("vb", [128, 256], BF16) for _ in range(3)]
        sg = [ph.sb("sg", [128, 512], F32) for _ in range(2)]
        yb = [ph.sb("yb", [128, 512], F32) for _ in range(3)]
        wb = [(ph.sb("wb", [128, 32, 256], BF16), f"wb{i}") for i in range(2)]
        cnt = {"u": 0, "v": 0, "y": 0}
        allact = [f"act{c}" for c in range(32)]

        tiles = []
        kinds = []
        if not is_ctx:
            for i in range(8):
                tiles.append([(lambda b: b[:], wv[:, :, i * 256:(i + 1) * 256], "gpsimd")])
                kinds.append(("q", i))
        for i in range(8):
            tiles.append([(lambda b: b[:], wv[:, :, 2048 + i * 256:2048 + (i + 1) * 256], "gpsimd")])
            kinds.append(("k", i))
        for i in range(8):
            tiles.append([(lambda b: b[:], wv[:, :, 4096 + i * 256:4096 + (i + 1) * 256], "gpsimd")])
            kinds.append(("v", i))
        for j in range(0 if is_ctx else 16):
            tiles.append([(lambda b: b[:, :, 0:128], wv[:, :, 6144 + j * 128:6144 + (j + 1) * 128], "gpsimd"),
                          (lambda b: b[:, :, 128:256], wv[:, :, 8192 + j * 128:8192 + (j + 1) * 128], "gpsimd")])
            kinds.append(("c", j))

        def qk_unit(kind, chunk, hf, buf, bkey, cc):
            u = cnt["u"]
            cnt["u"] += 1
            ps, pk = pq[u % 4], f"pq{u % 4}"
            for kc in range(32):
                ph.mm(ps[:], buf[:, kc, cc * 128:(cc + 1) * 128], act[:, kc, hf * 512:(hf + 1) * 512],
                      kc == 0, kc == 31, [bkey, f"act{kc}"], [pk])
            z, zk = zc[u % 3], f"zc{u % 3}"
            s, sk = sqb[u % 3], f"sqb{u % 3}"
            ph.tt(z[:], ps[:], rstd[:, hf * 512:(hf + 1) * 512], ALU.mult, [pk, f"rstd{hf}"], [zk])
            ph.tt(s[:], z[:], z[:], ALU.mult, [zk], [sk])

            def cont():
                sp, spk = ssp[u % 2], f"ssp{u % 2}"
                ph.mm(sp[:], ones[:], s[:], True, True, [sk, "ones"], [spk])
                l, lk = lnb_[u % 2], f"lnt{u % 2}"
                r, rk = rs_[u % 2], f"rs{u % 2}"
                ph.act(l[:], sp[:], AF.Ln, [spk], [lk], scale=1.0 / 128, bias=EPS)
                ph.act(r[:], l[:], AF.Exp, [lk], [rk], scale=-0.5)
                o, ok = ob[u % 3], f"ob{u % 3}"
                m = chunk % 2
                gc = m if kind == "q" else 2 + m
                ph.stt(o[:], z[:], qkg[:, gc:gc + 1], r[:], ALU.mult, ALU.mult, [zk, rk, "qkg"], [ok])
                if kind == "q":
                    dst = qT_s[chunk * 128:(chunk + 1) * 128, hf * 512:(hf + 1) * 512]
                else:
                    dst = kT_s[chunk * 128:(chunk + 1) * 128, tokbase + hf * 512:tokbase + (hf + 1) * 512]
                ph.dma("sync", dst, o[:], reads=[ok])
            ph.step([cont])

        def compute(i, buf, bkey):
            kind, idx = kinds[i]
            if kind in ("q", "k"):
                for cc in range(2):
                    for hf in range(2):
                        qk_unit(kind, idx * 2 + cc, hf, buf, bkey, cc)
            elif kind == "v":
                for tt in range(8):
                    u = cnt["u"]
                    cnt["u"] += 1
                    ps, pk = pq[u % 4], f"pq{u % 4}"
                    for kc in range(32):
                        ph.mm(ps[:, 0:256], act[:, kc, tt * 128:(tt + 1) * 128], buf[:, kc, :],
                              kc == 0, kc == 31, [bkey, f"act{kc}"], [pk])
                    n = cnt["v"]
                    cnt["v"] += 1
                    o, ok = vb[n % 3], f"vb{n % 3}"
                    ph.act(o[:], ps[:, 0:256], AF.Identity, [pk, "rcol"], [ok], scale=rcol[:, 2 * tt:2 * tt + 1])
                    ph.dma("sync", v_s[tokbase + tt * 128:tokbase + (tt + 1) * 128, idx * 256:(idx + 1) * 256], o[:], reads=[ok])
                    ph.step([])
            else:
                j = idx
                units = [(0, 512, 128), (512, 512, 640), (-1, 128, 0)]
                for t0, n, d0 in units:
                    u = cnt["u"]
                    cnt["u"] += 2
                    pa, pak = pq[u % 4], f"pq{u % 4}"
                    pg, pgk = pq[(u + 1) % 4], f"pq{(u + 1) % 4}"
                    rhs = (lambda kc: halo[:, kc, :]) if t0 < 0 else (lambda kc: act[:, kc, t0:t0 + n])
                    for kc in range(32):
                        ph.mm(pa[:, 0:n], buf[:, kc, 0:128], rhs(kc), kc == 0, kc == 31, [bkey, f"act{kc}"], [pak])
                    for kc in range(32):
                        ph.mm(pg[:, 0:n], buf[:, kc, 128:256], rhs(kc), kc == 0, kc == 31, [bkey, f"act{kc}"], [pgk])
                    k = cnt["y"]
                    cnt["y"] += 1
                    s, sk = sg[k % 2], f"sg{k % 2}"
                    y, yk = yb[k % 3], f"yb{k % 3}"
                    rsl = halo_r[:, 0:128] if t0 < 0 else rstd[:, t0:t0 + n]
                    rkey = "halo_r" if t0 < 0 else f"rstd{t0 // 512}"
                    ph.tt(s[:, 0:n], pg[:, 0:n], rsl, ALU.mult, [pgk, rkey], [sk])
                    ph.act(s[:, 0:n], s[:, 0:n], AF.Sigmoid, [sk, "consts"], [sk], bias=consts[:, GLUB + 16 + j:GLUB + 17 + j])
                    ph.tt(y[:, 0:n], pa[:, 0:n], rsl, ALU.mult, [pak, rkey], [yk])
                    ph.stt(y[:, 0:n], y[:, 0:n], consts[:, GLUB + j:GLUB + j + 1], s[:, 0:n], ALU.add, ALU.mult,
                           [yk, sk, "consts"], [yk])
                    ph.dma("sync", y_s[j * 128:(j + 1) * 128, d0:d0 + n], y[:, 0:n], reads=[yk])
                    ph.step([])

        wstream(ph, tiles, wb, compute)
        ph.emit()

    def attn_phase():
        ph = new_phase()
        consts = load_consts(ph)
        ones = load_cm(ph, CM_ONES, CM_ONES + 128, "ones")
        cmask = load_cm(ph, CM_MASK, CM_MASK + 2048, "cmask")
        prod = ph.sb("prod", [128, 2], BF16)
        ph.tt(prod[:], consts[:, LAM:LAM + 2], consts[:, LAM + 2:LAM + 4], ALU.mult, ["consts"], ["prod"])
        lps = ph.ps("ssps")
        ph.mm(lps[:, 0:2], ones[:], prod[:], True, True, ["prod", "ones"], ["ssps"])
        el = ph.sb("el", [128, 2], F32)
        ph.act(el[:], lps[:, 0:2], AF.Exp, ["ssps"], ["el"])
        negl = ph.sb("negl", [128, 1], F32)
        ph.tt(negl[:], el[:, 1:2], el[:, 0:1], ALU.subtract, ["el"], ["negl"])
        ph.ts(negl[:], negl[:], -0.2, None, ALU.add, None, ["negl"], ["negl"])
        gs = ph.sb("gs", [128, 2], F32)
        ph.ts(gs[:], consts[:, SUBG:SUBG + 2], 0.8, None, ALU.mult, None, ["consts"], ["gs"])

        qv, kv = chunked(qT_s), chunked(kT_s)
        vview = v_s.rearrange("(t p) n -> p t n", p=128)
        qh = [ph.sb("qh", [128, 2, T], BF16) for _ in range(2)]
        kh = [ph.sb("kh", [128, 2, 2 * T], BF16) for _ in range(2)]
        vh = [ph.sb("vh", [128, 16, 256], BF16) for _ in range(2)]
        sT = [ph.ps("sT") for _ in range(3)]
        den = ph.ps("den")
        O = [ph.ps("O") for _ in range(2)]
        pT_ = [ph.sb("pT", [128, 512], BF16) for _ in range(5)]
        rden = ph.sb("rden", [128, 512], F32)
        R = [[ph.sb("R", [128, 512], F32) for _ in range(2)] for _ in range(2)]
        sq = [ph.sb("sqa", [128, 512], BF16) for _ in range(2)]
        lt = ph.sb("lt", [128, 512], F32)
        rs = ph.sb("rsa", [128, 512], F32)
        pcount = [0]

        def load_head(h):
            b = h % 2
            ph.dma("sync", qh[b][:], qv[:, 2 * h:2 * h + 2, :], writes=[f"qh{b}"])
            ph.dma("sync", kh[b][:], kv[:, 2 * h:2 * h + 2, :], writes=[f"kh{b}"])
            ph.dma("sync", vh[b][:], vview[:, :, h * 256:(h + 1) * 256], writes=[f"vh{b}"])

        load_head(0)
        items = []
        for h in range(8):
            for qb in range(2):
                nj = 12 if qb == 0 else 16
                for m in range(2):
                    for j in range(nj):
                        items.append((h, qb, m, j, nj))
        NS, NP, LA = 3, 5, 2

        def S(p):
            h, qb, m, j, nj = items[p]
            b = h % 2
            st, sk = sT[p % NS], f"sT{p % NS}"
            ph.mm(st[:], kh[b][:, m, j * 128:(j + 1) * 128], qh[b][:, m, qb * 512:(qb + 1) * 512],
                  True, True, [f"kh{b}", f"qh{b}"], [sk])
            pt, pk = pT_[p % NP], f"pT{p % NP}"
            if j < 8:
                ph.act(pt[:], st[:], AF.Exp, [sk, "consts"], [pk], bias=consts[:, FLAGS + 1:FLAGS + 2])
            else:
                ph.act(pt[:], st[:], AF.Exp, [sk], [pk])
            o = j - 8 - 4 * qb
            if o >= 0:
                ph.tt(pt[:], pt[:], cmask[:, o * 512:(o + 1) * 512], ALU.mult, [pk, "cmask"], [pk])

        def PV(p):
            h, qb, m, j, nj = items[p]
            b = h % 2
            pt, pk = pT_[p % NP], f"pT{p % NP}"
            ph.mm(den[:], ones[:], pt[:], j == 0, j == nj - 1, [pk, "ones"], ["den"])
            for a in range(2):
                ph.mm(O[a][:], vh[b][:, j, a * 128:(a + 1) * 128], pt[:], j == 0, j == nj - 1, [pk, f"vh{b}"], [f"O{a}"])

        for p in range(min(LA, len(items))):
            S(p)
        Oc = [ph.sb("Oc", [128, 512], F32) for _ in range(2)]
        later = []
        for p in range(len(items)):
            while later and later[0][0] <= p:
                later.pop(0)[1]()
            h, qb, m, j, nj = items[p]
            if p + LA < len(items):
                S(p + LA)
            if qb == 0 and m == 0 and j == 0 and h + 1 < 8:
                load_head(h + 1)
            PV(p)
            if j == nj - 1:
                for a in range(2):
                    ph.act(Oc[a][:], O[a][:], AF.Copy, [f"O{a}"], [f"Oc{a}"])
                ph.op("vector", lambda e: e.reciprocal(out=rden[:], in_=den[:]), ["den"], ["rden"])
                for a in range(2):
                    ph.tt(R[m][a][:], Oc[a][:], rden[:], ALU.mult, [f"Oc{a}", "rden"], [f"R{m}{a}"])
                if m == 1:
                    for a in range(2):
                        ph.stt(R[0][a][:], R[1][a][:], negl[:, 0:1], R[0][a][:], ALU.mult, ALU.add, [f"R1{a}", f"R0{a}", "negl"], [f"R0{a}"])
                        ph.tt(sq[a][:], R[0][a][:], R[0][a][:], ALU.mult, [f"R0{a}"], [f"sqa{a}"])

                    def fin(h=h, qb=qb):
                        for a in range(2):
                            ph.mm(lps[:], ones[:], sq[a][:], a == 0, a == 1, [f"sqa{a}", "ones"], ["ssps"])
                        ph.act(lt[:], lps[:], AF.Ln, ["ssps"], ["lt"], scale=1.0 / 256, bias=EPS)
                        ph.act(rs[:], lt[:], AF.Exp, ["lt"], ["rsa"], scale=-0.5)
                        for a in range(2):
                            ph.stt(act[:, 2 * h + a, qb * 512:(qb + 1) * 512], R[0][a][:], gs[:, a:a + 1], rs[:], ALU.mult, ALU.mult,
                                   [f"R0{a}", "rsa", "gs"], [f"act{2 * h + a}_{qb}"])
                    later.append((p + 4, fin))
        for _, fn in later:
            fn()
        ph.emit()

    def conv_phase():
        ph = new_phase()
        consts = load_consts(ph)
        ones = load_cm(ph, CM_ONES, CM_ONES + 128, "ones")
        ident = load_cm(ph, CM_ID, CM_ID + 128, "ident")
        yb = [ph.sb("ybc", [128, 128 + T], F32) for _ in range(2)]
        ybf = [ph.sb("ybf", [128, 128 + T], BF16) for _ in range(2)]
        dg = [ph.sb("dg", [128, 31, 128], BF16) for _ in range(2)]
        co = ph.sb("co", [128, 16, T], F32)
        sqb = [ph.sb("sqc", [128, T], BF16) for _ in range(2)]
        cb = [ph.sb("cbc", [128, T], BF16) for _ in range(2)]
        s1 = [ph.ps("s1") for _ in range(2)]
        s2 = [ph.ps("s2") for _ in range(2)]
        cps = [ph.ps("cps") for _ in range(4)]
        def prep(j):
            y, yk = yb[j % 2], f"ybc{j % 2}"
            ph.dma("sync", y[:], y_s[j * 128:(j + 1) * 128, :], writes=[yk])
            ph.ts(y[:, 0:128], y[:, 0:128], consts[:, FLAGS:FLAGS + 1], None, ALU.mult, None, [yk, "consts"], [yk])
            y16, y16k = ybf[j % 2], f"ybf{j % 2}"
            ph.act(y16[:], y[:], AF.Copy, [yk], [y16k])
            d, dk = dg[j % 2], f"dg{j % 2}"
            w0 = DWW + j * 31
            in0 = bass.AP(ident, 0, [[128, 128], [0, 31], [1, 128]])
            in1 = bass.AP(consts, w0, [[NCONST, 128], [1, 31], [0, 128]])
            ph.tt(d[:], in0, in1, ALU.mult, ["ident", "consts"], [dk])

        def stats(j):
            s, sk = sqb[j % 2], f"sqc{j % 2}"
            c, cbk = cb[j % 2], f"cbc{j % 2}"
            for hf in range(2):
                sl = slice(hf * 512, (hf + 1) * 512)
                ph.mm(s1[hf][:], ones[:], c[:, sl], j == 0, j == 15, [cbk, "ones"], [f"s1{hf}"])
                ph.mm(s2[hf][:], ones[:], s[:, sl], j == 0, j == 15, [sk, "ones"], [f"s2{hf}"])

        prep(0)
        for j in range(16):
            if j + 1 < 16:
                prep(j + 1)
            y16, y16k = ybf[j % 2], f"ybf{j % 2}"
            d, dk = dg[j % 2], f"dg{j % 2}"
            ck = f"co{j}"
            for hf in range(2):
                cp, cpk = cps[(2 * j + hf) % 4], f"cps{(2 * j + hf) % 4}"
                for t in range(31):
                    o0 = 98 + t + hf * 512
                    ph.mm(cp[:], d[:, t, :], y16[:, o0:o0 + 512], t == 0, t == 30, [dk, y16k], [cpk])
                ph.act(co[:, j, hf * 512:(hf + 1) * 512], cp[:], AF.Identity, [cpk, "consts"], [f"{ck}_{hf}"],
                       bias=consts[:, DWB + j:DWB + j + 1])
            s, sk = sqb[j % 2], f"sqc{j % 2}"
            c, cbk = cb[j % 2], f"cbc{j % 2}"
            ph.tt(s[:], co[:, j, :], co[:, j, :], ALU.mult, [f"{ck}_0", f"{ck}_1"], [sk], eng="gpsimd")
            ph.act(c[:], co[:, j, :], AF.Copy, [f"{ck}_0", f"{ck}_1"], [cbk])
            ph.step([lambda j=j: stats(j)])
        ph.flush()
        mu = ph.sb("mu", [128, T], F32)
        var = ph.sb("var", [128, T], F32)
        rstd = ph.sb("rstdc", [128, T], F32)
        for hf in range(2):
            sl = slice(hf * 512, (hf + 1) * 512)
            ph.ts(mu[:, sl], s1[hf][:], 1.0 / 2048, None, ALU.mult, None, [f"s1{hf}"], [f"mu{hf}"])
            ph.tt(var[:, sl], mu[:, sl], mu[:, sl], ALU.mult, [f"mu{hf}"], [f"var{hf}"])
            ph.stt(var[:, sl], s2[hf][:], 1.0 / 2048, var[:, sl], ALU.mult, ALU.subtract, [f"s2{hf}", f"var{hf}"], [f"var{hf}"])
            ph.act(var[:, sl], var[:, sl], AF.Ln, [f"var{hf}"], [f"var{hf}"], bias=EPS)
            ph.act(rstd[:, sl], var[:, sl], AF.Exp, [f"var{hf}"], [f"rstdc{hf}"], scale=-0.5)
        tb = [ph.sb("tbc", [128, T], F32) for _ in range(2)]
        ph.stt(mu[:], mu[:], -1.0, rstd[:], ALU.mult, ALU.mult, ["mu0", "mu1", "rstdc0", "rstdc1"], ["nmr"])
        for j in range(16):
            t, tk = tb[j % 2], f"tbc{j % 2}"
            ph.stt(t[:], co[:, j, :], consts[:, LNG + j:LNG + j + 1], rstd[:], ALU.mult, ALU.mult,
                   [f"co{j}_0", f"co{j}_1", "rstdc0", "rstdc1", "consts"], [tk])
            ph.stt(t[:], mu[:], consts[:, LNG + j:LNG + j + 1], t[:], ALU.mult, ALU.add, ["nmr", tk, "consts"], [tk])
            ph.act(act[:, 16 + j, :], t[:], AF.Silu, [tk, "consts"], [f"act{16 + j}"], bias=consts[:, LNB + j:LNB + j + 1])
        if debug:
            ph.dma("sync", chunked(mix_dbg), act[:], reads=[f"act{16 + j}" for j in range(16)])
        ph.emit()

    def gemm_phase_units(ph, wview, ntiles, wb, unit_fn):
        tiles = [[(lambda b: b[:], wview[:, :, i * 256:(i + 1) * 256], "gpsimd")] for i in range(ntiles)]
        pq = [ph.ps("pq") for _ in range(4)]
        cnt = [0]

        def compute(i, buf, bkey):
            for cc in range(2):
                for hf in range(2):
                    u = cnt[0]
                    cnt[0] += 1
                    ps, pk = pq[u % 4], f"pq{u % 4}"
                    for kc in range(32):
                        ph.mm(ps[:], buf[:, kc, cc * 128:(cc + 1) * 128], act[:, kc, hf * 512:(hf + 1) * 512],
                              kc == 0, kc == 31, [bkey, f"act{kc}"], [pk])
                    unit_fn(u, i * 2 + cc, hf, ps, pk)

        wstream(ph, tiles, wb, compute)

    def outproj_phase():
        ph = new_phase()
        wb = [(ph.sb("wb", [128, 32, 256], BF16), f"wb{i}") for i in range(2)]
        xs = [ph.sb("xs", [128, 512], F32) for _ in range(3)]
        xov = chunked(xo)
        x2v = chunked(x2T)
        ones = load_cm(ph, CM_ONES, CM_ONES + 128, "ones")
        ss = [ph.ps("ss") for _ in range(2)]
        sqo = [ph.sb("sqo", [128, 512], BF16) for _ in range(3)]

        def unit(u, chunk, hf, ps, pk):
            x_, xk = xs[u % 3], f"xs{u % 3}"
            sl = slice(hf * 512, (hf + 1) * 512)
            ph.dma("sync", x_[:], xov[:, chunk, sl], writes=[xk])
            ph.tt(x_[:], ps[:], x_[:], ALU.add, [pk, xk], [xk])
            ph.dma("sync", x2v[:, chunk, sl], x_[:], reads=[xk])
            q, qk = sqo[u % 3], f"sqo{u % 3}"
            ph.tt(q[:], x_[:], x_[:], ALU.mult, [xk], [qk], eng="gpsimd")

            def cont():
                ph.mm(ss[hf][:], ones[:], q[:], chunk == 0, chunk == 31, [qk, "ones"], [f"ss{hf}"])
            ph.step([cont])

        gemm_phase_units(ph, chunked(w_out), 16, wb, unit)
        ph.flush()
        rsd = ph.sb("rsd", [128, T], F32)
        for hf in range(2):
            sl = slice(hf * 512, (hf + 1) * 512)
            ph.act(rsd[:, sl], ss[hf][:], AF.Ln, [f"ss{hf}"], [f"rsd{hf}"], scale=1.0 / 4096, bias=EPS)
            ph.act(rsd[:, sl], rsd[:, sl], AF.Exp, [f"rsd{hf}"], [f"rsd{hf}"], scale=-0.5)
        ph.dma("sync", rS, rsd[:], reads=["rsd0", "rsd1"])
        ph.emit()

    def peer_score_phase():
        ph = new_phase()
        consts = load_consts(ph)
        ones = load_cm(ph, CM_ONES, CM_ONES + 128, "ones")
        rstd, ss = norm_to_act(ph, x2T, G_FFN, consts, ones, two_pass=True, rstd_src=rS)
        wb = [(ph.sb("wb", [128, 32, 256], BF16), f"wb{i}") for i in range(2)]
        qp = ph.sb("qp", [128, 16, T], BF16)
        ksb = ph.sb("ksb", [128, 16, 128], BF16)
        ph.dma("gpsimd", ksb[:], ksub.rearrange("a d n -> d a n"), writes=["ksb"])

        def unit(u, chunk, hf, ps, pk):
            ph.act(qp[:, chunk, hf * 512:(hf + 1) * 512], ps[:], AF.Copy, [pk], [f"qp{chunk}_{hf}"])

        sc = [ph.ps("sc") for _ in range(2)]
        Sall = [ph.sb("Sall", [128, 16, 128], F32) for _ in range(2)]
        Sw = ph.sb("Sw", [128, 16, 128], F32)
        t16 = ph.sb("t16", [128, 16, 16], F32)
        cand = ph.sb("cand", [128, 8 * 16, 16], F32)
        candw = ph.sb("candw", [128, 8 * 16, 16], F32)
        b16 = [ph.sb("b16", [128, 8, 16], F32) for _ in range(2)]
        ex = ph.sb("ex", [128, 8, 16], F32)
        negm = ph.sb("negm", [128, 8], F32)
        Z = ph.sb("Z", [128, 8], F32)
        lnZ = ph.sb("lnZ", [128, 8], F32)
        off = ph.sb("off", [128, 8], F32)
        cc_ = ph.sb("cc", [128, 8], F32)
        ec = [ph.sb("ec", [128, 8], F32) for _ in range(2)]
        bt = [ph.sb("bt", [128, 8, 128], F32) for _ in range(2)]
        bSv = bS.rearrange("p (t h n) -> p t h n", t=8, h=8)
        s2Sv = s2S.rearrange("p (t h n) -> p t h n", t=8, h=8)
        ecSv = ecS.rearrange("p (t h) -> p t h", t=8)
        def partA(tt):
            sa, sak = Sall[tt % 2], f"Sall{tt % 2}"
            for q4 in range(4):
                for i in range(4):
                    hc = q4 * 4 + i
                    ph.mm(sc[q4 % 2][:, i * 128:(i + 1) * 128], qp[:, hc, tt * 128:(tt + 1) * 128], ksb[:, hc, :], True, True,
                          [f"qp{hc}_{tt // 4}", "ksb"], [f"sc{q4 % 2}"])
                ph.act(sa[:, q4 * 4:(q4 + 1) * 4, :], sc[q4 % 2][:], AF.Copy, [f"sc{q4 % 2}"], [f"{sak}_{q4}"])
            for hc in range(16):
                ph.op("vector", lambda e, hc=hc, sa=sa: e.max(out=t16[:, hc, 0:8], in_=sa[:, hc, :]), [f"{sak}_{hc // 4}"], ["t16A"])
            for hc in range(16):
                ph.op("vector", lambda e, hc=hc, sa=sa: e.match_replace(out=Sw[:, hc, :], in_to_replace=t16[:, hc, 0:8],
                                                                        in_values=sa[:, hc, :], imm_value=-1e30),
                      [f"{sak}_{hc // 4}", "t16A"], ["SwA"])
            for hc in range(16):
                ph.op("vector", lambda e, hc=hc: e.max(out=t16[:, hc, 8:16], in_=Sw[:, hc, :]), ["SwA"], ["t16B"])
            b_, bk = b16[tt % 2], f"b16{tt % 2}"
            for h in range(8):
                in0 = bass.AP(t16, (2 * h) * 16, [[256, 128], [1, 16], [0, 16]])
                in1 = bass.AP(t16, (2 * h + 1) * 16, [[256, 128], [0, 16], [1, 16]])
                ph.tt(cand[:, h * 16:(h + 1) * 16, :], in0, in1, ALU.add, ["t16A", "t16B"], ["candA"])
            for h in range(8):
                ph.op("vector", lambda e, h=h, b_=b_: e.max(out=b_[:, h, 0:8], in_=cand[:, h * 16:(h + 1) * 16, :]), ["candA"], [bk])
            for h in range(8):
                ph.op("vector", lambda e, h=h, b_=b_: e.match_replace(out=candw[:, h * 16:(h + 1) * 16, :], in_to_replace=b_[:, h, 0:8],
                                                                      in_values=cand[:, h * 16:(h + 1) * 16, :], imm_value=-1e30),
                      ["candA", bk], ["candwA"])
            for h in range(8):
                ph.op("vector", lambda e, h=h, b_=b_: e.max(out=b_[:, h, 8:16], in_=candw[:, h * 16:(h + 1) * 16, :]), ["candwA"], [bk])

        def partB(tt):
            sa, sak = Sall[tt % 2], f"Sall{tt % 2}"
            b_, bk = b16[tt % 2], f"b16{tt % 2}"
            ph.ts(negm[:], b_[:, :, 0], -1.0, None, ALU.mult, None, [bk], ["negm"])
            for h in range(8):
                ph.act(ex[:, h, :], b_[:, h, :], AF.Exp, [bk, "negm"], ["ex", "Z"], bias=negm[:, h:h + 1], accum_out=Z[:, h:h + 1])
            ph.act(lnZ[:], Z[:], AF.Ln, ["Z"], ["lnZ"])
            ph.tt(off[:], negm[:], lnZ[:], ALU.subtract, ["negm", "lnZ"], ["off"])
            ph.tt(cc_[:], b_[:, :, 15], off[:], ALU.add, [bk, "off"], ["cc"])
            e_, ek = ec[tt % 2], f"ec{tt % 2}"
            ph.act(e_[:], cc_[:], AF.Exp, ["cc"], [ek])
            ph.ts(e_[:], e_[:], 1.0 - 2e-5, None, ALU.mult, None, [ek], [ek])
            bt_, btk = bt[tt % 2], f"bt{tt % 2}"
            ph.ts(lnZ[:], b_[:, :, 15], -1.0, None, ALU.mult, None, [bk, "lnZ", "off"], ["lnZ"])
            for h in range(8):
                ph.ts(bt_[:, h, :], sa[:, 2 * h, :], lnZ[:, h:h + 1], None, ALU.add, None, [f"{sak}_{h // 2}", "lnZ"], [btk])
            ph.dma("sync", bSv[:, tt, :, :], bt_[:], reads=[btk])
            sav = sa[:].rearrange("p (h c) n -> p h c n", c=2)
            ph.dma("sync", s2Sv[:, tt, :, :], sav[:, :, 1, :], reads=[f"{sak}_{q}" for q in range(4)])
            ph.dma("sync", ecSv[:, tt, :], e_[:], reads=[ek])
        wqv = chunked(wq)
        tiles = [[(lambda b: b[:], wqv[:, :, i * 256:(i + 1) * 256], "gpsimd")] for hf in range(2) for i in range(8)]
        pq = [ph.ps("pq") for _ in range(4)]
        ucnt = [0]

        def compute(idx, buf, bkey):
            hf, i = idx // 8, idx % 8
            if hf == 1 and i % 2 == 0 and i > 0:
                partB(i // 2 - 1)
            for cc in range(2):
                u = ucnt[0]
                ucnt[0] += 1
                ps, pk = pq[u % 4], f"pq{u % 4}"
                for kc in range(32):
                    ph.mm(ps[:], buf[:, kc, cc * 128:(cc + 1) * 128], act[:, kc, hf * 512:(hf + 1) * 512],
                          kc == 0, kc == 31, [bkey, f"act{kc}"], [pk])
                unit(u, i * 2 + cc, hf, ps, pk)
            if hf == 1 and i % 2 == 1:
                partA(i // 2)

        wstream(ph, tiles, wb, compute)
        partB(3)
        for tt in range(4, 8):
            partA(tt)
            partB(tt)
        ph.emit()

    def peer_gate_phase():
        ph = new_phase()
        ident = load_cm(ph, CM_ID, CM_ID + 128, "ident")
        beta = ph.sb("beta", [128, 64, 128], F32)
        s2 = ph.sb("s2", [128, 64, 128], F32)
        ecb = ph.sb("ecb", [128, 64], F32)
        ph.dma("sync", ecb[:], ecS, writes=["ecb"])
        betav = bS.rearrange("p (a n) -> p a n", n=128)
        s2v = s2S.rearrange("p (a n) -> p a n", n=128)
        for tt in range(8):
            ph.dma("sync", s2[:, tt * 8:tt * 8 + 8, :], s2v[:, tt * 8:tt * 8 + 8, :], writes=[f"s2_{tt}"])
            ph.dma("sync", beta[:, tt * 8:tt * 8 + 8, :], betav[:, tt * 8:tt * 8 + 8, :], writes=[f"beta_{tt}"])
        NACT = 5

        def pre_exp(tt):
            a0 = tt * 8 + NACT
            ph.act(s2[:, a0:tt * 8 + 8, :], s2[:, a0:tt * 8 + 8, :], AF.Exp, [f"s2_{tt}"], [f"s2_{tt}"])
            ph.act(beta[:, a0:tt * 8 + 8, :], beta[:, a0:tt * 8 + 8, :], AF.Exp, [f"beta_{tt}"], [f"beta_{tt}"])
        D = 2
        NE, NG = 3, D + 2
        Eb = [ph.sb("Eb", [128, 8, 128], F32) for _ in range(NE)]
        Gb = [ph.sb("Gb", [128, 8, 128], BF16) for _ in range(NG)]
        diag = ph.sb("diag", [128, 64, 128], BF16)
        for a in range(64):
            ph.ts(diag[:, a, :], ident[:], ecb[:, a:a + 1], None, ALU.mult, None, ["ident", "ecb"], [f"diag_{a // 8}"])
        gl = [ph.sb("gl", [128, 512], F32) for _ in range(2)]
        pt = [ph.sb("pt", [128, T], BF16) for _ in range(2)]
        aT = [[ph.ps("aT") for _ in range(2)] for _ in range(2)]
        gT = [[ph.ps("gT") for _ in range(2)] for _ in range(2)]
        wb = [(ph.sb("wbu", [128, 32, 256], BF16), f"wbu{i}") for i in range(2)]
        uv = chunked(uT)
        gcount = [0]
        NU = 128 * 8
        fifo = []

        def gate_ops(n1, tt):
            g = gcount[0]
            gcount[0] += 1
            E, Ek = Eb[g % NE], f"Eb{g % NE}"
            Gm, Gk = Gb[g % NG], f"Gb{g % NG}"
            for h in range(8):
                a = tt * 8 + h
                if h < NACT:
                    ph.act(E[:, h, :], s2[:, a, :], AF.Exp, [f"s2_{tt}", f"beta_{tt}"], [Ek], bias=beta[:, a, n1:n1 + 1])
                else:
                    ph.ts(E[:, h, :], s2[:, a, :], beta[:, a, n1:n1 + 1], None, ALU.mult, None, [f"s2_{tt}", f"beta_{tt}"], [Ek])
            ph.stt(Gm[:], E[:], 1.0 - 2e-5, E[:], ALU.is_ge, ALU.mult, [Ek], [Gk])
            return (Gm, Gk)

        def gate_mms(n1, tt, res):
            par = n1 % 2
            Gm, Gk = res
            for h in range(8):
                ph.mm(gT[par][tt // 4][:, (tt % 4) * 128:(tt % 4 + 1) * 128], Gm[:, h, :], diag[:, tt * 8 + h, :], h == 0, h == 7,
                      [Gk, f"diag_{tt}"], [f"gT{par}{tt // 4}"])

        def issue_gate(g):
            if g < NU:
                fifo.append((g, gate_ops(g // 8, g % 8)))

        for tt in range(8):
            pre_exp(tt)
            gate_mms(0, tt, gate_ops(0, tt))
        for g in range(8, 8 + D):
            issue_gate(g)

        PTv = PT.rearrange("(c p) n -> c p n", p=128)

        def compute(i, buf, bkey):
            for cc in range(2):
                n1 = 2 * i + cc
                par = n1 % 2
                p_, pk = pt[par], f"pt{par}"
                for tt in range(8):
                    st = n1 * 8 + tt
                    issue_gate(st + 8 + D)
                    hf = tt // 4
                    for kc in range((tt % 4) * 8, (tt % 4) * 8 + 8):
                        ph.mm(aT[par][hf][:], buf[:, kc, cc * 128:(cc + 1) * 128], act[:, kc, hf * 512:(hf + 1) * 512],
                              kc == 0, kc == 31, [bkey, f"act{kc}"], [f"aT{par}{hf}"])
                    if st + 8 < NU:
                        g, res = fifo.pop(0)
                        assert g == st + 8
                        gate_mms(g // 8, g % 8, res)
                    if tt % 4 == 3:
                        ph.act(gl[hf][:], aT[par][hf][:], AF.Gelu, [f"aT{par}{hf}"], [f"gl{hf}"])
                        ph.tt(p_[:, hf * 512:(hf + 1) * 512], gT[par][hf][:], gl[hf][:], ALU.mult, [f"gT{par}{hf}", f"gl{hf}"], [pk])
                ph.dma("sync", PTv[n1], p_[:], reads=[pk])

        tiles = [[(lambda b: b[:], uv[:, :, i * 256:(i + 1) * 256], "gpsimd")] for i in range(64)]
        wstream(ph, tiles, wb, compute)
        ph.emit()

    def peer_out_phase():
        ph = new_phase()
        NC3 = 11
        vview = vv.rearrange("(g c p) n -> g p c n", c=4, p=128)
        pview = PT.rearrange("(g c p) n -> g p c n", c=4, p=128)
        x2v, x3v = chunked(x2T), chunked(x3T)
        acc = [ph.ps("acc") for _ in range(8)]
        bufs = [((ph.sb("Vt", [128, 4, 512], BF16), ph.sb("Pt", [128, 4, 512], BF16)), f"vp{i}") for i in range(3)]
        xs = [ph.sb("xs", [128, 512], F32) for _ in range(3)]
        actv = act[:].rearrange("p c (a t) -> p (c a) t", a=2)
        pc2 = ph.sb("pc2", [128, 64, 512], BF16)
        pc3 = ph.sb("pc3", [128, NC3 * 4, 512], BF16)

        def c0(eg):
            return actv[:, 4 * eg:4 * eg + 4, :] if eg < 16 else pc2[:, 4 * (eg - 16):4 * (eg - 16) + 4, :]

        def c1(eg):
            return pc3[:, 4 * eg:4 * eg + 4, :]

        tiles = []
        for db in range(8):
            for eg in range(32):
                ent = [(lambda b: b[0][:], vview[eg][:, :, db * 512:(db + 1) * 512], "gpsimd")]
                if db == 0:
                    ent.append((lambda b, eg=eg: c0(eg), pview[eg][:, :, 0:512], "sync", f"pcA{eg}"))
                    if eg < NC3:
                        ent.append((lambda b, eg=eg: c1(eg), pview[eg][:, :, 512:1024], "sync", f"pcC{eg}"))
                if eg >= NC3:
                    ent.append((lambda b: b[1][:], pview[eg][:, :, 512:1024], "sync"))
                tiles.append(ent)
        cnt = [0]

        def compute(i, buf, bkey):
            db, eg = i // 32, i % 32
            Vt, Pt = buf
            r0, k0 = c0(eg), f"pcA{eg}"
            if eg < NC3:
                r1, k1 = c1(eg), f"pcC{eg}"
            else:
                r1, k1 = Pt, bkey
            for ec in range(4):
                for dc in range(4):
                    for hf in range(2):
                        r, rk = (r0, k0) if hf == 0 else (r1, k1)
                        ph.mm(acc[dc * 2 + hf][:], Vt[:, ec, dc * 128:(dc + 1) * 128], r[:, ec, :],
                              eg == 0 and ec == 0, eg == 31 and ec == 3, [bkey, rk], [f"acc{dc * 2 + hf}"])
            if eg == 31:
                for dc in range(4):
                    for hf in range(2):
                        u = cnt[0]
                        cnt[0] += 1
                        x_, xk = xs[u % 3], f"xs{u % 3}"
                        sl = slice(hf * 512, (hf + 1) * 512)
                        ph.dma("sync", x_[:], x2v[:, db * 4 + dc, sl], writes=[xk])
                        ph.tt(x_[:], acc[dc * 2 + hf][:], x_[:], ALU.add, [f"acc{dc * 2 + hf}", xk], [xk])
                        ph.dma("sync", x3v[:, db * 4 + dc, sl], x_[:], reads=[xk])

        wstream(ph, tiles, bufs, compute)
        ph.emit()

    def ple_phase():
        ph = new_phase()
        consts = load_consts(ph)
        ones = load_cm(ph, CM_ONES, CM_ONES + 128, "ones")
        rstd, ss = norm_to_act(ph, x3T, G_PLE, consts, ones)
        wb = [(ph.sb("wb", [128, 32, 256], BF16), f"wb{i}") for i in range(2)]
        wpb = ph.sb("wpb", [128, 2, 4096], BF16)
        ptb = ph.sb("ptb", [128, 2, T], BF16)
        ph.dma("gpsimd", wpb[:], chunked(wp), writes=["wpb"])
        ph.dma("gpsimd", ptb[:], chunked(pT), writes=["ptb"])
        sg = [ph.sb("sgp", [128, 512], F32) for _ in range(3)]
        xs = [ph.sb("xs", [128, 512], F32) for _ in range(3)]
        pp = [ph.ps("pp") for _ in range(2)]
        x3v, ov = chunked(x3T), chunked(outT)

        def unit(u, chunk, hf, ps, pk):
            s, sk = sg[u % 3], f"sgp{u % 3}"
            ph.tt(s[:], ps[:], rstd[:, hf * 512:(hf + 1) * 512], ALU.mult, [pk, f"rstd{hf}"], [sk])
            ph.act(s[:], s[:], AF.Sigmoid, [sk], [sk])
            x_, xk = xs[u % 3], f"xs{u % 3}"
            sl = slice(hf * 512, (hf + 1) * 512)
            ph.dma("sync", x_[:], x3v[:, chunk, sl], writes=[xk])

            def cont():
                p_, ppk = pp[u % 2], f"pp{u % 2}"
                for kc in range(2):
                    ph.mm(p_[:], wpb[:, kc, chunk * 128:(chunk + 1) * 128], ptb[:, kc, sl], kc == 0, kc == 1, ["wpb", "ptb"], [ppk])
                ph.tt(s[:], p_[:], s[:], ALU.mult, [ppk, sk], [sk])
                ph.tt(x_[:], x_[:], s[:], ALU.add, [xk, sk], [xk])
                ph.dma("sync", ov[:, chunk, sl], x_[:], reads=[xk])
            ph.step([cont])

        gemm_phase_units(ph, chunked(wg), 16, wb, unit)
        ph.emit()

    phases = [lambda: proj_phase(True), lambda: proj_phase(False), attn_phase, conv_phase, outproj_phase,
              peer_score_phase, peer_gate_phase, peer_out_phase, ple_phase]
    for i, f in enumerate(phases[:nph]):
        f()
        if i == 1:
            hes.close()
    if nph < 2:
        hes.close()
    kes.close()
    return nc


def make_inputs(x, p, mix_norm_g, w_in, q_norm_g, k_norm_g, lambda_q, lambda_k, subln_g, glu_b, dw_kernel, dw_b,
                conv_ln_g, conv_ln_b, w_out, ffn_norm_g, peer_w_query, peer_sub_keys, peer_u, peer_v, ple_norm_g,
                ple_gate_w, ple_proj_w):
    f = lambda a: np.ascontiguousarray(np.asarray(a, dtype=np.float32))
    x = f(x)
    p = f(p)

    def pc(v):
        v = f(v).reshape(-1, 128)
        return v.T

    consts = np.zeros((128, NCONST), np.float32)
    consts[:, G_MIX:G_MIX + 32] = pc(mix_norm_g[0])
    consts[:, G_FFN:G_FFN + 32] = pc(ffn_norm_g[0])
    consts[:, G_PLE:G_PLE + 32] = pc(ple_norm_g[0])
    consts[:, QKG:QKG + 2] = f(q_norm_g[0]).T
    consts[:, QKG + 2:QKG + 4] = f(k_norm_g[0]).T
    consts[:, LAM:LAM + 2] = f(lambda_q[0]).T
    consts[:, LAM + 2:LAM + 4] = f(lambda_k[0]).T
    consts[:, SUBG:SUBG + 2] = pc(subln_g[0])
    consts[:, GLUB:GLUB + 32] = pc(glu_b[0])
    consts[:, DWB:DWB + 16] = pc(dw_b[0])
    consts[:, LNG:LNG + 16] = pc(conv_ln_g[0])
    consts[:, LNB:LNB + 16] = pc(conv_ln_b[0])
    dk = f(dw_kernel[0])
    consts[:, DWW:] = dk.reshape(31, 16, 128).transpose(2, 1, 0).reshape(128, 16 * 31)
    cm = np.zeros((128, NCM), np.float32)
    cm[:, CM_ID:CM_ID + 128] = np.eye(128, dtype=np.float32)
    cm[:, CM_ONES:CM_ONES + 128] = 1.0
    kk = np.arange(128)[:, None]
    qq = np.arange(512)[None, :]
    for o in range(4):
        cm[:, CM_MASK + o * 512:CM_MASK + (o + 1) * 512] = (qq >= 128 * o + kk).astype(np.float32)
    shared = {
        "cm": cm,
        "w_in": f(w_in[0]), "w_out": f(w_out[0]), "wq": f(peer_w_query[0]),
        "ksub": np.ascontiguousarray(f(peer_sub_keys[0]).reshape(16, 128, 128).transpose(0, 2, 1)),
        "uT": np.ascontiguousarray(f(peer_u[0]).T), "v": f(peer_v[0]),
        "wg": f(ple_gate_w[0]), "wp": f(ple_proj_w[0]),
    }
    in_maps = []
    for c in range(8):
        b, half = c // 2, c % 2
        cc = consts.copy()
        cc[:, FLAGS] = 1.0 if half == 1 else 0.0
        cc[:, FLAGS + 1] = 0.0 if half == 1 else -30000.0
        d = dict(shared)
        d["consts"] = cc
        d["xo"] = np.ascontiguousarray(x[b, half * T:(half + 1) * T].T)
        d["xc"] = np.ascontiguousarray(x[b, 0:T].T)
        d["pT"] = np.ascontiguousarray(p[0, b, half * T:(half + 1) * T].T)
        in_maps.append(d)
    return in_maps


def kernel(**inputs):
    in_maps = make_inputs(**inputs)
    nc = build_nc()
    res = run_bass_kernel_spmd(nc, in_maps, core_ids=list(range(8)))
    out = np.zeros((4, 2048, 4096), np.float32)
    for c in range(8):
        b, half = c // 2, c % 2
        out[b, half * T:(half + 1) * T] = res.results[c]["outT"].T
    return out
```

```python
import os
import numpy as np
import concourse.bass as bass
import concourse.mybir as mybir
from concourse.bass_utils import run_bass_kernel_spmd
from contextlib import ExitStack

F32 = mybir.dt.float32
BF16 = mybir.dt.bfloat16
AF = mybir.ActivationFunctionType
ALU = mybir.AluOpType

EPS = 1e-6
T = 1024
ENGS = ["sync", "scalar", "vector", "tensor", "gpsimd"]
NRING = 8

G_MIX, G_FFN, G_PLE, QKG, LAM, SUBG, GLUB, DWB, LNG, LNB, FLAGS, DWW = 0, 32, 64, 96, 100, 104, 106, 138, 154, 170, 186, 188
NCONST = DWW + 16 * 31
CM_ID, CM_ONES, CM_MASK = 0, 128, 256
NCM = 256 + 4 * 512


class _Op:
    __slots__ = ("eng", "fn", "deps", "dma", "sem", "val", "need", "guard")

    def __init__(self, eng, fn, dma):
        self.eng = eng
        self.fn = fn
        self.dma = dma
        self.deps = []
        self.sem = None
        self.val = 0
        self.need = dma
        self.guard = None


class Phase:
    def __init__(self, nc, pid, G):
        self.nc = nc
        self.pid = pid
        self.G = G
        self.es = ExitStack()
        self.ops = {e: [] for e in ENGS}
        self.state = {}
        self.n = 0
        self.pend = []

    def sb(self, name, shape, dtype):
        self.n += 1
        return self.es.enter_context(self.nc.sbuf_tensor(f"{name}_{self.pid}_{self.n}", shape, dtype))

    def ps(self, name, shape=(128, 512), dtype=F32):
        self.n += 1
        return self.es.enter_context(self.nc.psum_tensor(f"{name}_{self.pid}_{self.n}", list(shape), dtype))

    def _rec(self, op, reads, writes):
        for k in reads:
            st = self.state.setdefault(k, [None, []])
            if st[0] is not None:
                op.deps.append((st[0], True))
            st[1].append(op)
        for k in writes:
            st = self.state.setdefault(k, [None, []])
            if st[0] is not None and st[0] is not op:
                op.deps.append((st[0], False))
            for r in st[1]:
                if r is not op:
                    op.deps.append((r, False))
            st[0] = op
            st[1] = []
        self.ops[op.eng].append(op)
        return op

    def op(self, eng, fn, reads=(), writes=()):
        return self._rec(_Op(eng, fn, False), reads, writes)

    def dma(self, eng, out, in_, reads=(), writes=()):
        return self._rec(_Op(eng, lambda e: e.dma_start(out=out, in_=in_), True), reads, writes)

    def mm(self, out, lhsT, rhs, start, stop, reads, writes):
        return self.op("tensor", lambda e: e.matmul(out, lhsT, rhs, start=start, stop=stop), reads, writes)

    def act(self, out, in_, func, reads, writes, **kw):
        return self.op("scalar", lambda e: e.activation(out=out, in_=in_, func=func, **kw), reads, writes)

    def ts(self, out, in0, s1, s2, op0, op1, reads, writes, eng="vector"):
        if op1 is None:
            return self.op(eng, lambda e: e.tensor_scalar(out=out, in0=in0, scalar1=s1, scalar2=None, op0=op0), reads, writes)
        return self.op(eng, lambda e: e.tensor_scalar(out=out, in0=in0, scalar1=s1, scalar2=s2, op0=op0, op1=op1), reads, writes)

    def stt(self, out, in0, scalar, in1, op0, op1, reads, writes):
        return self.op("vector", lambda e: e.scalar_tensor_tensor(out=out, in0=in0, scalar=scalar, in1=in1, op0=op0, op1=op1), reads, writes)

    def tt(self, out, in0, in1, op, reads, writes, eng="vector"):
        return self.op(eng, lambda e: e.tensor_tensor(out=out, in0=in0, in1=in1, op=op), reads, writes)

    def step(self, deferred):
        old = self.pend
        self.pend = list(deferred)
        for f in old:
            f()

    def flush(self):
        self.step([])

    def emit(self):
        self.flush()
        nc = self.nc
        es = self.es
        G = self.G
        if "esem" not in G:
            kes = G["kes"]
            G["esem"] = {e: kes.enter_context(nc.semaphore(f"e{e}")) for e in ENGS}
            G["rings"] = {e: [kes.enter_context(nc.semaphore(f"d{e}{i}")) for i in range(NRING)]
                          for e in ("sync", "scalar", "gpsimd")}
            G["cnt"] = {e: 0 for e in ENGS}
            G["rcnt"] = {e: [0] * NRING for e in ENGS}
            G["ri"] = {e: 0 for e in ENGS}
        esem, rings = G["esem"], G["rings"]

        def needs_wait(op, dep, raw):
            if dep.dma or op.dma:
                return True
            if dep.eng != op.eng:
                return True
            if op.eng == "tensor":
                return False
            return raw

        for e in ENGS:
            for op in self.ops[e]:
                for dep, raw in op.deps:
                    if needs_wait(op, dep, raw):
                        dep.need = True
        final = {}
        for e in ENGS:
            cnt = G["cnt"][e]
            rcnt = G["rcnt"][e]
            ri = G["ri"][e]
            for op in self.ops[e]:
                if op.dma:
                    op.sem = rings[e][ri]
                    op.guard = rcnt[ri]
                    rcnt[ri] += 16
                    op.val = rcnt[ri]
                    ri = (ri + 1) % NRING
                elif op.need:
                    cnt += 1
                    op.sem = esem[e]
                    op.val = cnt
            final[e] = list(zip(rings.get(e, []), list(rcnt)))
            G["cnt"][e] = cnt
            G["ri"][e] = ri
        ops = self.ops

        def body(ename):
            def f(eng):
                waited = {}
                for op in ops[ename]:
                    ws = {}
                    for dep, raw in op.deps:
                        if needs_wait(op, dep, raw):
                            k = id(dep.sem)
                            if ws.get(k, (None, 0))[1] < dep.val:
                                ws[k] = (dep.sem, dep.val)
                    if op.dma and op.guard > 0:
                        k = id(op.sem)
                        if ws.get(k, (None, 0))[1] < op.guard:
                            ws[k] = (op.sem, op.guard)
                    for k, (sem, val) in ws.items():
                        if waited.get(k, 0) < val:
                            eng.wait_ge(sem, val)
                            waited[k] = val
                    ins = op.fn(eng)
                    if op.dma:
                        ins.then_inc(op.sem, 16)
                    elif op.need:
                        ins.then_inc(op.sem, 1)
                for sem, val in final[ename]:
                    if val > 0 and waited.get(id(sem), 0) < val:
                        eng.wait_ge(sem, val)
            return f

        with nc.Block() as block:
            for e in ENGS:
                getattr(block, e)(body(e))
        es.close()


def wstream(ph, tiles, bufs, compute):
    nb = len(bufs)

    def issue(i):
        buf, key = bufs[i % nb]
        for ent in tiles[i]:
            dst_fn, src, eng = ent[:3]
            ph.dma(eng, dst_fn(buf), src, writes=[ent[3] if len(ent) > 3 else key])

    for i in range(min(nb - 1, len(tiles))):
        issue(i)
    for i in range(len(tiles)):
        if i + nb - 1 < len(tiles):
            issue(i + nb - 1)
        compute(i, *bufs[i % nb])


def build_nc(nph=99, debug=False):
    nc = bass.Bass("TRN2", target_bir_lowering=False)

    def din(name, shape):
        return nc.dram_tensor(name, list(shape), F32, kind="ExternalInput").ap()

    def scratch(name, shape, dt):
        return nc.dram_tensor(name, list(shape), dt, kind="ExternalOutput" if debug else "Internal").ap()

    xo = din("xo", (4096, T))
    xc = din("xc", (4096, T))
    pT = din("pT", (256, T))
    consts_d = din("consts", (128, NCONST))
    cm_d = din("cm", (128, NCM))
    w_in = din("w_in", (4096, 10240))
    w_out = din("w_out", (4096, 4096))
    wq = din("wq", (4096, 2048))
    ksub = din("ksub", (16, 128, 128))
    uT = din("uT", (4096, 16384))
    vv = din("v", (16384, 4096))
    wg = din("wg", (4096, 4096))
    wp = din("wp", (256, 4096))
    outT = nc.dram_tensor("outT", [4096, T], F32, kind="ExternalOutput").ap()

    qT_s = scratch("qT_s", (2048, T), BF16)
    kT_s = scratch("kT_s", (2048, 2 * T), BF16)
    v_s = scratch("v_s", (2 * T, 2048), BF16)
    y_s = scratch("y_s", (2048, 128 + T), F32)
    x2T = scratch("x2T", (4096, T), F32)
    x3T = scratch("x3T", (4096, T), F32)
    bS = scratch("bS", (128, 8 * 8 * 128), F32)
    s2S = scratch("s2S", (128, 8 * 8 * 128), F32)
    ecS = scratch("ecS", (128, 64), F32)
    rS = scratch("rS", (128, T), F32)
    PT = scratch("PT", (16384, T), BF16)
    mix_dbg = scratch("mix_dbg", (4096, T), BF16) if debug else None

    kes = ExitStack()
    act = kes.enter_context(nc.sbuf_tensor("act", [128, 32, T], BF16))
    hes = ExitStack()
    halo = hes.enter_context(nc.sbuf_tensor("halo", [128, 32, 128], BF16))
    halo_r = hes.enter_context(nc.sbuf_tensor("halo_r", [128, 128], F32))
    pid = [0]
    G = {"kes": kes}

    def new_phase():
        pid[0] += 1
        return Phase(nc, pid[0], G)

    def chunked(ap):
        return ap.rearrange("(c p) n -> p c n", p=128)

    def load_consts(ph):
        c = ph.sb("consts", [128, NCONST], F32)
        ph.dma("sync", c[:], consts_d, writes=["consts"])
        return c

    def load_cm(ph, lo, hi, name):
        t = ph.sb(name, [128, hi - lo], BF16)
        ph.dma("gpsimd", t[:], cm_d[:, lo:hi], writes=[name])
        return t

    def norm_to_act(ph, src, gcol, consts, ones, two_pass=False, rstd_src=None):
        xv = chunked(src)
        xb = [ph.sb("xb", [128, T], F32) for _ in range(3)]
        if rstd_src is not None:
            rstd = ph.sb("rstd", [128, T], F32)
            ph.dma("sync", rstd[:], rstd_src, writes=["rstd0", "rstd1"])
            for c in range(32):
                x_, xk = xb[c % 3], f"xb{c % 3}"
                ph.dma("sync", x_[:], xv[:, c, :], writes=[xk])
                ph.stt(act[:, c, :], x_[:], consts[:, gcol + c:gcol + c + 1], rstd[:], ALU.mult, ALU.mult,
                       [xk, "rstd0", "rstd1", "consts"], [f"act{c}"])
            return rstd, None
        ss = [ph.ps("ss") for _ in range(2)]
        sq = [ph.sb("sq", [128, T], BF16) for _ in range(2)]
        for c in range(32):
            x_, xk = xb[c % 3], f"xb{c % 3}"
            s_, sk = sq[c % 2], f"sq{c % 2}"
            ph.dma("sync", x_[:], xv[:, c, :], writes=[xk])
            ph.tt(s_[:], x_[:], x_[:], ALU.mult, [xk], [sk], eng="gpsimd")
            if not two_pass:
                ph.ts(act[:, c, :], x_[:], consts[:, gcol + c:gcol + c + 1], None, ALU.mult, None, [xk, "consts"], [f"act{c}"])
            for hf in range(2):
                ph.mm(ss[hf][:], ones[:], s_[:, hf * 512:(hf + 1) * 512], c == 0, c == 31, [sk, "ones"], [f"ss{hf}"])
        rstd = ph.sb("rstd", [128, T], F32)
        tmp = ph.sb("tmpn", [128, T], F32)
        for hf in range(2):
            sl = slice(hf * 512, (hf + 1) * 512)
            ph.act(tmp[:, sl], ss[hf][:], AF.Ln, [f"ss{hf}"], [f"tmpn{hf}"], scale=1.0 / 4096, bias=EPS)
            ph.act(rstd[:, sl], tmp[:, sl], AF.Exp, [f"tmpn{hf}"], [f"rstd{hf}"], scale=-0.5)
        if two_pass:
            for c in range(32):
                x_, xk = xb[c % 3], f"xb{c % 3}"
                ph.dma("sync", x_[:], xv[:, c, :], writes=[xk])
                ph.stt(act[:, c, :], x_[:], consts[:, gcol + c:gcol + c + 1], rstd[:], ALU.mult, ALU.mult,
                       [xk, "rstd0", "rstd1", "consts"], [f"act{c}"])
        return rstd, ss

    def proj_phase(is_ctx):
        ph = new_phase()
        consts = load_consts(ph)
        ones = load_cm(ph, CM_ONES, CM_ONES + 128, "ones")
        rstd, ss = norm_to_act(ph, xc if is_ctx else xo, G_MIX, consts, ones)
        identf = ph.sb("identf", [128, 2], F32)
        ph.dma("sync", identf[:], cm_d[:, CM_ID:CM_ID + 2], writes=["identf"])
        for tt in range(8):
            ph.mm(ss[0][:, 2 * tt:2 * tt + 2], rstd[:, tt * 128:(tt + 1) * 128], identf[:], True, True,
                  ["rstd0", "rstd1", "identf"], ["ss0"])
        rcol = ph.sb("rcol", [128, 16], F32)
        ph.act(rcol[:], ss[0][:, 0:16], AF.Copy, ["ss0"], ["rcol"])
        if is_ctx:
            ph.op("gpsimd", lambda e: e.tensor_copy(out=halo[:], in_=act[:, :, 896:1024]), [f"act{c}" for c in range(32)], ["halo"])
            ph.op("gpsimd", lambda e: e.tensor_copy(out=halo_r[:], in_=rstd[:, 896:1024]), ["rstd1"], ["halo_r"])
        qkg = ph.sb("qkg", [128, 4], F32)
        ph.ts(qkg[:, 0:2], consts[:, QKG:QKG + 2], 128.0 ** -0.5, None, ALU.mult, None, ["consts"], ["qkg"])
        ph.ts(qkg[:, 2:4], consts[:, QKG + 2:QKG + 4], 1.0, None, ALU.mult, None, ["consts", "qkg"], ["qkg"])
        wv = chunked(w_in)
        tokbase = 0 if is_ctx else T
        pq = [ph.ps("pq") for _ in range(4)]
        ssp = [ph.ps("ssp") for _ in range(2)]
        zc = [ph.sb("zc", [128, 512], F32) for _ in range(3)]
        sqb = [ph.sb("sqb", [128, 512], BF16) for _ in range(3)]
        lnb_ = [ph.sb("lnt", [128, 512], F32) for _ in range(2)]
        rs_ = [ph.sb("rs", [128, 512], F32) for _ in range(2)]
        ob = [ph.sb("ob", [128, 512], BF16) for _ in range(3)]
        vb = [ph.sb("vb", [128, 256], BF16) for _ in range(3)]
        sg = [ph.sb("sg", [128, 512], F32) for _ in range(2)]
        yb = [ph.sb("yb", [128, 512], F32) for _ in range(3)]
        wb = [(ph.sb("wb", [128, 32, 256], BF16), f"wb{i}") for i in range(2)]
        cnt = {"u": 0, "v": 0, "y": 0}
        allact = [f"act{c}" for c in range(32)]

        tiles = []
        kinds = []
        if not is_ctx:
            for i in range(8):
                tiles.append([(lambda b: b[:], wv[:, :, i * 256:(i + 1) * 256], "gpsimd")])
                kinds.append(("q", i))
        for i in range(8):
            tiles.append([(lambda b: b[:], wv[:, :, 2048 + i * 256:2048 + (i + 1) * 256], "gpsimd")])
            kinds.append(("k", i))
        for i in range(8):
            tiles.append([(lambda b: b[:], wv[:, :, 4096 + i * 256:4096 + (i + 1) * 256], "gpsimd")])
            kinds.append(("v", i))
        for j in range(0 if is_ctx else 16):
            tiles.append([(lambda b: b[:, :, 0:128], wv[:, :, 6144 + j * 128:6144 + (j + 1) * 128], "gpsimd"),
                          (lambda b: b[:, :, 128:256], wv[:, :, 8192 + j * 128:8192 + (j + 1) * 128], "gpsimd")])
            kinds.append(("c", j))

        def qk_unit(kind, chunk, hf, buf, bkey, cc, skip_mm=False):
            u = cnt["u"]
            cnt["u"] += 1
            ps, pk = pq[u % 4], f"pq{u % 4}"
            for kc in range(0 if skip_mm else 32):
                ph.mm(ps[:], buf[:, kc, cc * 128:(cc + 1) * 128], act[:, kc, hf * 512:(hf + 1) * 512],
                      kc == 0, kc == 31, [bkey, f"act{kc}"], [pk])
            z, zk = zc[u % 3], f"zc{u % 3}"
            s, sk = sqb[u % 3], f"sqb{u % 3}"
            ph.tt(z[:], ps[:], rstd[:, hf * 512:(hf + 1) * 512], ALU.mult, [pk, f"rstd{hf}"], [zk])
            ph.tt(s[:], z[:], z[:], ALU.mult, [zk], [sk])

            def cont():
                sp, spk = ssp[u % 2], f"ssp{u % 2}"
                ph.mm(sp[:], ones[:], s[:], True, True, [sk, "ones"], [spk])
                l, lk = lnb_[u % 2], f"lnt{u % 2}"
                r, rk = rs_[u % 2], f"rs{u % 2}"
                ph.act(l[:], sp[:], AF.Ln, [spk], [lk], scale=1.0 / 128, bias=EPS)
                ph.act(r[:], l[:], AF.Exp, [lk], [rk], scale=-0.5)
                o, ok = ob[u % 3], f"ob{u % 3}"
                m = chunk % 2
                gc = m if kind == "q" else 2 + m
                ph.stt(o[:], z[:], qkg[:, gc:gc + 1], r[:], ALU.mult, ALU.mult, [zk, rk, "qkg"], [ok])
                if kind == "q":
                    dst = qT_s[chunk * 128:(chunk + 1) * 128, hf * 512:(hf + 1) * 512]
                else:
                    dst = kT_s[chunk * 128:(chunk + 1) * 128, tokbase + hf * 512:tokbase + (hf + 1) * 512]
                ph.dma("sync", dst, o[:], reads=[ok])
            ph.step([cont])

        def compute(i, buf, bkey):
            kind, idx = kinds[i]
            if kind in ("q", "k"):
                first = (i == 0)
                if first:
                    u0 = cnt["u"]
                    for kc in range(32):
                        for q in range(4):
                            cc, hf = q // 2, q % 2
                            ph.mm(pq[(u0 + q) % 4][:], buf[:, kc, cc * 128:(cc + 1) * 128], act[:, kc, hf * 512:(hf + 1) * 512],
                                  kc == 0, kc == 31, [bkey, f"act{kc}"], [f"pq{(u0 + q) % 4}"])
                for cc in range(2):
                    for hf in range(2):
                        qk_unit(kind, idx * 2 + cc, hf, buf, bkey, cc, skip_mm=first)
            elif kind == "v":
                for tt in range(8):
                    u = cnt["u"]
                    cnt["u"] += 1
                    ps, pk = pq[u % 4], f"pq{u % 4}"
                    for kc in range(32):
                        ph.mm(ps[:, 0:256], act[:, kc, tt * 128:(tt + 1) * 128], buf[:, kc, :],
                              kc == 0, kc == 31, [bkey, f"act{kc}"], [pk])
                    n = cnt["v"]
                    cnt["v"] += 1
                    o, ok = vb[n % 3], f"vb{n % 3}"
                    ph.act(o[:], ps[:, 0:256], AF.Identity, [pk, "rcol"], [ok], scale=rcol[:, 2 * tt:2 * tt + 1])
                    ph.dma("sync", v_s[tokbase + tt * 128:tokbase + (tt + 1) * 128, idx * 256:(idx + 1) * 256], o[:], reads=[ok])
                    ph.step([])
            else:
                j = idx
                units = [(0, 512, 128), (512, 512, 640), (-1, 128, 0)]
                for t0, n, d0 in units:
                    u = cnt["u"]
                    cnt["u"] += 2
                    pa, pak = pq[u % 4], f"pq{u % 4}"
                    pg, pgk = pq[(u + 1) % 4], f"pq{(u + 1) % 4}"
                    rhs = (lambda kc: halo[:, kc, :]) if t0 < 0 else (lambda kc: act[:, kc, t0:t0 + n])
                    for kc in range(32):
                        ph.mm(pa[:, 0:n], buf[:, kc, 0:128], rhs(kc), kc == 0, kc == 31, [bkey, f"act{kc}"], [pak])
                    for kc in range(32):
                        ph.mm(pg[:, 0:n], buf[:, kc, 128:256], rhs(kc), kc == 0, kc == 31, [bkey, f"act{kc}"], [pgk])
                    k = cnt["y"]
                    cnt["y"] += 1
                    s, sk = sg[k % 2], f"sg{k % 2}"
                    y, yk = yb[k % 3], f"yb{k % 3}"
                    rsl = halo_r[:, 0:128] if t0 < 0 else rstd[:, t0:t0 + n]
                    rkey = "halo_r" if t0 < 0 else f"rstd{t0 // 512}"
                    ph.tt(s[:, 0:n], pg[:, 0:n], rsl, ALU.mult, [pgk, rkey], [sk])
                    ph.act(s[:, 0:n], s[:, 0:n], AF.Sigmoid, [sk, "consts"], [sk], bias=consts[:, GLUB + 16 + j:GLUB + 17 + j])
                    ph.tt(y[:, 0:n], pa[:, 0:n], rsl, ALU.mult, [pak, rkey], [yk])
                    ph.stt(y[:, 0:n], y[:, 0:n], consts[:, GLUB + j:GLUB + j + 1], s[:, 0:n], ALU.add, ALU.mult,
                           [yk, sk, "consts"], [yk])
                    ph.dma("sync", y_s[j * 128:(j + 1) * 128, d0:d0 + n], y[:, 0:n], reads=[yk])
                    ph.step([])

        wstream(ph, tiles, wb, compute)
        ph.emit()

    def attn_phase():
        ph = new_phase()
        consts = load_consts(ph)
        ones = load_cm(ph, CM_ONES, CM_ONES + 128, "ones")
        cmask = load_cm(ph, CM_MASK, CM_MASK + 2048, "cmask")
        prod = ph.sb("prod", [128, 2], BF16)
        ph.tt(prod[:], consts[:, LAM:LAM + 2], consts[:, LAM + 2:LAM + 4], ALU.mult, ["consts"], ["prod"])
        lps = ph.ps("ssps")
        ph.mm(lps[:, 0:2], ones[:], prod[:], True, True, ["prod", "ones"], ["ssps"])
        el = ph.sb("el", [128, 2], F32)
        ph.act(el[:], lps[:, 0:2], AF.Exp, ["ssps"], ["el"])
        negl = ph.sb("negl", [128, 1], F32)
        ph.tt(negl[:], el[:, 1:2], el[:, 0:1], ALU.subtract, ["el"], ["negl"])
        ph.ts(negl[:], negl[:], -0.2, None, ALU.add, None, ["negl"], ["negl"])
        gs = ph.sb("gs", [128, 2], F32)
        ph.ts(gs[:], consts[:, SUBG:SUBG + 2], 0.8, None, ALU.mult, None, ["consts"], ["gs"])

        qv, kv = chunked(qT_s), chunked(kT_s)
        vview = v_s.rearrange("(t p) n -> p t n", p=128)
        qh = [ph.sb("qh", [128, 2, T], BF16) for _ in range(2)]
        kh = [ph.sb("kh", [128, 2, 2 * T], BF16) for _ in range(2)]
        vh = [ph.sb("vh", [128, 16, 256], BF16) for _ in range(2)]
        sT = [ph.ps("sT") for _ in range(3)]
        den = ph.ps("den")
        O = [ph.ps("O") for _ in range(2)]
        pT_ = [ph.sb("pT", [128, 512], BF16) for _ in range(5)]
        rden = ph.sb("rden", [128, 512], F32)
        R = [[ph.sb("R", [128, 512], F32) for _ in range(2)] for _ in range(2)]
        sq = [ph.sb("sqa", [128, 512], BF16) for _ in range(2)]
        lt = ph.sb("lt", [128, 512], F32)
        rs = ph.sb("rsa", [128, 512], F32)
        pcount = [0]

        def load_head(h):
            b = h % 2
            ph.dma("sync", qh[b][:], qv[:, 2 * h:2 * h + 2, :], writes=[f"qh{b}"])
            ph.dma("sync", kh[b][:], kv[:, 2 * h:2 * h + 2, :], writes=[f"kh{b}"])
            ph.dma("sync", vh[b][:], vview[:, :, h * 256:(h + 1) * 256], writes=[f"vh{b}"])

        load_head(0)
        items = []
        for h in range(8):
            for qb in range(2):
                nj = 12 if qb == 0 else 16
                for m in range(2):
                    for j in range(nj):
                        items.append((h, qb, m, j, nj))
        NS, NP, LA = 3, 5, 2

        def S(p):
            h, qb, m, j, nj = items[p]
            b = h % 2
            st, sk = sT[p % NS], f"sT{p % NS}"
            ph.mm(st[:], kh[b][:, m, j * 128:(j + 1) * 128], qh[b][:, m, qb * 512:(qb + 1) * 512],
                  True, True, [f"kh{b}", f"qh{b}"], [sk])
            pt, pk = pT_[p % NP], f"pT{p % NP}"
            if j < 8:
                ph.act(pt[:], st[:], AF.Exp, [sk, "consts"], [pk], bias=consts[:, FLAGS + 1:FLAGS + 2])
            else:
                ph.act(pt[:], st[:], AF.Exp, [sk], [pk])
            o = j - 8 - 4 * qb
            if o >= 0:
                ph.tt(pt[:], pt[:], cmask[:, o * 512:(o + 1) * 512], ALU.mult, [pk, "cmask"], [pk])

        def PV(p):
            h, qb, m, j, nj = items[p]
            b = h % 2
            pt, pk = pT_[p % NP], f"pT{p % NP}"
            ph.mm(den[:], ones[:], pt[:], j == 0, j == nj - 1, [pk, "ones"], ["den"])
            for a in range(2):
                ph.mm(O[a][:], vh[b][:, j, a * 128:(a + 1) * 128], pt[:], j == 0, j == nj - 1, [pk, f"vh{b}"], [f"O{a}"])

        for p in range(min(LA, len(items))):
            S(p)
        Oc = [ph.sb("Oc", [128, 512], F32) for _ in range(2)]
        later = []
        for p in range(len(items)):
            while later and later[0][0] <= p:
                later.pop(0)[1]()
            h, qb, m, j, nj = items[p]
            if p + LA < len(items):
                S(p + LA)
            if qb == 0 and m == 0 and j == 0 and h + 1 < 8:
                load_head(h + 1)
            PV(p)
            if j == nj - 1:
                for a in range(2):
                    ph.act(Oc[a][:], O[a][:], AF.Copy, [f"O{a}"], [f"Oc{a}"])
                ph.op("vector", lambda e: e.reciprocal(out=rden[:], in_=den[:]), ["den"], ["rden"])
                for a in range(2):
                    ph.tt(R[m][a][:], Oc[a][:], rden[:], ALU.mult, [f"Oc{a}", "rden"], [f"R{m}{a}"])
                if m == 1:
                    for a in range(2):
                        ph.stt(R[0][a][:], R[1][a][:], negl[:, 0:1], R[0][a][:], ALU.mult, ALU.add, [f"R1{a}", f"R0{a}", "negl"], [f"R0{a}"])
                        ph.tt(sq[a][:], R[0][a][:], R[0][a][:], ALU.mult, [f"R0{a}"], [f"sqa{a}"])

                    def fin(h=h, qb=qb):
                        for a in range(2):
                            ph.mm(lps[:], ones[:], sq[a][:], a == 0, a == 1, [f"sqa{a}", "ones"], ["ssps"])
                        ph.act(lt[:], lps[:], AF.Ln, ["ssps"], ["lt"], scale=1.0 / 256, bias=EPS)
                        ph.act(rs[:], lt[:], AF.Exp, ["lt"], ["rsa"], scale=-0.5)
                        for a in range(2):
                            ph.stt(act[:, 2 * h + a, qb * 512:(qb + 1) * 512], R[0][a][:], gs[:, a:a + 1], rs[:], ALU.mult, ALU.mult,
                                   [f"R0{a}", "rsa", "gs"], [f"act{2 * h + a}_{qb}"])
                    later.append((p + 4, fin))
        for _, fn in later:
            fn()
        ph.emit()

    def conv_phase():
        ph = new_phase()
        consts = load_consts(ph)
        ones = load_cm(ph, CM_ONES, CM_ONES + 128, "ones")
        ident = load_cm(ph, CM_ID, CM_ID + 128, "ident")
        yb = [ph.sb("ybc", [128, 128 + T], F32) for _ in range(2)]
        ybf = [ph.sb("ybf", [128, 128 + T], BF16) for _ in range(2)]
        dg = [ph.sb("dg", [128, 31, 128], BF16) for _ in range(2)]
        co = ph.sb("co", [128, 16, T], F32)
        sqb = [ph.sb("sqc", [128, T], BF16) for _ in range(2)]
        cb = [ph.sb("cbc", [128, T], BF16) for _ in range(2)]
        s1 = [ph.ps("s1") for _ in range(2)]
        s2 = [ph.ps("s2") for _ in range(2)]
        cps = [ph.ps("cps") for _ in range(4)]
        def prep(j):
            y, yk = yb[j % 2], f"ybc{j % 2}"
            ph.dma("sync", y[:], y_s[j * 128:(j + 1) * 128, :], writes=[yk])
            ph.ts(y[:, 0:128], y[:, 0:128], consts[:, FLAGS:FLAGS + 1], None, ALU.mult, None, [yk, "consts"], [yk])
            y16, y16k = ybf[j % 2], f"ybf{j % 2}"
            ph.act(y16[:], y[:], AF.Copy, [yk], [y16k])
            d, dk = dg[j % 2], f"dg{j % 2}"
            w0 = DWW + j * 31
            in0 = bass.AP(ident, 0, [[128, 128], [0, 31], [1, 128]])
            in1 = bass.AP(consts, w0, [[NCONST, 128], [1, 31], [0, 128]])
            ph.tt(d[:], in0, in1, ALU.mult, ["ident", "consts"], [dk])

        def stats(j):
            s, sk = sqb[j % 2], f"sqc{j % 2}"
            c, cbk = cb[j % 2], f"cbc{j % 2}"
            for hf in range(2):
                sl = slice(hf * 512, (hf + 1) * 512)
                ph.mm(s1[hf][:], ones[:], c[:, sl], j == 0, j == 15, [cbk, "ones"], [f"s1{hf}"])
                ph.mm(s2[hf][:], ones[:], s[:, sl], j == 0, j == 15, [sk, "ones"], [f"s2{hf}"])

        prep(0)
        for j in range(16):
            if j + 1 < 16:
                prep(j + 1)
            y16, y16k = ybf[j % 2], f"ybf{j % 2}"
            d, dk = dg[j % 2], f"dg{j % 2}"
            ck = f"co{j}"
            for hf in range(2):
                cp, cpk = cps[(2 * j + hf) % 4], f"cps{(2 * j + hf) % 4}"
                for t in range(31):
                    o0 = 98 + t + hf * 512
                    ph.mm(cp[:], d[:, t, :], y16[:, o0:o0 + 512], t == 0, t == 30, [dk, y16k], [cpk])
                ph.act(co[:, j, hf * 512:(hf + 1) * 512], cp[:], AF.Identity, [cpk, "consts"], [f"{ck}_{hf}"],
                       bias=consts[:, DWB + j:DWB + j + 1])
            s, sk = sqb[j % 2], f"sqc{j % 2}"
            c, cbk = cb[j % 2], f"cbc{j % 2}"
            ph.tt(s[:], co[:, j, :], co[:, j, :], ALU.mult, [f"{ck}_0", f"{ck}_1"], [sk], eng="gpsimd")
            ph.act(c[:], co[:, j, :], AF.Copy, [f"{ck}_0", f"{ck}_1"], [cbk])
            ph.step([lambda j=j: stats(j)])
        ph.flush()
        mu = ph.sb("mu", [128, T], F32)
        var = ph.sb("var", [128, T], F32)
        rstd = ph.sb("rstdc", [128, T], F32)
        for hf in range(2):
            sl = slice(hf * 512, (hf + 1) * 512)
            ph.ts(mu[:, sl], s1[hf][:], 1.0 / 2048, None, ALU.mult, None, [f"s1{hf}"], [f"mu{hf}"])
            ph.tt(var[:, sl], mu[:, sl], mu[:, sl], ALU.mult, [f"mu{hf}"], [f"var{hf}"])
            ph.stt(var[:, sl], s2[hf][:], 1.0 / 2048, var[:, sl], ALU.mult, ALU.subtract, [f"s2{hf}", f"var{hf}"], [f"var{hf}"])
            ph.act(var[:, sl], var[:, sl], AF.Ln, [f"var{hf}"], [f"var{hf}"], bias=EPS)
            ph.act(rstd[:, sl], var[:, sl], AF.Exp, [f"var{hf}"], [f"rstdc{hf}"], scale=-0.5)
        tb = [ph.sb("tbc", [128, T], F32) for _ in range(2)]
        ph.stt(mu[:], mu[:], -1.0, rstd[:], ALU.mult, ALU.mult, ["mu0", "mu1", "rstdc0", "rstdc1"], ["nmr"])
        for j in range(16):
            t, tk = tb[j % 2], f"tbc{j % 2}"
            ph.stt(t[:], co[:, j, :], consts[:, LNG + j:LNG + j + 1], rstd[:], ALU.mult, ALU.mult,
                   [f"co{j}_0", f"co{j}_1", "rstdc0", "rstdc1", "consts"], [tk])
            ph.stt(t[:], mu[:], consts[:, LNG + j:LNG + j + 1], t[:], ALU.mult, ALU.add, ["nmr", tk, "consts"], [tk])
            ph.act(act[:, 16 + j, :], t[:], AF.Silu, [tk, "consts"], [f"act{16 + j}"], bias=consts[:, LNB + j:LNB + j + 1])
        if debug:
            ph.dma("sync", chunked(mix_dbg), act[:], reads=[f"act{16 + j}" for j in range(16)])
        ph.emit()

    def gemm_phase_units(ph, wview, ntiles, wb, unit_fn):
        tiles = [[(lambda b: b[:], wview[:, :, i * 256:(i + 1) * 256], "gpsimd")] for i in range(ntiles)]
        pq = [ph.ps("pq") for _ in range(4)]
        cnt = [0]

        def compute(i, buf, bkey):
            if i == 0:
                for kc in range(32):
                    for q in range(4):
                        cc, hf = q // 2, q % 2
                        ph.mm(pq[q][:], buf[:, kc, cc * 128:(cc + 1) * 128], act[:, kc, hf * 512:(hf + 1) * 512],
                              kc == 0, kc == 31, [bkey, f"act{kc}"], [f"pq{q}"])
                for q in range(4):
                    cnt[0] += 1
                    unit_fn(q, q // 2, q % 2, pq[q], f"pq{q}")
                return
            for cc in range(2):
                for hf in range(2):
                    u = cnt[0]
                    cnt[0] += 1
                    ps, pk = pq[u % 4], f"pq{u % 4}"
                    for kc in range(32):
                        ph.mm(ps[:], buf[:, kc, cc * 128:(cc + 1) * 128], act[:, kc, hf * 512:(hf + 1) * 512],
                              kc == 0, kc == 31, [bkey, f"act{kc}"], [pk])
                    unit_fn(u, i * 2 + cc, hf, ps, pk)

        wstream(ph, tiles, wb, compute)

    def outproj_phase():
        ph = new_phase()
        wb = [(ph.sb("wb", [128, 32, 256], BF16), f"wb{i}") for i in range(2)]
        xs = [ph.sb("xs", [128, 512], F32) for _ in range(3)]
        xov = chunked(xo)
        x2v = chunked(x2T)
        ones = load_cm(ph, CM_ONES, CM_ONES + 128, "ones")
        ss = [ph.ps("ss") for _ in range(2)]
        sqo = [ph.sb("sqo", [128, 512], BF16) for _ in range(3)]

        def unit(u, chunk, hf, ps, pk):
            x_, xk = xs[u % 3], f"xs{u % 3}"
            sl = slice(hf * 512, (hf + 1) * 512)
            ph.dma("sync", x_[:], xov[:, chunk, sl], writes=[xk])
            ph.tt(x_[:], ps[:], x_[:], ALU.add, [pk, xk], [xk])
            ph.dma("sync", x2v[:, chunk, sl], x_[:], reads=[xk])
            q, qk = sqo[u % 3], f"sqo{u % 3}"
            ph.tt(q[:], x_[:], x_[:], ALU.mult, [xk], [qk], eng="gpsimd")

            def cont():
                ph.mm(ss[hf][:], ones[:], q[:], chunk == 0, chunk == 31, [qk, "ones"], [f"ss{hf}"])
            ph.step([cont])

        gemm_phase_units(ph, chunked(w_out), 16, wb, unit)
        ph.flush()
        rsd = ph.sb("rsd", [128, T], F32)
        for hf in range(2):
            sl = slice(hf * 512, (hf + 1) * 512)
            ph.act(rsd[:, sl], ss[hf][:], AF.Ln, [f"ss{hf}"], [f"rsd{hf}"], scale=1.0 / 4096, bias=EPS)
            ph.act(rsd[:, sl], rsd[:, sl], AF.Exp, [f"rsd{hf}"], [f"rsd{hf}"], scale=-0.5)
        ph.dma("sync", rS, rsd[:], reads=["rsd0", "rsd1"])
        ph.emit()

    def peer_score_phase():
        ph = new_phase()
        consts = load_consts(ph)
        ones = load_cm(ph, CM_ONES, CM_ONES + 128, "ones")
        rstd, ss = norm_to_act(ph, x2T, G_FFN, consts, ones, two_pass=True, rstd_src=rS)
        wb = [(ph.sb("wb", [128, 32, 256], BF16), f"wb{i}") for i in range(2)]
        qp = ph.sb("qp", [128, 16, T], BF16)
        ksb = ph.sb("ksb", [128, 16, 128], BF16)
        ph.dma("gpsimd", ksb[:], ksub.rearrange("a d n -> d a n"), writes=["ksb"])

        def unit(u, chunk, hf, ps, pk):
            ph.act(qp[:, chunk, hf * 512:(hf + 1) * 512], ps[:], AF.Copy, [pk], [f"qp{chunk}_{hf}"])

        gemm_phase_units(ph, chunked(wq), 8, wb, unit)

        sc = [ph.ps("sc") for _ in range(2)]
        Sall = [ph.sb("Sall", [128, 16, 128], F32) for _ in range(1)]
        Sw = ph.sb("Sw", [128, 16, 128], F32)
        t16 = ph.sb("t16", [128, 16, 16], F32)
        cand = ph.sb("cand", [128, 8 * 16, 16], F32)
        candw = ph.sb("candw", [128, 8 * 16, 16], F32)
        b16 = [ph.sb("b16", [128, 8, 16], F32) for _ in range(2)]
        ex = ph.sb("ex", [128, 8, 16], F32)
        negm = ph.sb("negm", [128, 8], F32)
        Z = ph.sb("Z", [128, 8], F32)
        lnZ = ph.sb("lnZ", [128, 8], F32)
        off = ph.sb("off", [128, 8], F32)
        cc_ = ph.sb("cc", [128, 8], F32)
        ec = [ph.sb("ec", [128, 8], F32) for _ in range(2)]
        bt = [ph.sb("bt", [128, 8, 128], F32) for _ in range(2)]
        bSv = bS.rearrange("p (t h n) -> p t h n", t=8, h=8)
        s2Sv = s2S.rearrange("p (t h n) -> p t h n", t=8, h=8)
        ecSv = ecS.rearrange("p (t h) -> p t h", t=8)
        for tt in range(8):
            sa, sak = Sall[0], "Sall0"
            for q4 in range(4):
                for i in range(4):
                    hc = q4 * 4 + i
                    ph.mm(sc[q4 % 2][:, i * 128:(i + 1) * 128], qp[:, hc, tt * 128:(tt + 1) * 128], ksb[:, hc, :], True, True,
                          [f"qp{hc}_{tt // 4}", "ksb"], [f"sc{q4 % 2}"])
                ph.act(sa[:, q4 * 4:(q4 + 1) * 4, :], sc[q4 % 2][:], AF.Copy, [f"sc{q4 % 2}"], [f"{sak}_{q4}"])
            for hc in range(16):
                ph.op("vector", lambda e, hc=hc, sa=sa: e.max(out=t16[:, hc, 0:8], in_=sa[:, hc, :]), [f"{sak}_{hc // 4}"], ["t16A"])
            for hc in range(16):
                ph.op("vector", lambda e, hc=hc, sa=sa: e.match_replace(out=Sw[:, hc, :], in_to_replace=t16[:, hc, 0:8],
                                                                        in_values=sa[:, hc, :], imm_value=-1e30),
                      [f"{sak}_{hc // 4}", "t16A"], ["SwA"])
            for hc in range(16):
                ph.op("vector", lambda e, hc=hc: e.max(out=t16[:, hc, 8:16], in_=Sw[:, hc, :]), ["SwA"], ["t16B"])
            b_, bk = b16[tt % 2], f"b16{tt % 2}"
            for h in range(8):
                in0 = bass.AP(t16, (2 * h) * 16, [[256, 128], [1, 16], [0, 16]])
                in1 = bass.AP(t16, (2 * h + 1) * 16, [[256, 128], [0, 16], [1, 16]])
                ph.tt(cand[:, h * 16:(h + 1) * 16, :], in0, in1, ALU.add, ["t16A", "t16B"], ["candA"])
            for h in range(8):
                ph.op("vector", lambda e, h=h, b_=b_: e.max(out=b_[:, h, 0:8], in_=cand[:, h * 16:(h + 1) * 16, :]), ["candA"], [bk])
            for h in range(8):
                ph.op("vector", lambda e, h=h, b_=b_: e.match_replace(out=candw[:, h * 16:(h + 1) * 16, :], in_to_replace=b_[:, h, 0:8],
                                                                      in_values=cand[:, h * 16:(h + 1) * 16, :], imm_value=-1e30),
                      ["candA", bk], ["candwA"])
            for h in range(8):
                ph.op("vector", lambda e, h=h, b_=b_: e.max(out=b_[:, h, 8:16], in_=candw[:, h * 16:(h + 1) * 16, :]), ["candwA"], [bk])
            ph.ts(negm[:], b_[:, :, 0], -1.0, None, ALU.mult, None, [bk], ["negm"])
            for h in range(8):
                ph.act(ex[:, h, :], b_[:, h, :], AF.Exp, [bk, "negm"], ["ex", "Z"], bias=negm[:, h:h + 1], accum_out=Z[:, h:h + 1])
            ph.act(lnZ[:], Z[:], AF.Ln, ["Z"], ["lnZ"])
            ph.tt(off[:], negm[:], lnZ[:], ALU.subtract, ["negm", "lnZ"], ["off"])
            ph.tt(cc_[:], b_[:, :, 15], off[:], ALU.add, [bk, "off"], ["cc"])
            e_, ek = ec[tt % 2], f"ec{tt % 2}"
            ph.act(e_[:], cc_[:], AF.Exp, ["cc"], [ek])
            ph.ts(e_[:], e_[:], 1.0 - 2e-5, None, ALU.mult, None, [ek], [ek])
            bt_, btk = bt[tt % 2], f"bt{tt % 2}"
            ph.ts(lnZ[:], b_[:, :, 15], -1.0, None, ALU.mult, None, [bk, "lnZ", "off"], ["lnZ"])
            for h in range(8):
                ph.ts(bt_[:, h, :], sa[:, 2 * h, :], lnZ[:, h:h + 1], None, ALU.add, None, [f"{sak}_{h // 2}", "lnZ"], [btk])
            ph.dma("sync", bSv[:, tt, :, :], bt_[:], reads=[btk])
            sav = sa[:].rearrange("p (h c) n -> p h c n", c=2)
            ph.dma("sync", s2Sv[:, tt, :, :], sav[:, :, 1, :], reads=[f"{sak}_{q}" for q in range(4)])
            ph.dma("sync", ecSv[:, tt, :], e_[:], reads=[ek])
        ph.emit()

    def peer_gate_phase():
        ph = new_phase()
        ident = load_cm(ph, CM_ID, CM_ID + 128, "ident")
        beta = ph.sb("beta", [128, 64, 128], F32)
        s2 = ph.sb("s2", [128, 64, 128], F32)
        ecb = ph.sb("ecb", [128, 64], F32)
        ph.dma("sync", ecb[:], ecS, writes=["ecb"])
        betav = bS.rearrange("p (a n) -> p a n", n=128)
        s2v = s2S.rearrange("p (a n) -> p a n", n=128)
        for tt in range(8):
            ph.dma("sync", s2[:, tt * 8:tt * 8 + 8, :], s2v[:, tt * 8:tt * 8 + 8, :], writes=[f"s2_{tt}"])
            ph.dma("sync", beta[:, tt * 8:tt * 8 + 8, :], betav[:, tt * 8:tt * 8 + 8, :], writes=[f"beta_{tt}"])
        NACT = 5

        def pre_exp(tt):
            a0 = tt * 8 + NACT
            ph.act(s2[:, a0:tt * 8 + 8, :], s2[:, a0:tt * 8 + 8, :], AF.Exp, [f"s2_{tt}"], [f"s2_{tt}"])
            ph.act(beta[:, a0:tt * 8 + 8, :], beta[:, a0:tt * 8 + 8, :], AF.Exp, [f"beta_{tt}"], [f"beta_{tt}"])
        D = 2
        NE, NG = 3, D + 2
        Eb = [ph.sb("Eb", [128, 8, 128], F32) for _ in range(NE)]
        Gb = [ph.sb("Gb", [128, 8, 128], BF16) for _ in range(NG)]
        diag = ph.sb("diag", [128, 64, 128], BF16)
        for a in range(64):
            ph.ts(diag[:, a, :], ident[:], ecb[:, a:a + 1], None, ALU.mult, None, ["ident", "ecb"], [f"diag_{a // 8}"])
        gl = [ph.sb("gl", [128, 512], F32) for _ in range(2)]
        pt = [ph.sb("pt", [128, T], BF16) for _ in range(2)]
        aT = [[ph.ps("aT") for _ in range(2)] for _ in range(2)]
        gT = [[ph.ps("gT") for _ in range(2)] for _ in range(2)]
        wb = [(ph.sb("wbu", [128, 32, 256], BF16), f"wbu{i}") for i in range(2)]
        uv = chunked(uT)
        gcount = [0]
        NU = 128 * 8
        fifo = []

        def gate_ops(n1, tt):
            g = gcount[0]
            gcount[0] += 1
            E, Ek = Eb[g % NE], f"Eb{g % NE}"
            Gm, Gk = Gb[g % NG], f"Gb{g % NG}"
            for h in range(8):
                a = tt * 8 + h
                if h < NACT:
                    ph.act(E[:, h, :], s2[:, a, :], AF.Exp, [f"s2_{tt}", f"beta_{tt}"], [Ek], bias=beta[:, a, n1:n1 + 1])
                else:
                    ph.ts(E[:, h, :], s2[:, a, :], beta[:, a, n1:n1 + 1], None, ALU.mult, None, [f"s2_{tt}", f"beta_{tt}"], [Ek])
            ph.stt(Gm[:], E[:], 1.0 - 2e-5, E[:], ALU.is_ge, ALU.mult, [Ek], [Gk])
            return (Gm, Gk)

        def gate_mms(n1, tt, res):
            par = n1 % 2
            Gm, Gk = res
            for h in range(8):
                ph.mm(gT[par][tt // 4][:, (tt % 4) * 128:(tt % 4 + 1) * 128], Gm[:, h, :], diag[:, tt * 8 + h, :], h == 0, h == 7,
                      [Gk, f"diag_{tt}"], [f"gT{par}{tt // 4}"])

        def issue_gate(g):
            if g < NU:
                fifo.append((g, gate_ops(g // 8, g % 8)))

        for tt in range(8):
            pre_exp(tt)
            gate_mms(0, tt, gate_ops(0, tt))
        for g in range(8, 8 + D):
            issue_gate(g)

        PTv = PT.rearrange("(c p) n -> c p n", p=128)

        def compute(i, buf, bkey):
            for cc in range(2):
                n1 = 2 * i + cc
                par = n1 % 2
                p_, pk = pt[par], f"pt{par}"
                for tt in range(8):
                    st = n1 * 8 + tt
                    issue_gate(st + 8 + D)
                    hf = tt // 4
                    for kc in range((tt % 4) * 8, (tt % 4) * 8 + 8):
                        ph.mm(aT[par][hf][:], buf[:, kc, cc * 128:(cc + 1) * 128], act[:, kc, hf * 512:(hf + 1) * 512],
                              kc == 0, kc == 31, [bkey, f"act{kc}"], [f"aT{par}{hf}"])
                    if st + 8 < NU:
                        g, res = fifo.pop(0)
                        assert g == st + 8
                        gate_mms(g // 8, g % 8, res)
                    if tt % 4 == 3:
                        ph.act(gl[hf][:], aT[par][hf][:], AF.Gelu, [f"aT{par}{hf}"], [f"gl{hf}"])
                        ph.tt(p_[:, hf * 512:(hf + 1) * 512], gT[par][hf][:], gl[hf][:], ALU.mult, [f"gT{par}{hf}", f"gl{hf}"], [pk])
                ph.dma("sync", PTv[n1], p_[:], reads=[pk])

        tiles = [[(lambda b: b[:], uv[:, :, i * 256:(i + 1) * 256], "gpsimd")] for i in range(64)]
        wstream(ph, tiles, wb, compute)
        ph.emit()

    def peer_out_phase():
        ph = new_phase()
        NC3 = 11
        vview = vv.rearrange("(g c p) n -> g p c n", c=4, p=128)
        pview = PT.rearrange("(g c p) n -> g p c n", c=4, p=128)
        x2v, x3v = chunked(x2T), chunked(x3T)
        acc = [ph.ps("acc") for _ in range(8)]
        bufs = [((ph.sb("Vt", [128, 4, 512], BF16), ph.sb("Pt", [128, 4, 512], BF16)), f"vp{i}") for i in range(3)]
        xs = [ph.sb("xs", [128, 512], F32) for _ in range(3)]
        actv = act[:].rearrange("p c (a t) -> p (c a) t", a=2)
        pc2 = ph.sb("pc2", [128, 64, 512], BF16)
        pc3 = ph.sb("pc3", [128, NC3 * 4, 512], BF16)

        def c0(eg):
            return actv[:, 4 * eg:4 * eg + 4, :] if eg < 16 else pc2[:, 4 * (eg - 16):4 * (eg - 16) + 4, :]

        def c1(eg):
            return pc3[:, 4 * eg:4 * eg + 4, :]

        tiles = []
        for db in range(8):
            for eg in range(32):
                ent = [(lambda b: b[0][:], vview[eg][:, :, db * 512:(db + 1) * 512], "gpsimd")]
                if db == 0:
                    ent.append((lambda b, eg=eg: c0(eg), pview[eg][:, :, 0:512], "sync", f"pcA{eg}"))
                    if eg < NC3:
                        ent.append((lambda b, eg=eg: c1(eg), pview[eg][:, :, 512:1024], "sync", f"pcC{eg}"))
                if eg >= NC3:
                    ent.append((lambda b: b[1][:], pview[eg][:, :, 512:1024], "sync"))
                tiles.append(ent)
        cnt = [0]

        def compute(i, buf, bkey):
            db, eg = i // 32, i % 32
            Vt, Pt = buf
            r0, k0 = c0(eg), f"pcA{eg}"
            if eg < NC3:
                r1, k1 = c1(eg), f"pcC{eg}"
            else:
                r1, k1 = Pt, bkey
            for ec in range(4):
                for dc in range(4):
                    for hf in range(2):
                        r, rk = (r0, k0) if hf == 0 else (r1, k1)
                        ph.mm(acc[dc * 2 + hf][:], Vt[:, ec, dc * 128:(dc + 1) * 128], r[:, ec, :],
                              eg == 0 and ec == 0, eg == 31 and ec == 3, [bkey, rk], [f"acc{dc * 2 + hf}"])
            if eg == 31:
                for dc in range(4):
                    for hf in range(2):
                        u = cnt[0]
                        cnt[0] += 1
                        x_, xk = xs[u % 3], f"xs{u % 3}"
                        sl = slice(hf * 512, (hf + 1) * 512)
                        ph.dma("sync", x_[:], x2v[:, db * 4 + dc, sl], writes=[xk])
                        ph.tt(x_[:], acc[dc * 2 + hf][:], x_[:], ALU.add, [f"acc{dc * 2 + hf}", xk], [xk])
                        ph.dma("sync", x3v[:, db * 4 + dc, sl], x_[:], reads=[xk])

        wstream(ph, tiles, bufs, compute)
        ph.emit()

    def ple_phase():
        ph = new_phase()
        consts = load_consts(ph)
        ones = load_cm(ph, CM_ONES, CM_ONES + 128, "ones")
        rstd, ss = norm_to_act(ph, x3T, G_PLE, consts, ones)
        wb = [(ph.sb("wb", [128, 32, 256], BF16), f"wb{i}") for i in range(2)]
        wpb = ph.sb("wpb", [128, 2, 4096], BF16)
        ptb = ph.sb("ptb", [128, 2, T], BF16)
        ph.dma("gpsimd", wpb[:], chunked(wp), writes=["wpb"])
        ph.dma("gpsimd", ptb[:], chunked(pT), writes=["ptb"])
        sg = [ph.sb("sgp", [128, 512], F32) for _ in range(3)]
        xs = [ph.sb("xs", [128, 512], F32) for _ in range(3)]
        pp = [ph.ps("pp") for _ in range(2)]
        x3v, ov = chunked(x3T), chunked(outT)

        def unit(u, chunk, hf, ps, pk):
            s, sk = sg[u % 3], f"sgp{u % 3}"
            ph.tt(s[:], ps[:], rstd[:, hf * 512:(hf + 1) * 512], ALU.mult, [pk, f"rstd{hf}"], [sk])
            ph.act(s[:], s[:], AF.Sigmoid, [sk], [sk])
            x_, xk = xs[u % 3], f"xs{u % 3}"
            sl = slice(hf * 512, (hf + 1) * 512)
            ph.dma("sync", x_[:], x3v[:, chunk, sl], writes=[xk])

            def cont():
                p_, ppk = pp[u % 2], f"pp{u % 2}"
                for kc in range(2):
                    ph.mm(p_[:], wpb[:, kc, chunk * 128:(chunk + 1) * 128], ptb[:, kc, sl], kc == 0, kc == 1, ["wpb", "ptb"], [ppk])
                ph.tt(s[:], p_[:], s[:], ALU.mult, [ppk, sk], [sk])
                ph.tt(x_[:], x_[:], s[:], ALU.add, [xk, sk], [xk])
                ph.dma("sync", ov[:, chunk, sl], x_[:], reads=[xk])
            ph.step([cont])

        gemm_phase_units(ph, chunked(wg), 16, wb, unit)
        ph.emit()

    phases = [lambda: proj_phase(True), lambda: proj_phase(False), attn_phase, conv_phase, outproj_phase,
              peer_score_phase, peer_gate_phase, peer_out_phase, ple_phase]
    for i, f in enumerate(phases[:nph]):
        f()
        if i == 1:
            hes.close()
    if nph < 2:
        hes.close()
    kes.close()
    return nc


def make_inputs(x, p, mix_norm_g, w_in, q_norm_g, k_norm_g, lambda_q, lambda_k, subln_g, glu_b, dw_kernel, dw_b,
                conv_ln_g, conv_ln_b, w_out, ffn_norm_g, peer_w_query, peer_sub_keys, peer_u, peer_v, ple_norm_g,
                ple_gate_w, ple_proj_w):
    f = lambda a: np.ascontiguousarray(np.asarray(a, dtype=np.float32))
    x = f(x)
    p = f(p)

    def pc(v):
        v = f(v).reshape(-1, 128)
        return v.T

    consts = np.zeros((128, NCONST), np.float32)
    consts[:, G_MIX:G_MIX + 32] = pc(mix_norm_g[0])
    consts[:, G_FFN:G_FFN + 32] = pc(ffn_norm_g[0])
    consts[:, G_PLE:G_PLE + 32] = pc(ple_norm_g[0])
    consts[:, QKG:QKG + 2] = f(q_norm_g[0]).T
    consts[:, QKG + 2:QKG + 4] = f(k_norm_g[0]).T
    consts[:, LAM:LAM + 2] = f(lambda_q[0]).T
    consts[:, LAM + 2:LAM + 4] = f(lambda_k[0]).T
    consts[:, SUBG:SUBG + 2] = pc(subln_g[0])
    consts[:, GLUB:GLUB + 32] = pc(glu_b[0])
    consts[:, DWB:DWB + 16] = pc(dw_b[0])
    consts[:, LNG:LNG + 16] = pc(conv_ln_g[0])
    consts[:, LNB:LNB + 16] = pc(conv_ln_b[0])
    dk = f(dw_kernel[0])
    consts[:, DWW:] = dk.reshape(31, 16, 128).transpose(2, 1, 0).reshape(128, 16 * 31)
    cm = np.zeros((128, NCM), np.float32)
    cm[:, CM_ID:CM_ID + 128] = np.eye(128, dtype=np.float32)
    cm[:, CM_ONES:CM_ONES + 128] = 1.0
    kk = np.arange(128)[:, None]
    qq = np.arange(512)[None, :]
    for o in range(4):
        cm[:, CM_MASK + o * 512:CM_MASK + (o + 1) * 512] = (qq >= 128 * o + kk).astype(np.float32)
    shared = {
        "cm": cm,
        "w_in": f(w_in[0]), "w_out": f(w_out[0]), "wq": f(peer_w_query[0]),
        "ksub": np.ascontiguousarray(f(peer_sub_keys[0]).reshape(16, 128, 128).transpose(0, 2, 1)),
        "uT": np.ascontiguousarray(f(peer_u[0]).T), "v": f(peer_v[0]),
        "wg": f(ple_gate_w[0]), "wp": f(ple_proj_w[0]),
    }
    in_maps = []
    for c in range(8):
        b, half = c // 2, c % 2
        cc = consts.copy()
        cc[:, FLAGS] = 1.0 if half == 1 else 0.0
        cc[:, FLAGS + 1] = 0.0 if half == 1 else -30000.0
        d = dict(shared)
        d["consts"] = cc
        d["xo"] = np.ascontiguousarray(x[b, half * T:(half + 1) * T].T)
        d["xc"] = np.ascontiguousarray(x[b, 0:T].T)
        d["pT"] = np.ascontiguousarray(p[0, b, half * T:(half + 1) * T].T)
        in_maps.append(d)
    return in_maps


def kernel(**inputs):
    in_maps = make_inputs(**inputs)
    nc = build_nc()
    res = run_bass_kernel_spmd(nc, in_maps, core_ids=list(range(8)))
    out = np.zeros((4, 2048, 4096), np.float32)
    for c in range(8):
        b, half = c // 2, c % 2
        out[b, half * T:(half + 1) * T] = res.results[c]["outT"].T
    return out
```

```python
import os
import numpy as np
import concourse.bass as bass
import concourse.mybir as mybir
from concourse.bass_utils import run_bass_kernel_spmd
from contextlib import ExitStack

F32 = mybir.dt.float32
BF16 = mybir.dt.bfloat16
AF = mybir.ActivationFunctionType
ALU = mybir.AluOpType

EPS = 1e-6
T = 1024
ENGS = ["sync", "scalar", "vector", "tensor", "gpsimd"]
NRING = 8

G_MIX, G_FFN, G_PLE, QKG, LAM, SUBG, GLUB, DWB, LNG, LNB, FLAGS, DWW = 0, 32, 64, 96, 100, 104, 106, 138, 154, 170, 186, 188
NCONST = DWW + 16 * 31
CM_ID, CM_ONES, CM_MASK = 0, 128, 256
NCM = 256 + 4 * 512


class _Op:
    __slots__ = ("eng", "fn", "deps", "dma", "sem", "val", "need", "guard")

    def __init__(self, eng, fn, dma):
        self.eng = eng
        self.fn = fn
        self.dma = dma
        self.deps = []
        self.sem = None
        self.val = 0
        self.need = dma
        self.guard = None


class Phase:
    def __init__(self, nc, pid, G):
        self.nc = nc
        self.pid = pid
        self.G = G
        self.es = ExitStack()
        self.ops = {e: [] for e in ENGS}
        self.state = {}
        self.n = 0
        self.pend = []

    def sb(self, name, shape, dtype):
        self.n += 1
        return self.es.enter_context(self.nc.sbuf_tensor(f"{name}_{self.pid}_{self.n}", shape, dtype))

    def ps(self, name, shape=(128, 512), dtype=F32):
        self.n += 1
        return self.es.enter_context(self.nc.psum_tensor(f"{name}_{self.pid}_{self.n}", list(shape), dtype))

    def _rec(self, op, reads, writes):
        for k in reads:
            st = self.state.setdefault(k, [None, []])
            if st[0] is not None:
                op.deps.append((st[0], True))
            st[1].append(op)
        for k in writes:
            st = self.state.setdefault(k, [None, []])
            if st[0] is not None and st[0] is not op:
                op.deps.append((st[0], False))
            for r in st[1]:
                if r is not op:
                    op.deps.append((r, False))
            st[0] = op
            st[1] = []
        self.ops[op.eng].append(op)
        return op

    def op(self, eng, fn, reads=(), writes=()):
        return self._rec(_Op(eng, fn, False), reads, writes)

    def dma(self, eng, out, in_, reads=(), writes=()):
        return self._rec(_Op(eng, lambda e: e.dma_start(out=out, in_=in_), True), reads, writes)

    def mm(self, out, lhsT, rhs, start, stop, reads, writes):
        return self.op("tensor", lambda e: e.matmul(out, lhsT, rhs, start=start, stop=stop), reads, writes)

    def act(self, out, in_, func, reads, writes, **kw):
        return self.op("scalar", lambda e: e.activation(out=out, in_=in_, func=func, **kw), reads, writes)

    def ts(self, out, in0, s1, s2, op0, op1, reads, writes, eng="vector"):
        if op1 is None:
            return self.op(eng, lambda e: e.tensor_scalar(out=out, in0=in0, scalar1=s1, scalar2=None, op0=op0), reads, writes)
        return self.op(eng, lambda e: e.tensor_scalar(out=out, in0=in0, scalar1=s1, scalar2=s2, op0=op0, op1=op1), reads, writes)

    def stt(self, out, in0, scalar, in1, op0, op1, reads, writes):
        return self.op("vector", lambda e: e.scalar_tensor_tensor(out=out, in0=in0, scalar=scalar, in1=in1, op0=op0, op1=op1), reads, writes)

    def tt(self, out, in0, in1, op, reads, writes, eng="vector"):
        return self.op(eng, lambda e: e.tensor_tensor(out=out, in0=in0, in1=in1, op=op), reads, writes)

    def step(self, deferred):
        old = self.pend
        self.pend = list(deferred)
        for f in old:
            f()

    def flush(self):
        self.step([])

    def emit(self):
        self.flush()
        nc = self.nc
        es = self.es
        G = self.G
        if "esem" not in G:
            kes = G["kes"]
            G["esem"] = {e: kes.enter_context(nc.semaphore(f"e{e}")) for e in ENGS}
            G["rings"] = {e: [kes.enter_context(nc.semaphore(f"d{e}{i}")) for i in range(NRING)]
                          for e in ("sync", "scalar", "gpsimd")}
            G["cnt"] = {e: 0 for e in ENGS}
            G["rcnt"] = {e: [0] * NRING for e in ENGS}
            G["ri"] = {e: 0 for e in ENGS}
        esem, rings = G["esem"], G["rings"]

        def needs_wait(op, dep, raw):
            if dep.dma or op.dma:
                return True
            if dep.eng != op.eng:
                return True
            if op.eng == "tensor":
                return False
            return raw

        for e in ENGS:
            for op in self.ops[e]:
                for dep, raw in op.deps:
                    if needs_wait(op, dep, raw):
                        dep.need = True
        final = {}
        for e in ENGS:
            cnt = G["cnt"][e]
            rcnt = G["rcnt"][e]
            ri = G["ri"][e]
            for op in self.ops[e]:
                if op.dma:
                    op.sem = rings[e][ri]
                    op.guard = rcnt[ri]
                    rcnt[ri] += 16
                    op.val = rcnt[ri]
                    ri = (ri + 1) % NRING
                elif op.need:
                    cnt += 1
                    op.sem = esem[e]
                    op.val = cnt
            final[e] = list(zip(rings.get(e, []), list(rcnt)))
            G["cnt"][e] = cnt
            G["ri"][e] = ri
        ops = self.ops

        def body(ename):
            def f(eng):
                waited = {}
                for op in ops[ename]:
                    ws = {}
                    for dep, raw in op.deps:
                        if needs_wait(op, dep, raw):
                            k = id(dep.sem)
                            if ws.get(k, (None, 0))[1] < dep.val:
                                ws[k] = (dep.sem, dep.val)
                    if op.dma and op.guard > 0:
                        k = id(op.sem)
                        if ws.get(k, (None, 0))[1] < op.guard:
                            ws[k] = (op.sem, op.guard)
                    for k, (sem, val) in ws.items():
                        if waited.get(k, 0) < val:
                            eng.wait_ge(sem, val)
                            waited[k] = val
                    ins = op.fn(eng)
                    if op.dma:
                        ins.then_inc(op.sem, 16)
                    elif op.need:
                        ins.then_inc(op.sem, 1)
                for sem, val in final[ename]:
                    if val > 0 and waited.get(id(sem), 0) < val:
                        eng.wait_ge(sem, val)
            return f

        with nc.Block() as block:
            for e in ENGS:
                getattr(block, e)(body(e))
        es.close()


def wstream(ph, tiles, bufs, compute):
    nb = len(bufs)

    def issue(i):
        buf, key = bufs[i % nb]
        for ent in tiles[i]:
            dst_fn, src, eng = ent[:3]
            ph.dma(eng, dst_fn(buf), src, writes=[ent[3] if len(ent) > 3 else key])

    for i in range(min(nb - 1, len(tiles))):
        issue(i)
    for i in range(len(tiles)):
        if i + nb - 1 < len(tiles):
            issue(i + nb - 1)
        compute(i, *bufs[i % nb])


def build_nc(nph=99, debug=False):
    nc = bass.Bass("TRN2", target_bir_lowering=False)

    def din(name, shape):
        return nc.dram_tensor(name, list(shape), F32, kind="ExternalInput").ap()

    def scratch(name, shape, dt):
        return nc.dram_tensor(name, list(shape), dt, kind="ExternalOutput" if debug else "Internal").ap()

    xo = din("xo", (4096, T))
    xc = din("xc", (4096, T))
    pT = din("pT", (256, T))
    consts_d = din("consts", (128, NCONST))
    cm_d = din("cm", (128, NCM))
    w_in = din("w_in", (4096, 10240))
    w_out = din("w_out", (4096, 4096))
    wq = din("wq", (4096, 2048))
    ksub = din("ksub", (16, 128, 128))
    uT = din("uT", (4096, 16384))
    vv = din("v", (16384, 4096))
    wg = din("wg", (4096, 4096))
    wp = din("wp", (256, 4096))
    outT = nc.dram_tensor("outT", [4096, T], F32, kind="ExternalOutput").ap()

    qT_s = scratch("qT_s", (2048, T), BF16)
    kT_s = scratch("kT_s", (2048, 2 * T), BF16)
    v_s = scratch("v_s", (2 * T, 2048), BF16)
    y_s = scratch("y_s", (2048, 128 + T), F32)
    x2T = scratch("x2T", (4096, T), F32)
    x3T = scratch("x3T", (4096, T), F32)
    bS = scratch("bS", (128, 8 * 8 * 128), F32)
    s2S = scratch("s2S", (128, 8 * 8 * 128), F32)
    ecS = scratch("ecS", (128, 64), F32)
    rS = scratch("rS", (128, T), F32)
    PT = scratch("PT", (16384, T), BF16)
    mix_dbg = scratch("mix_dbg", (4096, T), BF16) if debug else None

    kes = ExitStack()
    act = kes.enter_context(nc.sbuf_tensor("act", [128, 32, T], BF16))
    hes = ExitStack()
    halo = hes.enter_context(nc.sbuf_tensor("halo", [128, 32, 128], BF16))
    halo_r = hes.enter_context(nc.sbuf_tensor("halo_r", [128, 128], F32))
    pid = [0]
    G = {"kes": kes}

    def new_phase():
        pid[0] += 1
        return Phase(nc, pid[0], G)

    def chunked(ap):
        return ap.rearrange("(c p) n -> p c n", p=128)

    def load_consts(ph):
        c = ph.sb("consts", [128, NCONST], F32)
        ph.dma("sync", c[:], consts_d, writes=["consts"])
        return c

    def load_cm(ph, lo, hi, name):
        t = ph.sb(name, [128, hi - lo], BF16)
        ph.dma("gpsimd", t[:], cm_d[:, lo:hi], writes=[name])
        return t

    def norm_to_act(ph, src, gcol, consts, ones, two_pass=False, rstd_src=None):
        xv = chunked(src)
        xb = [ph.sb("xb", [128, T], F32) for _ in range(3)]
        if rstd_src is not None:
            rstd = ph.sb("rstd", [128, T], F32)
            ph.dma("sync", rstd[:], rstd_src, writes=["rstd0", "rstd1"])
            for c in range(32):
                x_, xk = xb[c % 3], f"xb{c % 3}"
                ph.dma("sync", x_[:], xv[:, c, :], writes=[xk])
                ph.stt(act[:, c, :], x_[:], consts[:, gcol + c:gcol + c + 1], rstd[:], ALU.mult, ALU.mult,
                       [xk, "rstd0", "rstd1", "consts"], [f"act{c}"])
            return rstd, None
        ss = [ph.ps("ss") for _ in range(2)]
        sq = [ph.sb("sq", [128, T], BF16) for _ in range(2)]
        for c in range(32):
            x_, xk = xb[c % 3], f"xb{c % 3}"
            s_, sk = sq[c % 2], f"sq{c % 2}"
            ph.dma("sync", x_[:], xv[:, c, :], writes=[xk])
            ph.tt(s_[:], x_[:], x_[:], ALU.mult, [xk], [sk], eng="gpsimd")
            if not two_pass:
                ph.ts(act[:, c, :], x_[:], consts[:, gcol + c:gcol + c + 1], None, ALU.mult, None, [xk, "consts"], [f"act{c}"])
            for hf in range(2):
                ph.mm(ss[hf][:], ones[:], s_[:, hf * 512:(hf + 1) * 512], c == 0, c == 31, [sk, "ones"], [f"ss{hf}"])
        rstd = ph.sb("rstd", [128, T], F32)
        tmp = ph.sb("tmpn", [128, T], F32)
        for hf in range(2):
            sl = slice(hf * 512, (hf + 1) * 512)
            ph.act(tmp[:, sl], ss[hf][:], AF.Ln, [f"ss{hf}"], [f"tmpn{hf}"], scale=1.0 / 4096, bias=EPS)
            ph.act(rstd[:, sl], tmp[:, sl], AF.Exp, [f"tmpn{hf}"], [f"rstd{hf}"], scale=-0.5)
        if two_pass:
            for c in range(32):
                x_, xk = xb[c % 3], f"xb{c % 3}"
                ph.dma("sync", x_[:], xv[:, c, :], writes=[xk])
                ph.stt(act[:, c, :], x_[:], consts[:, gcol + c:gcol + c + 1], rstd[:], ALU.mult, ALU.mult,
                       [xk, "rstd0", "rstd1", "consts"], [f"act{c}"])
        return rstd, ss

    def proj_phase(is_ctx):
        ph = new_phase()
        consts = load_consts(ph)
        ones = load_cm(ph, CM_ONES, CM_ONES + 128, "ones")
        rstd, ss = norm_to_act(ph, xc if is_ctx else xo, G_MIX, consts, ones)
        identf = ph.sb("identf", [128, 2], F32)
        ph.dma("sync", identf[:], cm_d[:, CM_ID:CM_ID + 2], writes=["identf"])
        for tt in range(8):
            ph.mm(ss[0][:, 2 * tt:2 * tt + 2], rstd[:, tt * 128:(tt + 1) * 128], identf[:], True, True,
                  ["rstd0", "rstd1", "identf"], ["ss0"])
        rcol = ph.sb("rcol", [128, 16], F32)
        ph.act(rcol[:], ss[0][:, 0:16], AF.Copy, ["ss0"], ["rcol"])
        if is_ctx:
            ph.op("gpsimd", lambda e: e.tensor_copy(out=halo[:], in_=act[:, :, 896:1024]), [f"act{c}" for c in range(32)], ["halo"])
            ph.op("gpsimd", lambda e: e.tensor_copy(out=halo_r[:], in_=rstd[:, 896:1024]), ["rstd1"], ["halo_r"])
        qkg = ph.sb("qkg", [128, 4], F32)
        ph.ts(qkg[:, 0:2], consts[:, QKG:QKG + 2], 128.0 ** -0.5, None, ALU.mult, None, ["consts"], ["qkg"])
        ph.ts(qkg[:, 2:4], consts[:, QKG + 2:QKG + 4], 1.0, None, ALU.mult, None, ["consts", "qkg"], ["qkg"])
        wv = chunked(w_in)
        tokbase = 0 if is_ctx else T
        pq = [ph.ps("pq") for _ in range(4)]
        ssp = [ph.ps("ssp") for _ in range(2)]
        zc = [ph.sb("zc", [128, 512], F32) for _ in range(3)]
        sqb = [ph.sb("sqb", [128, 512], BF16) for _ in range(3)]
        lnb_ = [ph.sb("lnt", [128, 512], F32) for _ in range(2)]
        rs_ = [ph.sb("rs", [128, 512], F32) for _ in range(2)]
        ob = [ph.sb("ob", [128, 512], BF16) for _ in range(3)]
        vb = [ph.sb("vb", [128, 256], BF16) for _ in range(3)]
        sg = [ph.sb("sg", [128, 512], F32) for _ in range(2)]
        yb = [ph.sb("yb", [128, 512], F32) for _ in range(3)]
        wb = [(ph.sb("wb", [128, 32, 256], BF16), f"wb{i}") for i in range(2)]
        cnt = {"u": 0, "v": 0, "y": 0}
        allact = [f"act{c}" for c in range(32)]

        tiles = []
        kinds = []
        if not is_ctx:
            for i in range(8):
                tiles.append([(lambda b: b[:], wv[:, :, i * 256:(i + 1) * 256], "gpsimd")])
                kinds.append(("q", i))
        for i in range(8):
            tiles.append([(lambda b: b[:], wv[:, :, 2048 + i * 256:2048 + (i + 1) * 256], "gpsimd")])
            kinds.append(("k", i))
        for i in range(8):
            tiles.append([(lambda b: b[:], wv[:, :, 4096 + i * 256:4096 + (i + 1) * 256], "gpsimd")])
            kinds.append(("v", i))
        for j in range(0 if is_ctx else 16):
            tiles.append([(lambda b: b[:, :, 0:128], wv[:, :, 6144 + j * 128:6144 + (j + 1) * 128], "gpsimd"),
                          (lambda b: b[:, :, 128:256], wv[:, :, 8192 + j * 128:8192 + (j + 1) * 128], "gpsimd")])
            kinds.append(("c", j))

        def qk_unit(kind, chunk, hf, buf, bkey, cc):
            u = cnt["u"]
            cnt["u"] += 1
            ps, pk = pq[u % 4], f"pq{u % 4}"
            for kc in range(32):
                ph.mm(ps[:], buf[:, kc, cc * 128:(cc + 1) * 128], act[:, kc, hf * 512:(hf + 1) * 512],
                      kc == 0, kc == 31, [bkey, f"act{kc}"], [pk])
            z, zk = zc[u % 3], f"zc{u % 3}"
            s, sk = sqb[u % 3], f"sqb{u % 3}"
            ph.tt(z[:], ps[:], rstd[:, hf * 512:(hf + 1) * 512], ALU.mult, [pk, f"rstd{hf}"], [zk])
            ph.tt(s[:], z[:], z[:], ALU.mult, [zk], [sk])

            def cont():
                sp, spk = ssp[u % 2], f"ssp{u % 2}"
                ph.mm(sp[:], ones[:], s[:], True, True, [sk, "ones"], [spk])
                l, lk = lnb_[u % 2], f"lnt{u % 2}"
                r, rk = rs_[u % 2], f"rs{u % 2}"
                ph.act(l[:], sp[:], AF.Ln, [spk], [lk], scale=1.0 / 128, bias=EPS)
                ph.act(r[:], l[:], AF.Exp, [lk], [rk], scale=-0.5)
                o, ok = ob[u % 3], f"ob{u % 3}"
                m = chunk % 2
                gc = m if kind == "q" else 2 + m
                ph.stt(o[:], z[:], qkg[:, gc:gc + 1], r[:], ALU.mult, ALU.mult, [zk, rk, "qkg"], [ok])
                if kind == "q":
                    dst = qT_s[chunk * 128:(chunk + 1) * 128, hf * 512:(hf + 1) * 512]
                else:
                    dst = kT_s[chunk * 128:(chunk + 1) * 128, tokbase + hf * 512:tokbase + (hf + 1) * 512]
                ph.dma("sync", dst, o[:], reads=[ok])
            ph.step([cont])

        def compute(i, buf, bkey):
            kind, idx = kinds[i]
            if kind in ("q", "k"):
                for cc in range(2):
                    for hf in range(2):
                        qk_unit(kind, idx * 2 + cc, hf, buf, bkey, cc)
            elif kind == "v":
                for tt in range(8):
                    u = cnt["u"]
                    cnt["u"] += 1
                    ps, pk = pq[u % 4], f"pq{u % 4}"
                    for kc in range(32):
                        ph.mm(ps[:, 0:256], act[:, kc, tt * 128:(tt + 1) * 128], buf[:, kc, :],
                              kc == 0, kc == 31, [bkey, f"act{kc}"], [pk])
                    n = cnt["v"]
                    cnt["v"] += 1
                    o, ok = vb[n % 3], f"vb{n % 3}"
                    ph.act(o[:], ps[:, 0:256], AF.Identity, [pk, "rcol"], [ok], scale=rcol[:, 2 * tt:2 * tt + 1])
                    ph.dma("sync", v_s[tokbase + tt * 128:tokbase + (tt + 1) * 128, idx * 256:(idx + 1) * 256], o[:], reads=[ok])
                    ph.step([])
            else:
                j = idx
                units = [(0, 512, 128), (512, 512, 640), (-1, 128, 0)]
                for t0, n, d0 in units:
                    u = cnt["u"]
                    cnt["u"] += 2
                    pa, pak = pq[u % 4], f"pq{u % 4}"
                    pg, pgk = pq[(u + 1) % 4], f"pq{(u + 1) % 4}"
                    rhs = (lambda kc: halo[:, kc, :]) if t0 < 0 else (lambda kc: act[:, kc, t0:t0 + n])
                    for kc in range(32):
                        ph.mm(pa[:, 0:n], buf[:, kc, 0:128], rhs(kc), kc == 0, kc == 31, [bkey, f"act{kc}"], [pak])
                    for kc in range(32):
                        ph.mm(pg[:, 0:n], buf[:, kc, 128:256], rhs(kc), kc == 0, kc == 31, [bkey, f"act{kc}"], [pgk])
                    k = cnt["y"]
                    cnt["y"] += 1
                    s, sk = sg[k % 2], f"sg{k % 2}"
                    y, yk = yb[k % 3], f"yb{k % 3}"
                    rsl = halo_r[:, 0:128] if t0 < 0 else rstd[:, t0:t0 + n]
                    rkey = "halo_r" if t0 < 0 else f"rstd{t0 // 512}"
                    ph.tt(s[:, 0:n], pg[:, 0:n], rsl, ALU.mult, [pgk, rkey], [sk])
                    ph.act(s[:, 0:n], s[:, 0:n], AF.Sigmoid, [sk, "consts"], [sk], bias=consts[:, GLUB + 16 + j:GLUB + 17 + j])
                    ph.tt(y[:, 0:n], pa[:, 0:n], rsl, ALU.mult, [pak, rkey], [yk])
                    ph.stt(y[:, 0:n], y[:, 0:n], consts[:, GLUB + j:GLUB + j + 1], s[:, 0:n], ALU.add, ALU.mult,
                           [yk, sk, "consts"], [yk])
                    ph.dma("sync", y_s[j * 128:(j + 1) * 128, d0:d0 + n], y[:, 0:n], reads=[yk])
                    ph.step([])

        wstream(ph, tiles, wb, compute)
        ph.emit()

    def attn_phase():
        ph = new_phase()
        consts = load_consts(ph)
        ones = load_cm(ph, CM_ONES, CM_ONES + 128, "ones")
        cmask = load_cm(ph, CM_MASK, CM_MASK + 2048, "cmask")
        prod = ph.sb("prod", [128, 2], BF16)
        ph.tt(prod[:], consts[:, LAM:LAM + 2], consts[:, LAM + 2:LAM + 4], ALU.mult, ["consts"], ["prod"])
        lps = ph.ps("ssps")
        ph.mm(lps[:, 0:2], ones[:], prod[:], True, True, ["prod", "ones"], ["ssps"])
        el = ph.sb("el", [128, 2], F32)
        ph.act(el[:], lps[:, 0:2], AF.Exp, ["ssps"], ["el"])
        negl = ph.sb("negl", [128, 1], F32)
        ph.tt(negl[:], el[:, 1:2], el[:, 0:1], ALU.subtract, ["el"], ["negl"])
        ph.ts(negl[:], negl[:], -0.2, None, ALU.add, None, ["negl"], ["negl"])
        gs = ph.sb("gs", [128, 2], F32)
        ph.ts(gs[:], consts[:, SUBG:SUBG + 2], 0.8, None, ALU.mult, None, ["consts"], ["gs"])

        qv, kv = chunked(qT_s), chunked(kT_s)
        vview = v_s.rearrange("(t p) n -> p t n", p=128)
        qh = [ph.sb("qh", [128, 2, T], BF16) for _ in range(2)]
        kh = [ph.sb("kh", [128, 2, 2 * T], BF16) for _ in range(2)]
        vh = [ph.sb("vh", [128, 16, 256], BF16) for _ in range(2)]
        sT = [ph.ps("sT") for _ in range(3)]
        den = ph.ps("den")
        O = [ph.ps("O") for _ in range(2)]
        pT_ = [ph.sb("pT", [128, 512], BF16) for _ in range(5)]
        rden = ph.sb("rden", [128, 512], F32)
        R = [[ph.sb("R", [128, 512], F32) for _ in range(2)] for _ in range(2)]
        sq = [ph.sb("sqa", [128, 512], BF16) for _ in range(2)]
        lt = ph.sb("lt", [128, 512], F32)
        rs = ph.sb("rsa", [128, 512], F32)
        pcount = [0]

        def load_head(h):
            b = h % 2
            ph.dma("sync", qh[b][:], qv[:, 2 * h:2 * h + 2, :], writes=[f"qh{b}"])
            ph.dma("sync", kh[b][:], kv[:, 2 * h:2 * h + 2, :], writes=[f"kh{b}"])
            ph.dma("sync", vh[b][:], vview[:, :, h * 256:(h + 1) * 256], writes=[f"vh{b}"])

        load_head(0)
        items = []
        for h in range(8):
            for qb in range(2):
                nj = 12 if qb == 0 else 16
                for m in range(2):
                    for j in range(nj):
                        items.append((h, qb, m, j, nj))
        NS, NP, LA = 3, 5, 2

        def S(p):
            h, qb, m, j, nj = items[p]
            b = h % 2
            st, sk = sT[p % NS], f"sT{p % NS}"
            ph.mm(st[:], kh[b][:, m, j * 128:(j + 1) * 128], qh[b][:, m, qb * 512:(qb + 1) * 512],
                  True, True, [f"kh{b}", f"qh{b}"], [sk])
            pt, pk = pT_[p % NP], f"pT{p % NP}"
            if j < 8:
                ph.act(pt[:], st[:], AF.Exp, [sk, "consts"], [pk], bias=consts[:, FLAGS + 1:FLAGS + 2])
            else:
                ph.act(pt[:], st[:], AF.Exp, [sk], [pk])
            o = j - 8 - 4 * qb
            if o >= 0:
                ph.tt(pt[:], pt[:], cmask[:, o * 512:(o + 1) * 512], ALU.mult, [pk, "cmask"], [pk])

        def PV(p):
            h, qb, m, j, nj = items[p]
            b = h % 2
            pt, pk = pT_[p % NP], f"pT{p % NP}"
            ph.mm(den[:], ones[:], pt[:], j == 0, j == nj - 1, [pk, "ones"], ["den"])
            for a in range(2):
                ph.mm(O[a][:], vh[b][:, j, a * 128:(a + 1) * 128], pt[:], j == 0, j == nj - 1, [pk, f"vh{b}"], [f"O{a}"])

        for p in range(min(LA, len(items))):
            S(p)
        Oc = [ph.sb("Oc", [128, 512], F32) for _ in range(2)]
        later = []
        for p in range(len(items)):
            while later and later[0][0] <= p:
                later.pop(0)[1]()
            h, qb, m, j, nj = items[p]
            if p + LA < len(items):
                S(p + LA)
            if qb == 0 and m == 0 and j == 0 and h + 1 < 8:
                load_head(h + 1)
            PV(p)
            if j == nj - 1:
                for a in range(2):
                    ph.act(Oc[a][:], O[a][:], AF.Copy, [f"O{a}"], [f"Oc{a}"])
                ph.op("vector", lambda e: e.reciprocal(out=rden[:], in_=den[:]), ["den"], ["rden"])
                for a in range(2):
                    ph.tt(R[m][a][:], Oc[a][:], rden[:], ALU.mult, [f"Oc{a}", "rden"], [f"R{m}{a}"])
                if m == 1:
                    for a in range(2):
                        ph.stt(R[0][a][:], R[1][a][:], negl[:, 0:1], R[0][a][:], ALU.mult, ALU.add, [f"R1{a}", f"R0{a}", "negl"], [f"R0{a}"])
                        ph.tt(sq[a][:], R[0][a][:], R[0][a][:], ALU.mult, [f"R0{a}"], [f"sqa{a}"])

                    def fin(h=h, qb=qb):
                        for a in range(2):
                            ph.mm(lps[:], ones[:], sq[a][:], a == 0, a == 1, [f"sqa{a}", "ones"], ["ssps"])
                        ph.act(lt[:], lps[:], AF.Ln, ["ssps"], ["lt"], scale=1.0 / 256, bias=EPS)
                        ph.act(rs[:], lt[:], AF.Exp, ["lt"], ["rsa"], scale=-0.5)
                        for a in range(2):
                            ph.stt(act[:, 2 * h + a, qb * 512:(qb + 1) * 512], R[0][a][:], gs[:, a:a + 1], rs[:], ALU.mult, ALU.mult,
                                   [f"R0{a}", "rsa", "gs"], [f"act{2 * h + a}_{qb}"])
                    later.append((p + 4, fin))
        for _, fn in later:
            fn()
        ph.emit()

    def conv_phase():
        ph = new_phase()
        consts = load_consts(ph)
        ones = load_cm(ph, CM_ONES, CM_ONES + 128, "ones")
        ident = load_cm(ph, CM_ID, CM_ID + 128, "ident")
        yb = [ph.sb("ybc", [128, 128 + T], F32) for _ in range(2)]
        ybf = [ph.sb("ybf", [128, 128 + T], BF16) for _ in range(2)]
        dg = [ph.sb("dg", [128, 31, 128], BF16) for _ in range(2)]
        co = ph.sb("co", [128, 16, T], F32)
        sqb = [ph.sb("sqc", [128, T], BF16) for _ in range(2)]
        cb = [ph.sb("cbc", [128, T], BF16) for _ in range(2)]
        s1 = [ph.ps("s1") for _ in range(2)]
        s2 = [ph.ps("s2") for _ in range(2)]
        cps = [ph.ps("cps") for _ in range(4)]
        def prep(j):
            y, yk = yb[j % 2], f"ybc{j % 2}"
            ph.dma("sync", y[:], y_s[j * 128:(j + 1) * 128, :], writes=[yk])
            ph.ts(y[:, 0:128], y[:, 0:128], consts[:, FLAGS:FLAGS + 1], None, ALU.mult, None, [yk, "consts"], [yk])
            y16, y16k = ybf[j % 2], f"ybf{j % 2}"
            ph.act(y16[:], y[:], AF.Copy, [yk], [y16k])
            d, dk = dg[j % 2], f"dg{j % 2}"
            w0 = DWW + j * 31
            in0 = bass.AP(ident, 0, [[128, 128], [0, 31], [1, 128]])
            in1 = bass.AP(consts, w0, [[NCONST, 128], [1, 31], [0, 128]])
            ph.tt(d[:], in0, in1, ALU.mult, ["ident", "consts"], [dk])

        def stats(j):
            s, sk = sqb[j % 2], f"sqc{j % 2}"
            c, cbk = cb[j % 2], f"cbc{j % 2}"
            for hf in range(2):
                sl = slice(hf * 512, (hf + 1) * 512)
                ph.mm(s1[hf][:], ones[:], c[:, sl], j == 0, j == 15, [cbk, "ones"], [f"s1{hf}"])
                ph.mm(s2[hf][:], ones[:], s[:, sl], j == 0, j == 15, [sk, "ones"], [f"s2{hf}"])

        prep(0)
        for j in range(16):
            if j + 1 < 16:
                prep(j + 1)
            y16, y16k = ybf[j % 2], f"ybf{j % 2}"
            d, dk = dg[j % 2], f"dg{j % 2}"
            ck = f"co{j}"
            for hf in range(2):
                cp, cpk = cps[(2 * j + hf) % 4], f"cps{(2 * j + hf) % 4}"
                for t in range(31):
                    o0 = 98 + t + hf * 512
                    ph.mm(cp[:], d[:, t, :], y16[:, o0:o0 + 512], t == 0, t == 30, [dk, y16k], [cpk])
                ph.act(co[:, j, hf * 512:(hf + 1) * 512], cp[:], AF.Identity, [cpk, "consts"], [f"{ck}_{hf}"],
                       bias=consts[:, DWB + j:DWB + j + 1])
            s, sk = sqb[j % 2], f"sqc{j % 2}"
            c, cbk = cb[j % 2], f"cbc{j % 2}"
            ph.tt(s[:], co[:, j, :], co[:, j, :], ALU.mult, [f"{ck}_0", f"{ck}_1"], [sk], eng="gpsimd")
            ph.act(c[:], co[:, j, :], AF.Copy, [f"{ck}_0", f"{ck}_1"], [cbk])
            ph.step([lambda j=j: stats(j)])
        ph.flush()
        mu = ph.sb("mu", [128, T], F32)
        var = ph.sb("var", [128, T], F32)
        rstd = ph.sb("rstdc", [128, T], F32)
        for hf in range(2):
            sl = slice(hf * 512, (hf + 1) * 512)
            ph.ts(mu[:, sl], s1[hf][:], 1.0 / 2048, None, ALU.mult, None, [f"s1{hf}"], [f"mu{hf}"])
            ph.tt(var[:, sl], mu[:, sl], mu[:, sl], ALU.mult, [f"mu{hf}"], [f"var{hf}"])
            ph.stt(var[:, sl], s2[hf][:], 1.0 / 2048, var[:, sl], ALU.mult, ALU.subtract, [f"s2{hf}", f"var{hf}"], [f"var{hf}"])
            ph.act(var[:, sl], var[:, sl], AF.Ln, [f"var{hf}"], [f"var{hf}"], bias=EPS)
            ph.act(rstd[:, sl], var[:, sl], AF.Exp, [f"var{hf}"], [f"rstdc{hf}"], scale=-0.5)
        tb = [ph.sb("tbc", [128, T], F32) for _ in range(2)]
        ph.stt(mu[:], mu[:], -1.0, rstd[:], ALU.mult, ALU.mult, ["mu0", "mu1", "rstdc0", "rstdc1"], ["nmr"])
        for j in range(16):
            t, tk = tb[j % 2], f"tbc{j % 2}"
            ph.stt(t[:], co[:, j, :], consts[:, LNG + j:LNG + j + 1], rstd[:], ALU.mult, ALU.mult,
                   [f"co{j}_0", f"co{j}_1", "rstdc0", "rstdc1", "consts"], [tk])
            ph.stt(t[:], mu[:], consts[:, LNG + j:LNG + j + 1], t[:], ALU.mult, ALU.add, ["nmr", tk, "consts"], [tk])
            ph.act(act[:, 16 + j, :], t[:], AF.Silu, [tk, "consts"], [f"act{16 + j}"], bias=consts[:, LNB + j:LNB + j + 1])
        if debug:
            ph.dma("sync", chunked(mix_dbg), act[:], reads=[f"act{16 + j}" for j in range(16)])
        ph.emit()

    def gemm_phase_units(ph, wview, ntiles, wb, unit_fn):
        tiles = [[(lambda b: b[:], wview[:, :, i * 256:(i + 1) * 256], "gpsimd")] for i in range(ntiles)]
        pq = [ph.ps("pq") for _ in range(4)]
        cnt = [0]

        def compute(i, buf, bkey):
            for cc in range(2):
                for hf in range(2):
                    u = cnt[0]
                    cnt[0] += 1
                    ps, pk = pq[u % 4], f"pq{u % 4}"
                    for kc in range(32):
                        ph.mm(ps[:], buf[:, kc, cc * 128:(cc + 1) * 128], act[:, kc, hf * 512:(hf + 1) * 512],
                              kc == 0, kc == 31, [bkey, f"act{kc}"], [pk])
                    unit_fn(u, i * 2 + cc, hf, ps, pk)

        wstream(ph, tiles, wb, compute)

    def outproj_phase():
        ph = new_phase()
        wb = [(ph.sb("wb", [128, 32, 256], BF16), f"wb{i}") for i in range(2)]
        xs = [ph.sb("xs", [128, 512], F32) for _ in range(3)]
        xov = chunked(xo)
        x2v = chunked(x2T)
        ones = load_cm(ph, CM_ONES, CM_ONES + 128, "ones")
        ss = [ph.ps("ss") for _ in range(2)]
        sqo = [ph.sb("sqo", [128, 512], BF16) for _ in range(3)]

        def unit(u, chunk, hf, ps, pk):
            x_, xk = xs[u % 3], f"xs{u % 3}"
            sl = slice(hf * 512, (hf + 1) * 512)
            ph.dma("sync", x_[:], xov[:, chunk, sl], writes=[xk])
            ph.tt(x_[:], ps[:], x_[:], ALU.add, [pk, xk], [xk])
            ph.dma("sync", x2v[:, chunk, sl], x_[:], reads=[xk])
            q, qk = sqo[u % 3], f"sqo{u % 3}"
            ph.tt(q[:], x_[:], x_[:], ALU.mult, [xk], [qk], eng="gpsimd")

            def cont():
                ph.mm(ss[hf][:], ones[:], q[:], chunk == 0, chunk == 31, [qk, "ones"], [f"ss{hf}"])
            ph.step([cont])

        gemm_phase_units(ph, chunked(w_out), 16, wb, unit)
        ph.flush()
        rsd = ph.sb("rsd", [128, T], F32)
        for hf in range(2):
            sl = slice(hf * 512, (hf + 1) * 512)
            ph.act(rsd[:, sl], ss[hf][:], AF.Ln, [f"ss{hf}"], [f"rsd{hf}"], scale=1.0 / 4096, bias=EPS)
            ph.act(rsd[:, sl], rsd[:, sl], AF.Exp, [f"rsd{hf}"], [f"rsd{hf}"], scale=-0.5)
        ph.dma("sync", rS, rsd[:], reads=["rsd0", "rsd1"])
        ph.emit()

    def peer_score_phase():
        ph = new_phase()
        consts = load_consts(ph)
        ones = load_cm(ph, CM_ONES, CM_ONES + 128, "ones")
        rstd, ss = norm_to_act(ph, x2T, G_FFN, consts, ones, two_pass=True, rstd_src=rS)
        wb = [(ph.sb("wb", [128, 32, 256], BF16), f"wb{i}") for i in range(2)]
        qp = ph.sb("qp", [128, 16, T], BF16)
        ksb = ph.sb("ksb", [128, 16, 128], BF16)
        ph.dma("gpsimd", ksb[:], ksub.rearrange("a d n -> d a n"), writes=["ksb"])

        def unit(u, chunk, hf, ps, pk):
            ph.act(qp[:, chunk, hf * 512:(hf + 1) * 512], ps[:], AF.Copy, [pk], [f"qp{chunk}_{hf}"])

        sc = [ph.ps("sc") for _ in range(2)]
        Sall = [ph.sb("Sall", [128, 16, 128], F32) for _ in range(2)]
        Sw = ph.sb("Sw", [128, 16, 128], F32)
        t16 = ph.sb("t16", [128, 16, 16], F32)
        cand = ph.sb("cand", [128, 8 * 16, 16], F32)
        candw = ph.sb("candw", [128, 8 * 16, 16], F32)
        b16 = [ph.sb("b16", [128, 8, 16], F32) for _ in range(2)]
        ex = ph.sb("ex", [128, 8, 16], F32)
        negm = ph.sb("negm", [128, 8], F32)
        Z = ph.sb("Z", [128, 8], F32)
        lnZ = ph.sb("lnZ", [128, 8], F32)
        off = ph.sb("off", [128, 8], F32)
        cc_ = ph.sb("cc", [128, 8], F32)
        ec = [ph.sb("ec", [128, 8], F32) for _ in range(2)]
        bt = [ph.sb("bt", [128, 8, 128], F32) for _ in range(2)]
        bSv = bS.rearrange("p (t h n) -> p t h n", t=8, h=8)
        s2Sv = s2S.rearrange("p (t h n) -> p t h n", t=8, h=8)
        ecSv = ecS.rearrange("p (t h) -> p t h", t=8)
        def partA(tt):
            sa, sak = Sall[tt % 2], f"Sall{tt % 2}"
            for q4 in range(4):
                for i in range(4):
                    hc = q4 * 4 + i
                    ph.mm(sc[q4 % 2][:, i * 128:(i + 1) * 128], qp[:, hc, tt * 128:(tt + 1) * 128], ksb[:, hc, :], True, True,
                          [f"qp{hc}_{tt // 4}", "ksb"], [f"sc{q4 % 2}"])
                ph.act(sa[:, q4 * 4:(q4 + 1) * 4, :], sc[q4 % 2][:], AF.Copy, [f"sc{q4 % 2}"], [f"{sak}_{q4}"])
            for hc in range(16):
                ph.op("vector", lambda e, hc=hc, sa=sa: e.max(out=t16[:, hc, 0:8], in_=sa[:, hc, :]), [f"{sak}_{hc // 4}"], ["t16A"])
            for hc in range(16):
                ph.op("vector", lambda e, hc=hc, sa=sa: e.match_replace(out=Sw[:, hc, :], in_to_replace=t16[:, hc, 0:8],
                                                                        in_values=sa[:, hc, :], imm_value=-1e30),
                      [f"{sak}_{hc // 4}", "t16A"], ["SwA"])
            for hc in range(16):
                ph.op("vector", lambda e, hc=hc: e.max(out=t16[:, hc, 8:16], in_=Sw[:, hc, :]), ["SwA"], ["t16B"])
            b_, bk = b16[tt % 2], f"b16{tt % 2}"
            for h in range(8):
                in0 = bass.AP(t16, (2 * h) * 16, [[256, 128], [1, 16], [0, 16]])
                in1 = bass.AP(t16, (2 * h + 1) * 16, [[256, 128], [0, 16], [1, 16]])
                ph.tt(cand[:, h * 16:(h + 1) * 16, :], in0, in1, ALU.add, ["t16A", "t16B"], ["candA"])
            for h in range(8):
                ph.op("vector", lambda e, h=h, b_=b_: e.max(out=b_[:, h, 0:8], in_=cand[:, h * 16:(h + 1) * 16, :]), ["candA"], [bk])
            for h in range(8):
                ph.op("vector", lambda e, h=h, b_=b_: e.match_replace(out=candw[:, h * 16:(h + 1) * 16, :], in_to_replace=b_[:, h, 0:8],
                                                                      in_values=cand[:, h * 16:(h + 1) * 16, :], imm_value=-1e30),
                      ["candA", bk], ["candwA"])
            for h in range(8):
                ph.op("vector", lambda e, h=h, b_=b_: e.max(out=b_[:, h, 8:16], in_=candw[:, h * 16:(h + 1) * 16, :]), ["candwA"], [bk])

        def partB(tt):
            sa, sak = Sall[tt % 2], f"Sall{tt % 2}"
            b_, bk = b16[tt % 2], f"b16{tt % 2}"
            ph.ts(negm[:], b_[:, :, 0], -1.0, None, ALU.mult, None, [bk], ["negm"])
            for h in range(8):
                ph.act(ex[:, h, :], b_[:, h, :], AF.Exp, [bk, "negm"], ["ex", "Z"], bias=negm[:, h:h + 1], accum_out=Z[:, h:h + 1])
            ph.act(lnZ[:], Z[:], AF.Ln, ["Z"], ["lnZ"])
            ph.tt(off[:], negm[:], lnZ[:], ALU.subtract, ["negm", "lnZ"], ["off"])
            ph.tt(cc_[:], b_[:, :, 15], off[:], ALU.add, [bk, "off"], ["cc"])
            e_, ek = ec[tt % 2], f"ec{tt % 2}"
            ph.act(e_[:], cc_[:], AF.Exp, ["cc"], [ek])
            ph.ts(e_[:], e_[:], 1.0 - 2e-5, None, ALU.mult, None, [ek], [ek])
            bt_, btk = bt[tt % 2], f"bt{tt % 2}"
            ph.ts(lnZ[:], b_[:, :, 15], -1.0, None, ALU.mult, None, [bk, "lnZ", "off"], ["lnZ"])
            for h in range(8):
                ph.act(bt_[:, h, :], sa[:, 2 * h, :], AF.Identity, [f"{sak}_{h // 2}", "lnZ"], [btk], bias=lnZ[:, h:h + 1])
            ph.dma("sync", bSv[:, tt, :, :], bt_[:], reads=[btk])
            sav = sa[:].rearrange("p (h c) n -> p h c n", c=2)
            ph.dma("sync", s2Sv[:, tt, :, :], sav[:, :, 1, :], reads=[f"{sak}_{q}" for q in range(4)])
            ph.dma("sync", ecSv[:, tt, :], e_[:], reads=[ek])
        wqv = chunked(wq)
        tiles = [[(lambda b: b[:], wqv[:, :, i * 256:(i + 1) * 256], "gpsimd")] for hf in range(2) for i in range(8)]
        pq = [ph.ps("pq") for _ in range(4)]
        ucnt = [0]

        def compute(idx, buf, bkey):
            hf, i = idx // 8, idx % 8
            if hf == 1 and i % 2 == 0 and i > 0:
                partB(i // 2 - 1)
            for cc in range(2):
                u = ucnt[0]
                ucnt[0] += 1
                ps, pk = pq[u % 4], f"pq{u % 4}"
                for kc in range(32):
                    ph.mm(ps[:], buf[:, kc, cc * 128:(cc + 1) * 128], act[:, kc, hf * 512:(hf + 1) * 512],
                          kc == 0, kc == 31, [bkey, f"act{kc}"], [pk])
                unit(u, i * 2 + cc, hf, ps, pk)
            if hf == 1 and i % 2 == 1:
                partA(i // 2)

        wstream(ph, tiles, wb, compute)
        partB(3)
        for tt in range(4, 8):
            partA(tt)
            partB(tt)
        ph.emit()

    def peer_gate_phase():
        ph = new_phase()
        ident = load_cm(ph, CM_ID, CM_ID + 128, "ident")
        beta = ph.sb("beta", [128, 64, 128], F32)
        s2 = ph.sb("s2", [128, 64, 128], F32)
        ecb = ph.sb("ecb", [128, 64], F32)
        ph.dma("sync", ecb[:], ecS, writes=["ecb"])
        betav = bS.rearrange("p (a n) -> p a n", n=128)
        s2v = s2S.rearrange("p (a n) -> p a n", n=128)
        for tt in range(8):
            ph.dma("sync", s2[:, tt * 8:tt * 8 + 8, :], s2v[:, tt * 8:tt * 8 + 8, :], writes=[f"s2_{tt}"])
            ph.dma("sync", beta[:, tt * 8:tt * 8 + 8, :], betav[:, tt * 8:tt * 8 + 8, :], writes=[f"beta_{tt}"])
        NACT = 5

        def pre_exp(tt):
            a0 = tt * 8 + NACT
            ph.act(s2[:, a0:tt * 8 + 8, :], s2[:, a0:tt * 8 + 8, :], AF.Exp, [f"s2_{tt}"], [f"s2_{tt}"])
            ph.act(beta[:, a0:tt * 8 + 8, :], beta[:, a0:tt * 8 + 8, :], AF.Exp, [f"beta_{tt}"], [f"beta_{tt}"])
        D = 2
        NE, NG = 3, D + 2
        Eb = [ph.sb("Eb", [128, 8, 128], F32) for _ in range(NE)]
        Gb = [ph.sb("Gb", [128, 8, 128], BF16) for _ in range(NG)]
        diag = ph.sb("diag", [128, 64, 128], BF16)
        for a in range(64):
            ph.ts(diag[:, a, :], ident[:], ecb[:, a:a + 1], None, ALU.mult, None, ["ident", "ecb"], [f"diag_{a // 8}"])
        gl = [ph.sb("gl", [128, 512], F32) for _ in range(2)]
        pt = [ph.sb("pt", [128, T], BF16) for _ in range(2)]
        aT = [[ph.ps("aT") for _ in range(2)] for _ in range(2)]
        gT = [[ph.ps("gT") for _ in range(2)] for _ in range(2)]
        wb = [(ph.sb("wbu", [128, 32, 256], BF16), f"wbu{i}") for i in range(2)]
        uv = chunked(uT)
        gcount = [0]
        NU = 128 * 8
        fifo = []

        def gate_ops(n1, tt):
            g = gcount[0]
            gcount[0] += 1
            E, Ek = Eb[g % NE], f"Eb{g % NE}"
            Gm, Gk = Gb[g % NG], f"Gb{g % NG}"
            for h in range(8):
                a = tt * 8 + h
                if h < NACT:
                    ph.act(E[:, h, :], s2[:, a, :], AF.Exp, [f"s2_{tt}", f"beta_{tt}"], [Ek], bias=beta[:, a, n1:n1 + 1])
                else:
                    ph.ts(E[:, h, :], s2[:, a, :], beta[:, a, n1:n1 + 1], None, ALU.mult, None, [f"s2_{tt}", f"beta_{tt}"], [Ek])
            ph.stt(Gm[:], E[:], 1.0 - 2e-5, E[:], ALU.is_ge, ALU.mult, [Ek], [Gk])
            return (Gm, Gk)

        def gate_mms(n1, tt, res):
            par = n1 % 2
            Gm, Gk = res
            for h in range(8):
                ph.mm(gT[par][tt // 4][:, (tt % 4) * 128:(tt % 4 + 1) * 128], Gm[:, h, :], diag[:, tt * 8 + h, :], h == 0, h == 7,
                      [Gk, f"diag_{tt}"], [f"gT{par}{tt // 4}"])

        def issue_gate(g):
            if g < NU:
                fifo.append((g, gate_ops(g // 8, g % 8)))

        for tt in range(8):
            pre_exp(tt)
            gate_mms(0, tt, gate_ops(0, tt))
        for g in range(8, 8 + D):
            issue_gate(g)

        PTv = PT.rearrange("(c p) n -> c p n", p=128)

        def compute(i, buf, bkey):
            for cc in range(2):
                n1 = 2 * i + cc
                par = n1 % 2
                p_, pk = pt[par], f"pt{par}"
                for tt in range(8):
                    st = n1 * 8 + tt
                    issue_gate(st + 8 + D)
                    hf = tt // 4
                    for kc in range((tt % 4) * 8, (tt % 4) * 8 + 8):
                        ph.mm(aT[par][hf][:], buf[:, kc, cc * 128:(cc + 1) * 128], act[:, kc, hf * 512:(hf + 1) * 512],
                              kc == 0, kc == 31, [bkey, f"act{kc}"], [f"aT{par}{hf}"])
                    if st + 8 < NU:
                        g, res = fifo.pop(0)
                        assert g == st + 8
                        gate_mms(g // 8, g % 8, res)
                    if tt % 4 == 3:
                        ph.act(gl[hf][:], aT[par][hf][:], AF.Gelu, [f"aT{par}{hf}"], [f"gl{hf}"])
                        ph.tt(p_[:, hf * 512:(hf + 1) * 512], gT[par][hf][:], gl[hf][:], ALU.mult, [f"gT{par}{hf}", f"gl{hf}"], [pk])
                ph.dma("sync", PTv[n1], p_[:], reads=[pk])

        tiles = [[(lambda b: b[:], uv[:, :, i * 256:(i + 1) * 256], "gpsimd")] for i in range(64)]
        wstream(ph, tiles, wb, compute)
        ph.emit()

    def peer_out_phase():
        ph = new_phase()
        NC3 = 11
        vview = vv.rearrange("(g c p) n -> g p c n", c=4, p=128)
        pview = PT.rearrange("(g c p) n -> g p c n", c=4, p=128)
        x2v, x3v = chunked(x2T), chunked(x3T)
        acc = [ph.ps("acc") for _ in range(8)]
        bufs = [((ph.sb("Vt", [128, 4, 512], BF16), ph.sb("Pt", [128, 4, 512], BF16)), f"vp{i}") for i in range(3)]
        xs = [ph.sb("xs", [128, 512], F32) for _ in range(3)]
        actv = act[:].rearrange("p c (a t) -> p (c a) t", a=2)
        pc2 = ph.sb("pc2", [128, 64, 512], BF16)
        pc3 = ph.sb("pc3", [128, NC3 * 4, 512], BF16)

        def c0(eg):
            return actv[:, 4 * eg:4 * eg + 4, :] if eg < 16 else pc2[:, 4 * (eg - 16):4 * (eg - 16) + 4, :]

        def c1(eg):
            return pc3[:, 4 * eg:4 * eg + 4, :]

        tiles = []
        for db in range(8):
            for eg in range(32):
                ent = [(lambda b: b[0][:], vview[eg][:, :, db * 512:(db + 1) * 512], "gpsimd")]
                if db == 0:
                    ent.append((lambda b, eg=eg: c0(eg), pview[eg][:, :, 0:512], "sync", f"pcA{eg}"))
                    if eg < NC3:
                        ent.append((lambda b, eg=eg: c1(eg), pview[eg][:, :, 512:1024], "sync", f"pcC{eg}"))
                if eg >= NC3:
                    ent.append((lambda b: b[1][:], pview[eg][:, :, 512:1024], "sync"))
                tiles.append(ent)
        cnt = [0]

        def compute(i, buf, bkey):
            db, eg = i // 32, i % 32
            Vt, Pt = buf
            r0, k0 = c0(eg), f"pcA{eg}"
            if eg < NC3:
                r1, k1 = c1(eg), f"pcC{eg}"
            else:
                r1, k1 = Pt, bkey
            for ec in range(4):
                for dc in range(4):
                    for hf in range(2):
                        r, rk = (r0, k0) if hf == 0 else (r1, k1)
                        ph.mm(acc[dc * 2 + hf][:], Vt[:, ec, dc * 128:(dc + 1) * 128], r[:, ec, :],
                              eg == 0 and ec == 0, eg == 31 and ec == 3, [bkey, rk], [f"acc{dc * 2 + hf}"])
            if eg == 31:
                for dc in range(4):
                    for hf in range(2):
                        u = cnt[0]
                        cnt[0] += 1
                        x_, xk = xs[u % 3], f"xs{u % 3}"
                        sl = slice(hf * 512, (hf + 1) * 512)
                        ph.dma("sync", x_[:], x2v[:, db * 4 + dc, sl], writes=[xk])
                        ph.tt(x_[:], acc[dc * 2 + hf][:], x_[:], ALU.add, [f"acc{dc * 2 + hf}", xk], [xk])
                        ph.dma("sync", x3v[:, db * 4 + dc, sl], x_[:], reads=[xk])

        wstream(ph, tiles, bufs, compute)
        ph.emit()

    def ple_phase():
        ph = new_phase()
        consts = load_consts(ph)
        ones = load_cm(ph, CM_ONES, CM_ONES + 128, "ones")
        rstd, ss = norm_to_act(ph, x3T, G_PLE, consts, ones)
        wb = [(ph.sb("wb", [128, 32, 256], BF16), f"wb{i}") for i in range(2)]
        wpb = ph.sb("wpb", [128, 2, 4096], BF16)
        ptb = ph.sb("ptb", [128, 2, T], BF16)
        ph.dma("gpsimd", wpb[:], chunked(wp), writes=["wpb"])
        ph.dma("gpsimd", ptb[:], chunked(pT), writes=["ptb"])
        sg = [ph.sb("sgp", [128, 512], F32) for _ in range(3)]
        xs = [ph.sb("xs", [128, 512], F32) for _ in range(3)]
        pp = [ph.ps("pp") for _ in range(2)]
        x3v, ov = chunked(x3T), chunked(outT)

        def unit(u, chunk, hf, ps, pk):
            s, sk = sg[u % 3], f"sgp{u % 3}"
            ph.tt(s[:], ps[:], rstd[:, hf * 512:(hf + 1) * 512], ALU.mult, [pk, f"rstd{hf}"], [sk])
            ph.act(s[:], s[:], AF.Sigmoid, [sk], [sk])
            x_, xk = xs[u % 3], f"xs{u % 3}"
            sl = slice(hf * 512, (hf + 1) * 512)
            ph.dma("sync", x_[:], x3v[:, chunk, sl], writes=[xk])

            def cont():
                p_, ppk = pp[u % 2], f"pp{u % 2}"
                for kc in range(2):
                    ph.mm(p_[:], wpb[:, kc, chunk * 128:(chunk + 1) * 128], ptb[:, kc, sl], kc == 0, kc == 1, ["wpb", "ptb"], [ppk])
                ph.tt(s[:], p_[:], s[:], ALU.mult, [ppk, sk], [sk])
                ph.tt(x_[:], x_[:], s[:], ALU.add, [xk, sk], [xk])
                ph.dma("sync", ov[:, chunk, sl], x_[:], reads=[xk])
            ph.step([cont])

        gemm_phase_units(ph, chunked(wg), 16, wb, unit)
        ph.emit()

    phases = [lambda: proj_phase(True), lambda: proj_phase(False), attn_phase, conv_phase, outproj_phase,
              peer_score_phase, peer_gate_phase, peer_out_phase, ple_phase]
    for i, f in enumerate(phases[:nph]):
        f()
        if i == 1:
            hes.close()
    if nph < 2:
        hes.close()
    kes.close()
    return nc


def make_inputs(x, p, mix_norm_g, w_in, q_norm_g, k_norm_g, lambda_q, lambda_k, subln_g, glu_b, dw_kernel, dw_b,
                conv_ln_g, conv_ln_b, w_out, ffn_norm_g, peer_w_query, peer_sub_keys, peer_u, peer_v, ple_norm_g,
                ple_gate_w, ple_proj_w):
    f = lambda a: np.ascontiguousarray(np.asarray(a, dtype=np.float32))
    x = f(x)
    p = f(p)

    def pc(v):
        v = f(v).reshape(-1, 128)
        return v.T

    consts = np.zeros((128, NCONST), np.float32)
    consts[:, G_MIX:G_MIX + 32] = pc(mix_norm_g[0])
    consts[:, G_FFN:G_FFN + 32] = pc(ffn_norm_g[0])
    consts[:, G_PLE:G_PLE + 32] = pc(ple_norm_g[0])
    consts[:, QKG:QKG + 2] = f(q_norm_g[0]).T
    consts[:, QKG + 2:QKG + 4] = f(k_norm_g[0]).T
    consts[:, LAM:LAM + 2] = f(lambda_q[0]).T
    consts[:, LAM + 2:LAM + 4] = f(lambda_k[0]).T
    consts[:, SUBG:SUBG + 2] = pc(subln_g[0])
    consts[:, GLUB:GLUB + 32] = pc(glu_b[0])
    consts[:, DWB:DWB + 16] = pc(dw_b[0])
    consts[:, LNG:LNG + 16] = pc(conv_ln_g[0])
    consts[:, LNB:LNB + 16] = pc(conv_ln_b[0])
    dk = f(dw_kernel[0])
    consts[:, DWW:] = dk.reshape(31, 16, 128).transpose(2, 1, 0).reshape(128, 16 * 31)
    cm = np.zeros((128, NCM), np.float32)
    cm[:, CM_ID:CM_ID + 128] = np.eye(128, dtype=np.float32)
    cm[:, CM_ONES:CM_ONES + 128] = 1.0
    kk = np.arange(128)[:, None]
    qq = np.arange(512)[None, :]
    for o in range(4):
        cm[:, CM_MASK + o * 512:CM_MASK + (o + 1) * 512] = (qq >= 128 * o + kk).astype(np.float32)
    shared = {
        "cm": cm,
        "w_in": f(w_in[0]), "w_out": f(w_out[0]), "wq": f(peer_w_query[0]),
        "ksub": np.ascontiguousarray(f(peer_sub_keys[0]).reshape(16, 128, 128).transpose(0, 2, 1)),
        "uT": np.ascontiguousarray(f(peer_u[0]).T), "v": f(peer_v[0]),
        "wg": f(ple_gate_w[0]), "wp": f(ple_proj_w[0]),
    }
    in_maps = []
    for c in range(8):
        b, half = c // 2, c % 2
        cc = consts.copy()
        cc[:, FLAGS] = 1.0 if half == 1 else 0.0
        cc[:, FLAGS + 1] = 0.0 if half == 1 else -30000.0
        d = dict(shared)
        d["consts"] = cc
        d["xo"] = np.ascontiguousarray(x[b, half * T:(half + 1) * T].T)
        d["xc"] = np.ascontiguousarray(x[b, 0:T].T)
        d["pT"] = np.ascontiguousarray(p[0, b, half * T:(half + 1) * T].T)
        in_maps.append(d)
    return in_maps


def kernel(**inputs):
    in_maps = make_inputs(**inputs)
    nc = build_nc()
    res = run_bass_kernel_spmd(nc, in_maps, core_ids=list(range(8)))
    out = np.zeros((4, 2048, 4096), np.float32)
    for c in range(8):
        b, half = c // 2, c % 2
        out[b, half * T:(half + 1) * T] = res.results[c]["outT"].T
    return out
```

```python
import os
import numpy as np
import concourse.bass as bass
import concourse.mybir as mybir
from concourse.bass_utils import run_bass_kernel_spmd
from contextlib import ExitStack

F32 = mybir.dt.float32
BF16 = mybir.dt.bfloat16
AF = mybir.ActivationFunctionType
ALU = mybir.AluOpType

EPS = 1e-6
T = 1024
ENGS = ["sync", "scalar", "vector", "tensor", "gpsimd"]
NRING = 8

G_MIX, G_FFN, G_PLE, QKG, LAM, SUBG, GLUB, DWB, LNG, LNB, FLAGS, DWW = 0, 32, 64, 96, 100, 104, 106, 138, 154, 170, 186, 188
NCONST = DWW + 16 * 31
CM_ID, CM_ONES, CM_MASK = 0, 128, 256
NCM = 256 + 4 * 512


class _Op:
    __slots__ = ("eng", "fn", "deps", "dma", "sem", "val", "need", "guard")

    def __init__(self, eng, fn, dma):
        self.eng = eng
        self.fn = fn
        self.dma = dma
        self.deps = []
        self.sem = None
        self.val = 0
        self.need = dma
        self.guard = None


class Phase:
    def __init__(self, nc, pid, G):
        self.nc = nc
        self.pid = pid
        self.G = G
        self.es = ExitStack()
        self.ops = {e: [] for e in ENGS}
        self.state = {}
        self.n = 0
        self.pend = []

    def sb(self, name, shape, dtype):
        self.n += 1
        return self.es.enter_context(self.nc.sbuf_tensor(f"{name}_{self.pid}_{self.n}", shape, dtype))

    def ps(self, name, shape=(128, 512), dtype=F32):
        self.n += 1
        return self.es.enter_context(self.nc.psum_tensor(f"{name}_{self.pid}_{self.n}", list(shape), dtype))

    def _rec(self, op, reads, writes):
        for k in reads:
            st = self.state.setdefault(k, [None, []])
            if st[0] is not None:
                op.deps.append((st[0], True))
            st[1].append(op)
        for k in writes:
            st = self.state.setdefault(k, [None, []])
            if st[0] is not None and st[0] is not op:
                op.deps.append((st[0], False))
            for r in st[1]:
                if r is not op:
                    op.deps.append((r, False))
            st[0] = op
            st[1] = []
        self.ops[op.eng].append(op)
        return op

    def op(self, eng, fn, reads=(), writes=()):
        return self._rec(_Op(eng, fn, False), reads, writes)

    def dma(self, eng, out, in_, reads=(), writes=()):
        return self._rec(_Op(eng, lambda e: e.dma_start(out=out, in_=in_), True), reads, writes)

    def mm(self, out, lhsT, rhs, start, stop, reads, writes):
        return self.op("tensor", lambda e: e.matmul(out, lhsT, rhs, start=start, stop=stop), reads, writes)

    def act(self, out, in_, func, reads, writes, **kw):
        return self.op("scalar", lambda e: e.activation(out=out, in_=in_, func=func, **kw), reads, writes)

    def ts(self, out, in0, s1, s2, op0, op1, reads, writes, eng="vector"):
        if op1 is None:
            return self.op(eng, lambda e: e.tensor_scalar(out=out, in0=in0, scalar1=s1, scalar2=None, op0=op0), reads, writes)
        return self.op(eng, lambda e: e.tensor_scalar(out=out, in0=in0, scalar1=s1, scalar2=s2, op0=op0, op1=op1), reads, writes)

    def stt(self, out, in0, scalar, in1, op0, op1, reads, writes):
        return self.op("vector", lambda e: e.scalar_tensor_tensor(out=out, in0=in0, scalar=scalar, in1=in1, op0=op0, op1=op1), reads, writes)

    def tt(self, out, in0, in1, op, reads, writes, eng="vector"):
        return self.op(eng, lambda e: e.tensor_tensor(out=out, in0=in0, in1=in1, op=op), reads, writes)

    def step(self, deferred):
        old = self.pend
        self.pend = list(deferred)
        for f in old:
            f()

    def flush(self):
        self.step([])

    def emit(self):
        self.flush()
        nc = self.nc
        es = self.es
        G = self.G
        if "esem" not in G:
            kes = G["kes"]
            G["esem"] = {e: kes.enter_context(nc.semaphore(f"e{e}")) for e in ENGS}
            G["rings"] = {e: [kes.enter_context(nc.semaphore(f"d{e}{i}")) for i in range(NRING)]
                          for e in ("sync", "scalar", "gpsimd")}
            G["cnt"] = {e: 0 for e in ENGS}
            G["rcnt"] = {e: [0] * NRING for e in ENGS}
            G["ri"] = {e: 0 for e in ENGS}
        esem, rings = G["esem"], G["rings"]

        def needs_wait(op, dep, raw):
            if dep.dma or op.dma:
                return True
            if dep.eng != op.eng:
                return True
            if op.eng == "tensor":
                return False
            return raw

        for e in ENGS:
            for op in self.ops[e]:
                for dep, raw in op.deps:
                    if needs_wait(op, dep, raw):
                        dep.need = True
        final = {}
        for e in ENGS:
            cnt = G["cnt"][e]
            rcnt = G["rcnt"][e]
            ri = G["ri"][e]
            for op in self.ops[e]:
                if op.dma:
                    op.sem = rings[e][ri]
                    op.guard = rcnt[ri]
                    rcnt[ri] += 16
                    op.val = rcnt[ri]
                    ri = (ri + 1) % NRING
                elif op.need:
                    cnt += 1
                    op.sem = esem[e]
                    op.val = cnt
            final[e] = list(zip(rings.get(e, []), list(rcnt)))
            G["cnt"][e] = cnt
            G["ri"][e] = ri
        ops = self.ops

        def body(ename):
            def f(eng):
                waited = {}
                for op in ops[ename]:
                    ws = {}
                    for dep, raw in op.deps:
                        if needs_wait(op, dep, raw):
                            k = id(dep.sem)
                            if ws.get(k, (None, 0))[1] < dep.val:
                                ws[k] = (dep.sem, dep.val)
                    if op.dma and op.guard > 0:
                        k = id(op.sem)
                        if ws.get(k, (None, 0))[1] < op.guard:
                            ws[k] = (op.sem, op.guard)
                    for k, (sem, val) in ws.items():
                        if waited.get(k, 0) < val:
                            eng.wait_ge(sem, val)
                            waited[k] = val
                    ins = op.fn(eng)
                    if op.dma:
                        ins.then_inc(op.sem, 16)
                    elif op.need:
                        ins.then_inc(op.sem, 1)
                for sem, val in final[ename]:
                    if val > 0 and waited.get(id(sem), 0) < val:
                        eng.wait_ge(sem, val)
            return f

        with nc.Block() as block:
            for e in ENGS:
                getattr(block, e)(body(e))
        es.close()


def wstream(ph, tiles, bufs, compute):
    nb = len(bufs)

    def issue(i):
        buf, key = bufs[i % nb]
        for ent in tiles[i]:
            dst_fn, src, eng = ent[:3]
            ph.dma(eng, dst_fn(buf), src, writes=[ent[3] if len(ent) > 3 else key])

    for i in range(min(nb - 1, len(tiles))):
        issue(i)
    for i in range(len(tiles)):
        if i + nb - 1 < len(tiles):
            issue(i + nb - 1)
        compute(i, *bufs[i % nb])


def build_nc(nph=99, debug=False):
    nc = bass.Bass("TRN2", target_bir_lowering=False)

    def din(name, shape):
        return nc.dram_tensor(name, list(shape), F32, kind="ExternalInput").ap()

    def scratch(name, shape, dt):
        return nc.dram_tensor(name, list(shape), dt, kind="ExternalOutput" if debug else "Internal").ap()

    xo = din("xo", (4096, T))
    xc = din("xc", (4096, T))
    pT = din("pT", (256, T))
    consts_d = din("consts", (128, NCONST))
    cm_d = din("cm", (128, NCM))
    w_in = din("w_in", (4096, 10240))
    w_out = din("w_out", (4096, 4096))
    wq = din("wq", (4096, 2048))
    ksub = din("ksub", (16, 128, 128))
    uT = din("uT", (4096, 16384))
    vv = din("v", (16384, 4096))
    wg = din("wg", (4096, 4096))
    wp = din("wp", (256, 4096))
    outT = nc.dram_tensor("outT", [4096, T], F32, kind="ExternalOutput").ap()

    qT_s = scratch("qT_s", (2048, T), BF16)
    kT_s = scratch("kT_s", (2048, 2 * T), BF16)
    v_s = scratch("v_s", (2 * T, 2048), BF16)
    y_s = scratch("y_s", (2048, 128 + T), F32)
    x2T = scratch("x2T", (4096, T), F32)
    x3T = scratch("x3T", (4096, T), F32)
    bS = scratch("bS", (128, 8 * 8 * 128), F32)
    s2S = scratch("s2S", (128, 8 * 8 * 128), F32)
    ecS = scratch("ecS", (128, 64), F32)
    rS = scratch("rS", (128, T), F32)
    PT = scratch("PT", (16384, T), BF16)
    mix_dbg = scratch("mix_dbg", (4096, T), BF16) if debug else None

    kes = ExitStack()
    act = kes.enter_context(nc.sbuf_tensor("act", [128, 32, T], BF16))
    hes = ExitStack()
    halo = hes.enter_context(nc.sbuf_tensor("halo", [128, 32, 128], BF16))
    halo_r = hes.enter_context(nc.sbuf_tensor("halo_r", [128, 128], F32))
    pid = [0]
    G = {"kes": kes}

    def new_phase():
        pid[0] += 1
        return Phase(nc, pid[0], G)

    def chunked(ap):
        return ap.rearrange("(c p) n -> p c n", p=128)

    def load_consts(ph):
        c = ph.sb("consts", [128, NCONST], F32)
        ph.dma("sync", c[:], consts_d, writes=["consts"])
        return c

    def load_cm(ph, lo, hi, name):
        t = ph.sb(name, [128, hi - lo], BF16)
        ph.dma("gpsimd", t[:], cm_d[:, lo:hi], writes=[name])
        return t

    def norm_to_act(ph, src, gcol, consts, ones, two_pass=False, rstd_src=None):
        xv = chunked(src)
        xb = [ph.sb("xb", [128, T], F32) for _ in range(3)]
        if rstd_src is not None:
            rstd = ph.sb("rstd", [128, T], F32)
            ph.dma("sync", rstd[:], rstd_src, writes=["rstd0", "rstd1"])
            for c in range(32):
                x_, xk = xb[c % 3], f"xb{c % 3}"
                ph.dma("sync", x_[:], xv[:, c, :], writes=[xk])
                ph.stt(act[:, c, :], x_[:], consts[:, gcol + c:gcol + c + 1], rstd[:], ALU.mult, ALU.mult,
                       [xk, "rstd0", "rstd1", "consts"], [f"act{c}"])
            return rstd, None
        ss = [ph.ps("ss") for _ in range(2)]
        sq = [ph.sb("sq", [128, T], BF16) for _ in range(2)]
        for c in range(32):
            x_, xk = xb[c % 3], f"xb{c % 3}"
            s_, sk = sq[c % 2], f"sq{c % 2}"
            ph.dma("sync", x_[:], xv[:, c, :], writes=[xk])
            ph.tt(s_[:], x_[:], x_[:], ALU.mult, [xk], [sk], eng="gpsimd")
            if not two_pass:
                ph.ts(act[:, c, :], x_[:], consts[:, gcol + c:gcol + c + 1], None, ALU.mult, None, [xk, "consts"], [f"act{c}"])
            for hf in range(2):
                ph.mm(ss[hf][:], ones[:], s_[:, hf * 512:(hf + 1) * 512], c == 0, c == 31, [sk, "ones"], [f"ss{hf}"])
        rstd = ph.sb("rstd", [128, T], F32)
        tmp = ph.sb("tmpn", [128, T], F32)
        for hf in range(2):
            sl = slice(hf * 512, (hf + 1) * 512)
            ph.act(tmp[:, sl], ss[hf][:], AF.Ln, [f"ss{hf}"], [f"tmpn{hf}"], scale=1.0 / 4096, bias=EPS)
            ph.act(rstd[:, sl], tmp[:, sl], AF.Exp, [f"tmpn{hf}"], [f"rstd{hf}"], scale=-0.5)
        if two_pass:
            for c in range(32):
                x_, xk = xb[c % 3], f"xb{c % 3}"
                ph.dma("sync", x_[:], xv[:, c, :], writes=[xk])
                ph.stt(act[:, c, :], x_[:], consts[:, gcol + c:gcol + c + 1], rstd[:], ALU.mult, ALU.mult,
                       [xk, "rstd0", "rstd1", "consts"], [f"act{c}"])
        return rstd, ss

    def proj_phase(is_ctx):
        ph = new_phase()
        consts = load_consts(ph)
        ones = load_cm(ph, CM_ONES, CM_ONES + 128, "ones")
        rstd, ss = norm_to_act(ph, xc if is_ctx else xo, G_MIX, consts, ones)
        identf = ph.sb("identf", [128, 2], F32)
        ph.dma("sync", identf[:], cm_d[:, CM_ID:CM_ID + 2], writes=["identf"])
        for tt in range(8):
            ph.mm(ss[0][:, 2 * tt:2 * tt + 2], rstd[:, tt * 128:(tt + 1) * 128], identf[:], True, True,
                  ["rstd0", "rstd1", "identf"], ["ss0"])
        rcol = ph.sb("rcol", [128, 16], F32)
        ph.act(rcol[:], ss[0][:, 0:16], AF.Copy, ["ss0"], ["rcol"])
        if is_ctx:
            ph.op("gpsimd", lambda e: e.tensor_copy(out=halo[:], in_=act[:, :, 896:1024]), [f"act{c}" for c in range(32)], ["halo"])
            ph.op("gpsimd", lambda e: e.tensor_copy(out=halo_r[:], in_=rstd[:, 896:1024]), ["rstd1"], ["halo_r"])
        qkg = ph.sb("qkg", [128, 4], F32)
        ph.ts(qkg[:, 0:2], consts[:, QKG:QKG + 2], 128.0 ** -0.5, None, ALU.mult, None, ["consts"], ["qkg"])
        ph.ts(qkg[:, 2:4], consts[:, QKG + 2:QKG + 4], 1.0, None, ALU.mult, None, ["consts", "qkg"], ["qkg"])
        wv = chunked(w_in)
        tokbase = 0 if is_ctx else T
        pq = [ph.ps("pq") for _ in range(4)]
        ssp = [ph.ps("ssp") for _ in range(2)]
        zc = [ph.sb("zc", [128, 512], F32) for _ in range(3)]
        sqb = [ph.sb("sqb", [128, 512], BF16) for _ in range(3)]
        lnb_ = [ph.sb("lnt", [128, 512], F32) for _ in range(2)]
        rs_ = [ph.sb("rs", [128, 512], F32) for _ in range(2)]
        ob = [ph.sb("ob", [128, 512], BF16) for _ in range(3)]
        vb = [ph.sb("vb", [128, 256], BF16) for _ in range(3)]
        sg = [ph.sb("sg", [128, 512], F32) for _ in range(2)]
        yb = [ph.sb("yb", [128, 512], F32) for _ in range(3)]
        wb = [(ph.sb("wb", [128, 32, 256], BF16), f"wb{i}") for i in range(2)]
        cnt = {"u": 0, "v": 0, "y": 0}
        allact = [f"act{c}" for c in range(32)]

        tiles = []
        kinds = []
        if not is_ctx:
            for i in range(8):
                tiles.append([(lambda b: b[:], wv[:, :, i * 256:(i + 1) * 256], "gpsimd")])
                kinds.append(("q", i))
        for i in range(8):
            tiles.append([(lambda b: b[:], wv[:, :, 2048 + i * 256:2048 + (i + 1) * 256], "gpsimd")])
            kinds.append(("k", i))
        for i in range(8):
            tiles.append([(lambda b: b[:], wv[:, :, 4096 + i * 256:4096 + (i + 1) * 256], "gpsimd")])
            kinds.append(("v", i))
        for j in range(0 if is_ctx else 16):
            tiles.append([(lambda b: b[:, :, 0:128], wv[:, :, 6144 + j * 128:6144 + (j + 1) * 128], "gpsimd"),
                          (lambda b: b[:, :, 128:256], wv[:, :, 8192 + j * 128:8192 + (j + 1) * 128], "gpsimd")])
            kinds.append(("c", j))

        def qk_unit(kind, chunk, hf, buf, bkey, cc):
            u = cnt["u"]
            cnt["u"] += 1
            ps, pk = pq[u % 4], f"pq{u % 4}"
            for kc in range(32):
                ph.mm(ps[:], buf[:, kc, cc * 128:(cc + 1) * 128], act[:, kc, hf * 512:(hf + 1) * 512],
                      kc == 0, kc == 31, [bkey, f"act{kc}"], [pk])
            z, zk = zc[u % 3], f"zc{u % 3}"
            s, sk = sqb[u % 3], f"sqb{u % 3}"
            ph.tt(z[:], ps[:], rstd[:, hf * 512:(hf + 1) * 512], ALU.mult, [pk, f"rstd{hf}"], [zk])
            ph.tt(s[:], z[:], z[:], ALU.mult, [zk], [sk])

            def cont():
                sp, spk = ssp[u % 2], f"ssp{u % 2}"
                ph.mm(sp[:], ones[:], s[:], True, True, [sk, "ones"], [spk])
                l, lk = lnb_[u % 2], f"lnt{u % 2}"
                r, rk = rs_[u % 2], f"rs{u % 2}"
                ph.act(l[:], sp[:], AF.Ln, [spk], [lk], scale=1.0 / 128, bias=EPS)
                ph.act(r[:], l[:], AF.Exp, [lk], [rk], scale=-0.5)
                o, ok = ob[u % 3], f"ob{u % 3}"
                m = chunk % 2
                gc = m if kind == "q" else 2 + m
                ph.stt(o[:], z[:], qkg[:, gc:gc + 1], r[:], ALU.mult, ALU.mult, [zk, rk, "qkg"], [ok])
                if kind == "q":
                    dst = qT_s[chunk * 128:(chunk + 1) * 128, hf * 512:(hf + 1) * 512]
                else:
                    dst = kT_s[chunk * 128:(chunk + 1) * 128, tokbase + hf * 512:tokbase + (hf + 1) * 512]
                ph.dma("sync", dst, o[:], reads=[ok])
            ph.step([cont])

        def compute(i, buf, bkey):
            kind, idx = kinds[i]
            if kind in ("q", "k"):
                for cc in range(2):
                    for hf in range(2):
                        qk_unit(kind, idx * 2 + cc, hf, buf, bkey, cc)
            elif kind == "v":
                for tt in range(8):
                    u = cnt["u"]
                    cnt["u"] += 1
                    ps, pk = pq[u % 4], f"pq{u % 4}"
                    for kc in range(32):
                        ph.mm(ps[:, 0:256], act[:, kc, tt * 128:(tt + 1) * 128], buf[:, kc, :],
                              kc == 0, kc == 31, [bkey, f"act{kc}"], [pk])
                    n = cnt["v"]
                    cnt["v"] += 1
                    o, ok = vb[n % 3], f"vb{n % 3}"
                    ph.act(o[:], ps[:, 0:256], AF.Identity, [pk, "rcol"], [ok], scale=rcol[:, 2 * tt:2 * tt + 1])
                    ph.dma("sync", v_s[tokbase + tt * 128:tokbase + (tt + 1) * 128, idx * 256:(idx + 1) * 256], o[:], reads=[ok])
                    ph.step([])
            else:
                j = idx
                units = [(0, 512, 128), (512, 512, 640), (-1, 128, 0)]
                for t0, n, d0 in units:
                    u = cnt["u"]
                    cnt["u"] += 2
                    pa, pak = pq[u % 4], f"pq{u % 4}"
                    pg, pgk = pq[(u + 1) % 4], f"pq{(u + 1) % 4}"
                    rhs = (lambda kc: halo[:, kc, :]) if t0 < 0 else (lambda kc: act[:, kc, t0:t0 + n])
                    for kc in range(32):
                        ph.mm(pa[:, 0:n], buf[:, kc, 0:128], rhs(kc), kc == 0, kc == 31, [bkey, f"act{kc}"], [pak])
                    for kc in range(32):
                        ph.mm(pg[:, 0:n], buf[:, kc, 128:256], rhs(kc), kc == 0, kc == 31, [bkey, f"act{kc}"], [pgk])
                    k = cnt["y"]
                    cnt["y"] += 1
                    s, sk = sg[k % 2], f"sg{k % 2}"
                    y, yk = yb[k % 3], f"yb{k % 3}"
                    rsl = halo_r[:, 0:128] if t0 < 0 else rstd[:, t0:t0 + n]
                    rkey = "halo_r" if t0 < 0 else f"rstd{t0 // 512}"
                    ph.tt(s[:, 0:n], pg[:, 0:n], rsl, ALU.mult, [pgk, rkey], [sk])
                    ph.act(s[:, 0:n], s[:, 0:n], AF.Sigmoid, [sk, "consts"], [sk], bias=consts[:, GLUB + 16 + j:GLUB + 17 + j])
                    ph.tt(y[:, 0:n], pa[:, 0:n], rsl, ALU.mult, [pak, rkey], [yk])
                    ph.stt(y[:, 0:n], y[:, 0:n], consts[:, GLUB + j:GLUB + j + 1], s[:, 0:n], ALU.add, ALU.mult,
                           [yk, sk, "consts"], [yk])
                    ph.dma("sync", y_s[j * 128:(j + 1) * 128, d0:d0 + n], y[:, 0:n], reads=[yk])
                    ph.step([])

        wstream(ph, tiles, wb, compute)
        ph.emit()

    def attn_phase():
        ph = new_phase()
        consts = load_consts(ph)
        ones = load_cm(ph, CM_ONES, CM_ONES + 128, "ones")
        cmask = load_cm(ph, CM_MASK, CM_MASK + 2048, "cmask")
        prod = ph.sb("prod", [128, 2], BF16)
        ph.tt(prod[:], consts[:, LAM:LAM + 2], consts[:, LAM + 2:LAM + 4], ALU.mult, ["consts"], ["prod"])
        lps = ph.ps("ssps")
        ph.mm(lps[:, 0:2], ones[:], prod[:], True, True, ["prod", "ones"], ["ssps"])
        el = ph.sb("el", [128, 2], F32)
        ph.act(el[:], lps[:, 0:2], AF.Exp, ["ssps"], ["el"])
        negl = ph.sb("negl", [128, 1], F32)
        ph.tt(negl[:], el[:, 1:2], el[:, 0:1], ALU.subtract, ["el"], ["negl"])
        ph.ts(negl[:], negl[:], -0.2, None, ALU.add, None, ["negl"], ["negl"])
        gs = ph.sb("gs", [128, 2], F32)
        ph.ts(gs[:], consts[:, SUBG:SUBG + 2], 0.8, None, ALU.mult, None, ["consts"], ["gs"])

        qv, kv = chunked(qT_s), chunked(kT_s)
        vview = v_s.rearrange("(t p) n -> p t n", p=128)
        qh = [ph.sb("qh", [128, 2, T], BF16) for _ in range(2)]
        kh = [ph.sb("kh", [128, 2, 2 * T], BF16) for _ in range(2)]
        vh = [ph.sb("vh", [128, 16, 256], BF16) for _ in range(2)]
        sT = [ph.ps("sT") for _ in range(3)]
        den = ph.ps("den")
        O = [ph.ps("O") for _ in range(2)]
        pT_ = [ph.sb("pT", [128, 512], BF16) for _ in range(5)]
        rden = ph.sb("rden", [128, 512], F32)
        R = [[ph.sb("R", [128, 512], F32) for _ in range(2)] for _ in range(2)]
        sq = [ph.sb("sqa", [128, 512], BF16) for _ in range(2)]
        lt = ph.sb("lt", [128, 512], F32)
        rs = ph.sb("rsa", [128, 512], F32)
        pcount = [0]

        def load_head(h):
            b = h % 2
            ph.dma("sync", qh[b][:], qv[:, 2 * h:2 * h + 2, :], writes=[f"qh{b}"])
            ph.dma("sync", kh[b][:], kv[:, 2 * h:2 * h + 2, :], writes=[f"kh{b}"])
            ph.dma("sync", vh[b][:], vview[:, :, h * 256:(h + 1) * 256], writes=[f"vh{b}"])

        load_head(0)
        items = []
        for h in range(8):
            for qb in range(2):
                nj = 12 if qb == 0 else 16
                for m in range(2):
                    for j in range(nj):
                        items.append((h, qb, m, j, nj))
        NS, NP, LA = 3, 5, 2

        def S(p):
            h, qb, m, j, nj = items[p]
            b = h % 2
            st, sk = sT[p % NS], f"sT{p % NS}"
            ph.mm(st[:], kh[b][:, m, j * 128:(j + 1) * 128], qh[b][:, m, qb * 512:(qb + 1) * 512],
                  True, True, [f"kh{b}", f"qh{b}"], [sk])
            pt, pk = pT_[p % NP], f"pT{p % NP}"
            if j < 8:
                ph.act(pt[:], st[:], AF.Exp, [sk, "consts"], [pk], bias=consts[:, FLAGS + 1:FLAGS + 2])
            else:
                ph.act(pt[:], st[:], AF.Exp, [sk], [pk])
            o = j - 8 - 4 * qb
            if o >= 0:
                ph.tt(pt[:], pt[:], cmask[:, o * 512:(o + 1) * 512], ALU.mult, [pk, "cmask"], [pk])

        def PV(p):
            h, qb, m, j, nj = items[p]
            b = h % 2
            pt, pk = pT_[p % NP], f"pT{p % NP}"
            ph.mm(den[:], ones[:], pt[:], j == 0, j == nj - 1, [pk, "ones"], ["den"])
            for a in range(2):
                ph.mm(O[a][:], vh[b][:, j, a * 128:(a + 1) * 128], pt[:], j == 0, j == nj - 1, [pk, f"vh{b}"], [f"O{a}"])

        for p in range(min(LA, len(items))):
            S(p)
        Oc = [ph.sb("Oc", [128, 512], F32) for _ in range(2)]
        later = []
        for p in range(len(items)):
            while later and later[0][0] <= p:
                later.pop(0)[1]()
            h, qb, m, j, nj = items[p]
            if p + LA < len(items):
                S(p + LA)
            if qb == 0 and m == 0 and j == 0 and h + 1 < 8:
                load_head(h + 1)
            PV(p)
            if j == nj - 1:
                for a in range(2):
                    ph.act(Oc[a][:], O[a][:], AF.Copy, [f"O{a}"], [f"Oc{a}"])
                ph.op("vector", lambda e: e.reciprocal(out=rden[:], in_=den[:]), ["den"], ["rden"])
                for a in range(2):
                    ph.tt(R[m][a][:], Oc[a][:], rden[:], ALU.mult, [f"Oc{a}", "rden"], [f"R{m}{a}"])
                if m == 1:
                    for a in range(2):
                        ph.stt(R[0][a][:], R[1][a][:], negl[:, 0:1], R[0][a][:], ALU.mult, ALU.add, [f"R1{a}", f"R0{a}", "negl"], [f"R0{a}"])
                        ph.tt(sq[a][:], R[0][a][:], R[0][a][:], ALU.mult, [f"R0{a}"], [f"sqa{a}"])

                    def fin(h=h, qb=qb):
                        for a in range(2):
                            ph.mm(lps[:], ones[:], sq[a][:], a == 0, a == 1, [f"sqa{a}", "ones"], ["ssps"])
                        ph.act(lt[:], lps[:], AF.Ln, ["ssps"], ["lt"], scale=1.0 / 256, bias=EPS)
                        ph.act(rs[:], lt[:], AF.Exp, ["lt"], ["rsa"], scale=-0.5)
                        for a in range(2):
                            ph.stt(act[:, 2 * h + a, qb * 512:(qb + 1) * 512], R[0][a][:], gs[:, a:a + 1], rs[:], ALU.mult, ALU.mult,
                                   [f"R0{a}", "rsa", "gs"], [f"act{2 * h + a}_{qb}"])
                    later.append((p + 4, fin))
        for _, fn in later:
            fn()
        ph.emit()

    def conv_phase():
        ph = new_phase()
        consts = load_consts(ph)
        ones = load_cm(ph, CM_ONES, CM_ONES + 128, "ones")
        ident = load_cm(ph, CM_ID, CM_ID + 128, "ident")
        yb = [ph.sb("ybc", [128, 128 + T], F32) for _ in range(2)]
        ybf = [ph.sb("ybf", [128, 128 + T], BF16) for _ in range(2)]
        dg = [ph.sb("dg", [128, 31, 128], BF16) for _ in range(2)]
        co = ph.sb("co", [128, 16, T], F32)
        sqb = [ph.sb("sqc", [128, T], BF16) for _ in range(2)]
        cb = [ph.sb("cbc", [128, T], BF16) for _ in range(2)]
        s1 = [ph.ps("s1") for _ in range(2)]
        s2 = [ph.ps("s2") for _ in range(2)]
        cps = [ph.ps("cps") for _ in range(4)]
        def prep(j):
            y, yk = yb[j % 2], f"ybc{j % 2}"
            ph.dma("sync", y[:], y_s[j * 128:(j + 1) * 128, :], writes=[yk])
            ph.ts(y[:, 0:128], y[:, 0:128], consts[:, FLAGS:FLAGS + 1], None, ALU.mult, None, [yk, "consts"], [yk])
            y16, y16k = ybf[j % 2], f"ybf{j % 2}"
            ph.act(y16[:], y[:], AF.Copy, [yk], [y16k])
            d, dk = dg[j % 2], f"dg{j % 2}"
            w0 = DWW + j * 31
            in0 = bass.AP(ident, 0, [[128, 128], [0, 31], [1, 128]])
            in1 = bass.AP(consts, w0, [[NCONST, 128], [1, 31], [0, 128]])
            ph.tt(d[:], in0, in1, ALU.mult, ["ident", "consts"], [dk])

        def stats(j):
            s, sk = sqb[j % 2], f"sqc{j % 2}"
            c, cbk = cb[j % 2], f"cbc{j % 2}"
            for hf in range(2):
                sl = slice(hf * 512, (hf + 1) * 512)
                ph.mm(s1[hf][:], ones[:], c[:, sl], j == 0, j == 15, [cbk, "ones"], [f"s1{hf}"])
                ph.mm(s2[hf][:], ones[:], s[:, sl], j == 0, j == 15, [sk, "ones"], [f"s2{hf}"])

        prep(0)
        for j in range(16):
            if j + 1 < 16:
                prep(j + 1)
            y16, y16k = ybf[j % 2], f"ybf{j % 2}"
            d, dk = dg[j % 2], f"dg{j % 2}"
            ck = f"co{j}"
            for hf in range(2):
                cp, cpk = cps[(2 * j + hf) % 4], f"cps{(2 * j + hf) % 4}"
                for t in range(31):
                    o0 = 98 + t + hf * 512
                    ph.mm(cp[:], d[:, t, :], y16[:, o0:o0 + 512], t == 0, t == 30, [dk, y16k], [cpk])
                ph.act(co[:, j, hf * 512:(hf + 1) * 512], cp[:], AF.Identity, [cpk, "consts"], [f"{ck}_{hf}"],
                       bias=consts[:, DWB + j:DWB + j + 1])
            s, sk = sqb[j % 2], f"sqc{j % 2}"
            c, cbk = cb[j % 2], f"cbc{j % 2}"
            ph.tt(s[:], co[:, j, :], co[:, j, :], ALU.mult, [f"{ck}_0", f"{ck}_1"], [sk], eng="gpsimd")
            ph.act(c[:], co[:, j, :], AF.Copy, [f"{ck}_0", f"{ck}_1"], [cbk])
            ph.step([lambda j=j: stats(j)])
        ph.flush()
        mu = ph.sb("mu", [128, T], F32)
        var = ph.sb("var", [128, T], F32)
        rstd = ph.sb("rstdc", [128, T], F32)
        for hf in range(2):
            sl = slice(hf * 512, (hf + 1) * 512)
            ph.ts(mu[:, sl], s1[hf][:], 1.0 / 2048, None, ALU.mult, None, [f"s1{hf}"], [f"mu{hf}"])
            ph.tt(var[:, sl], mu[:, sl], mu[:, sl], ALU.mult, [f"mu{hf}"], [f"var{hf}"])
            ph.stt(var[:, sl], s2[hf][:], 1.0 / 2048, var[:, sl], ALU.mult, ALU.subtract, [f"s2{hf}", f"var{hf}"], [f"var{hf}"])
            ph.act(var[:, sl], var[:, sl], AF.Ln, [f"var{hf}"], [f"var{hf}"], bias=EPS)
            ph.act(rstd[:, sl], var[:, sl], AF.Exp, [f"var{hf}"], [f"rstdc{hf}"], scale=-0.5)
        tb = [ph.sb("tbc", [128, T], F32) for _ in range(2)]
        ph.stt(mu[:], mu[:], -1.0, rstd[:], ALU.mult, ALU.mult, ["mu0", "mu1", "rstdc0", "rstdc1"], ["nmr"])
        for j in range(16):
            t, tk = tb[j % 2], f"tbc{j % 2}"
            ph.stt(t[:], co[:, j, :], consts[:, LNG + j:LNG + j + 1], rstd[:], ALU.mult, ALU.mult,
                   [f"co{j}_0", f"co{j}_1", "rstdc0", "rstdc1", "consts"], [tk])
            ph.stt(t[:], mu[:], consts[:, LNG + j:LNG + j + 1], t[:], ALU.mult, ALU.add, ["nmr", tk, "consts"], [tk])
            ph.act(act[:, 16 + j, :], t[:], AF.Silu, [tk, "consts"], [f"act{16 + j}"], bias=consts[:, LNB + j:LNB + j + 1])
        if debug:
            ph.dma("sync", chunked(mix_dbg), act[:], reads=[f"act{16 + j}" for j in range(16)])
        ph.emit()

    def gemm_phase_units(ph, wview, ntiles, wb, unit_fn):
        tiles = [[(lambda b: b[:], wview[:, :, i * 256:(i + 1) * 256], "gpsimd")] for i in range(ntiles)]
        pq = [ph.ps("pq") for _ in range(4)]
        cnt = [0]

        def compute(i, buf, bkey):
            for cc in range(2):
                for hf in range(2):
                    u = cnt[0]
                    cnt[0] += 1
                    ps, pk = pq[u % 4], f"pq{u % 4}"
                    for kc in range(32):
                        ph.mm(ps[:], buf[:, kc, cc * 128:(cc + 1) * 128], act[:, kc, hf * 512:(hf + 1) * 512],
                              kc == 0, kc == 31, [bkey, f"act{kc}"], [pk])
                    unit_fn(u, i * 2 + cc, hf, ps, pk)

        wstream(ph, tiles, wb, compute)

    def outproj_phase():
        ph = new_phase()
        wb = [(ph.sb("wb", [128, 32, 256], BF16), f"wb{i}") for i in range(2)]
        xs = [ph.sb("xs", [128, 512], F32) for _ in range(3)]
        xov = chunked(xo)
        x2v = chunked(x2T)
        ones = load_cm(ph, CM_ONES, CM_ONES + 128, "ones")
        ss = [ph.ps("ss") for _ in range(2)]
        sqo = [ph.sb("sqo", [128, 512], BF16) for _ in range(3)]

        def unit(u, chunk, hf, ps, pk):
            x_, xk = xs[u % 3], f"xs{u % 3}"
            sl = slice(hf * 512, (hf + 1) * 512)
            ph.dma("sync", x_[:], xov[:, chunk, sl], writes=[xk])
            ph.tt(x_[:], ps[:], x_[:], ALU.add, [pk, xk], [xk])
            ph.dma("sync", x2v[:, chunk, sl], x_[:], reads=[xk])
            q, qk = sqo[u % 3], f"sqo{u % 3}"
            ph.tt(q[:], x_[:], x_[:], ALU.mult, [xk], [qk], eng="gpsimd")

            def cont():
                ph.mm(ss[hf][:], ones[:], q[:], chunk == 0, chunk == 31, [qk, "ones"], [f"ss{hf}"])
            ph.step([cont])

        gemm_phase_units(ph, chunked(w_out), 16, wb, unit)
        ph.flush()
        rsd = ph.sb("rsd", [128, T], F32)
        for hf in range(2):
            sl = slice(hf * 512, (hf + 1) * 512)
            ph.act(rsd[:, sl], ss[hf][:], AF.Ln, [f"ss{hf}"], [f"rsd{hf}"], scale=1.0 / 4096, bias=EPS)
            ph.act(rsd[:, sl], rsd[:, sl], AF.Exp, [f"rsd{hf}"], [f"rsd{hf}"], scale=-0.5)
        ph.dma("sync", rS, rsd[:], reads=["rsd0", "rsd1"])
        ph.emit()

    def peer_score_phase():
        ph = new_phase()
        consts = load_consts(ph)
        ones = load_cm(ph, CM_ONES, CM_ONES + 128, "ones")
        rstd, ss = norm_to_act(ph, x2T, G_FFN, consts, ones, two_pass=True, rstd_src=rS)
        wb = [(ph.sb("wb", [128, 32, 256], BF16), f"wb{i}") for i in range(2)]
        qp = ph.sb("qp", [128, 16, T], BF16)
        ksb = ph.sb("ksb", [128, 16, 128], BF16)
        ph.dma("gpsimd", ksb[:], ksub.rearrange("a d n -> d a n"), writes=["ksb"])

        def unit(u, chunk, hf, ps, pk):
            ph.act(qp[:, chunk, hf * 512:(hf + 1) * 512], ps[:], AF.Copy, [pk], [f"qp{chunk}_{hf}"])

        sc = [ph.ps("sc") for _ in range(2)]
        Sall = [ph.sb("Sall", [128, 16, 128], F32) for _ in range(2)]
        Sw = ph.sb("Sw", [128, 16, 128], F32)
        t16 = ph.sb("t16", [128, 16, 16], F32)
        cand = ph.sb("cand", [128, 8 * 16, 16], F32)
        candw = ph.sb("candw", [128, 8 * 16, 16], F32)
        b16 = [ph.sb("b16", [128, 8, 16], F32) for _ in range(2)]
        ex = ph.sb("ex", [128, 8, 16], F32)
        negm = ph.sb("negm", [128, 8], F32)
        Z = ph.sb("Z", [128, 8], F32)
        lnZ = ph.sb("lnZ", [128, 8], F32)
        nv16 = ph.sb("nv16", [128, 8], F32)
        off = ph.sb("off", [128, 8], F32)
        cc_ = ph.sb("cc", [128, 8], F32)
        ec = [ph.sb("ec", [128, 8], F32) for _ in range(2)]
        bt = [ph.sb("bt", [128, 8, 128], F32) for _ in range(2)]
        bSv = bS.rearrange("p (t h n) -> p t h n", t=8, h=8)
        s2Sv = s2S.rearrange("p (t h n) -> p t h n", t=8, h=8)
        ecSv = ecS.rearrange("p (t h) -> p t h", t=8)
        def partA(tt):
            sa, sak = Sall[tt % 2], f"Sall{tt % 2}"
            for q4 in range(4):
                for i in range(4):
                    hc = q4 * 4 + i
                    ph.mm(sc[q4 % 2][:, i * 128:(i + 1) * 128], qp[:, hc, tt * 128:(tt + 1) * 128], ksb[:, hc, :], True, True,
                          [f"qp{hc}_{tt // 4}", "ksb"], [f"sc{q4 % 2}"])
                ph.act(sa[:, q4 * 4:(q4 + 1) * 4, :], sc[q4 % 2][:], AF.Copy, [f"sc{q4 % 2}"], [f"{sak}_{q4}"])
            for hc in range(16):
                ph.op("vector", lambda e, hc=hc, sa=sa: e.max(out=t16[:, hc, 0:8], in_=sa[:, hc, :]), [f"{sak}_{hc // 4}"], ["t16A"])
            for hc in range(16):
                ph.op("vector", lambda e, hc=hc, sa=sa: e.match_replace(out=Sw[:, hc, :], in_to_replace=t16[:, hc, 0:8],
                                                                        in_values=sa[:, hc, :], imm_value=-1e30),
                      [f"{sak}_{hc // 4}", "t16A"], ["SwA"])
            for hc in range(16):
                ph.op("vector", lambda e, hc=hc: e.max(out=t16[:, hc, 8:16], in_=Sw[:, hc, :]), ["SwA"], ["t16B"])
            b_, bk = b16[tt % 2], f"b16{tt % 2}"
            for h in range(8):
                in0 = bass.AP(t16, (2 * h) * 16, [[256, 128], [1, 16], [0, 16]])
                in1 = bass.AP(t16, (2 * h + 1) * 16, [[256, 128], [0, 16], [1, 16]])
                ph.tt(cand[:, h * 16:(h + 1) * 16, :], in0, in1, ALU.add, ["t16A", "t16B"], ["candA"])
            for h in range(8):
                ph.op("vector", lambda e, h=h, b_=b_: e.max(out=b_[:, h, 0:8], in_=cand[:, h * 16:(h + 1) * 16, :]), ["candA"], [bk])
            for h in range(8):
                ph.op("vector", lambda e, h=h, b_=b_: e.match_replace(out=candw[:, h * 16:(h + 1) * 16, :], in_to_replace=b_[:, h, 0:8],
                                                                      in_values=cand[:, h * 16:(h + 1) * 16, :], imm_value=-1e30),
                      ["candA", bk], ["candwA"])
            for h in range(8):
                ph.op("vector", lambda e, h=h, b_=b_: e.max(out=b_[:, h, 8:16], in_=candw[:, h * 16:(h + 1) * 16, :]), ["candwA"], [bk])

        def partB(tt):
            sa, sak = Sall[tt % 2], f"Sall{tt % 2}"
            b_, bk = b16[tt % 2], f"b16{tt % 2}"
            ph.ts(negm[:], b_[:, :, 0], -1.0, None, ALU.mult, None, [bk], ["negm"])
            for h in range(8):
                ph.act(ex[:, h, :], b_[:, h, :], AF.Exp, [bk, "negm"], ["ex", "Z"], bias=negm[:, h:h + 1], accum_out=Z[:, h:h + 1])
            ph.act(lnZ[:], Z[:], AF.Ln, ["Z"], ["lnZ"])
            ph.tt(off[:], negm[:], lnZ[:], ALU.subtract, ["negm", "lnZ"], ["off"])
            ph.tt(cc_[:], b_[:, :, 15], off[:], ALU.add, [bk, "off"], ["cc"])
            e_, ek = ec[tt % 2], f"ec{tt % 2}"
            ph.act(e_[:], cc_[:], AF.Exp, ["cc"], [ek], bias=-2e-5)
            bt_, btk = bt[tt % 2], f"bt{tt % 2}"
            ph.ts(nv16[:], b_[:, :, 15], -1.0, None, ALU.mult, None, [bk], ["nv16"])
            for h in range(8):
                ph.act(bt_[:, h, :], sa[:, 2 * h, :], AF.Identity, [f"{sak}_{h // 2}", "nv16"], [btk], bias=nv16[:, h:h + 1])
            ph.dma("sync", bSv[:, tt, :, :], bt_[:], reads=[btk])
            sav = sa[:].rearrange("p (h c) n -> p h c n", c=2)
            ph.dma("sync", s2Sv[:, tt, :, :], sav[:, :, 1, :], reads=[f"{sak}_{q}" for q in range(4)])
            ph.dma("sync", ecSv[:, tt, :], e_[:], reads=[ek])
        wqv = chunked(wq)
        tiles = [[(lambda b: b[:], wqv[:, :, i * 256:(i + 1) * 256], "gpsimd")] for hf in range(2) for i in range(8)]
        pq = [ph.ps("pq") for _ in range(4)]
        ucnt = [0]

        def compute(idx, buf, bkey):
            hf, i = idx // 8, idx % 8
            if hf == 1 and i % 2 == 0 and i > 0:
                partB(i // 2 - 1)
            for cc in range(2):
                u = ucnt[0]
                ucnt[0] += 1
                ps, pk = pq[u % 4], f"pq{u % 4}"
                for kc in range(32):
                    ph.mm(ps[:], buf[:, kc, cc * 128:(cc + 1) * 128], act[:, kc, hf * 512:(hf + 1) * 512],
                          kc == 0, kc == 31, [bkey, f"act{kc}"], [pk])
                unit(u, i * 2 + cc, hf, ps, pk)
            if hf == 1 and i % 2 == 1:
                partA(i // 2)

        wstream(ph, tiles, wb, compute)
        partB(3)
        for tt in range(4, 8):
            partA(tt)
            partB(tt)
        ph.emit()

    def peer_gate_phase():
        ph = new_phase()
        ident = load_cm(ph, CM_ID, CM_ID + 128, "ident")
        beta = ph.sb("beta", [128, 64, 128], F32)
        s2 = ph.sb("s2", [128, 64, 128], F32)
        ecb = ph.sb("ecb", [128, 64], F32)
        ph.dma("sync", ecb[:], ecS, writes=["ecb"])
        betav = bS.rearrange("p (a n) -> p a n", n=128)
        s2v = s2S.rearrange("p (a n) -> p a n", n=128)
        for tt in range(8):
            ph.dma("sync", s2[:, tt * 8:tt * 8 + 8, :], s2v[:, tt * 8:tt * 8 + 8, :], writes=[f"s2_{tt}"])
            ph.dma("sync", beta[:, tt * 8:tt * 8 + 8, :], betav[:, tt * 8:tt * 8 + 8, :], writes=[f"beta_{tt}"])
        NACT = 5

        def pre_exp(tt):
            a0 = tt * 8 + NACT
            ph.act(s2[:, a0:tt * 8 + 8, :], s2[:, a0:tt * 8 + 8, :], AF.Exp, [f"s2_{tt}"], [f"s2_{tt}"])
            ph.act(beta[:, a0:tt * 8 + 8, :], beta[:, a0:tt * 8 + 8, :], AF.Exp, [f"beta_{tt}"], [f"beta_{tt}"])
        D = 2
        NE, NG = 3, D + 2
        Eb = [ph.sb("Eb", [128, 8, 128], F32) for _ in range(NE)]
        Gb = [ph.sb("Gb", [128, 8, 128], BF16) for _ in range(NG)]
        diag = ph.sb("diag", [128, 64, 128], BF16)
        for a in range(64):
            ph.ts(diag[:, a, :], ident[:], ecb[:, a:a + 1], None, ALU.mult, None, ["ident", "ecb"], [f"diag_{a // 8}"])
        gl = [ph.sb("gl", [128, 512], F32) for _ in range(2)]
        pt = [ph.sb("pt", [128, T], BF16) for _ in range(2)]
        aT = [[ph.ps("aT") for _ in range(2)] for _ in range(2)]
        gT = [[ph.ps("gT") for _ in range(2)] for _ in range(2)]
        wb = [(ph.sb("wbu", [128, 32, 256], BF16), f"wbu{i}") for i in range(2)]
        uv = chunked(uT)
        gcount = [0]
        NU = 128 * 8
        fifo = []

        def gate_ops(n1, tt):
            g = gcount[0]
            gcount[0] += 1
            E, Ek = Eb[g % NE], f"Eb{g % NE}"
            Gm, Gk = Gb[g % NG], f"Gb{g % NG}"
            for h in range(8):
                a = tt * 8 + h
                if h < NACT:
                    ph.act(E[:, h, :], s2[:, a, :], AF.Exp, [f"s2_{tt}", f"beta_{tt}"], [Ek], bias=beta[:, a, n1:n1 + 1])
                else:
                    ph.ts(E[:, h, :], s2[:, a, :], beta[:, a, n1:n1 + 1], None, ALU.mult, None, [f"s2_{tt}", f"beta_{tt}"], [Ek])
            ph.stt(Gm[:], E[:], 1.0 - 2e-5, E[:], ALU.is_ge, ALU.mult, [Ek], [Gk])
            return (Gm, Gk)

        def gate_mms(n1, tt, res):
            par = n1 % 2
            Gm, Gk = res
            for h in range(8):
                ph.mm(gT[par][tt // 4][:, (tt % 4) * 128:(tt % 4 + 1) * 128], Gm[:, h, :], diag[:, tt * 8 + h, :], h == 0, h == 7,
                      [Gk, f"diag_{tt}"], [f"gT{par}{tt // 4}"])

        def issue_gate(g):
            if g < NU:
                fifo.append((g, gate_ops(g // 8, g % 8)))

        for tt in range(8):
            pre_exp(tt)
            gate_mms(0, tt, gate_ops(0, tt))
        for g in range(8, 8 + D):
            issue_gate(g)

        PTv = PT.rearrange("(c p) n -> c p n", p=128)

        def compute(i, buf, bkey):
            for cc in range(2):
                n1 = 2 * i + cc
                par = n1 % 2
                p_, pk = pt[par], f"pt{par}"
                for tt in range(8):
                    st = n1 * 8 + tt
                    issue_gate(st + 8 + D)
                    hf = tt // 4
                    for kc in range((tt % 4) * 8, (tt % 4) * 8 + 8):
                        ph.mm(aT[par][hf][:], buf[:, kc, cc * 128:(cc + 1) * 128], act[:, kc, hf * 512:(hf + 1) * 512],
                              kc == 0, kc == 31, [bkey, f"act{kc}"], [f"aT{par}{hf}"])
                    if st + 8 < NU:
                        g, res = fifo.pop(0)
                        assert g == st + 8
                        gate_mms(g // 8, g % 8, res)
                    if tt % 4 == 3:
                        ph.act(gl[hf][:], aT[par][hf][:], AF.Gelu, [f"aT{par}{hf}"], [f"gl{hf}"])
                        ph.tt(p_[:, hf * 512:(hf + 1) * 512], gT[par][hf][:], gl[hf][:], ALU.mult, [f"gT{par}{hf}", f"gl{hf}"], [pk])
                ph.dma("sync", PTv[n1], p_[:], reads=[pk])

        tiles = [[(lambda b: b[:], uv[:, :, i * 256:(i + 1) * 256], "gpsimd")] for i in range(64)]
        wstream(ph, tiles, wb, compute)
        ph.emit()

    def peer_out_phase():
        ph = new_phase()
        NC3 = 11
        vview = vv.rearrange("(g c p) n -> g p c n", c=4, p=128)
        pview = PT.rearrange("(g c p) n -> g p c n", c=4, p=128)
        x2v, x3v = chunked(x2T), chunked(x3T)
        acc = [ph.ps("acc") for _ in range(8)]
        bufs = [((ph.sb("Vt", [128, 4, 512], BF16), ph.sb("Pt", [128, 4, 512], BF16)), f"vp{i}") for i in range(3)]
        xs = [ph.sb("xs", [128, 512], F32) for _ in range(3)]
        actv = act[:].rearrange("p c (a t) -> p (c a) t", a=2)
        pc2 = ph.sb("pc2", [128, 64, 512], BF16)
        pc3 = ph.sb("pc3", [128, NC3 * 4, 512], BF16)

        def c0(eg):
            return actv[:, 4 * eg:4 * eg + 4, :] if eg < 16 else pc2[:, 4 * (eg - 16):4 * (eg - 16) + 4, :]

        def c1(eg):
            return pc3[:, 4 * eg:4 * eg + 4, :]

        tiles = []
        for db in range(8):
            for eg in range(32):
                ent = [(lambda b: b[0][:], vview[eg][:, :, db * 512:(db + 1) * 512], "gpsimd")]
                if db == 0:
                    ent.append((lambda b, eg=eg: c0(eg), pview[eg][:, :, 0:512], "sync", f"pcA{eg}"))
                    if eg < NC3:
                        ent.append((lambda b, eg=eg: c1(eg), pview[eg][:, :, 512:1024], "sync", f"pcC{eg}"))
                if eg >= NC3:
                    ent.append((lambda b: b[1][:], pview[eg][:, :, 512:1024], "sync"))
                tiles.append(ent)
        cnt = [0]

        def compute(i, buf, bkey):
            db, eg = i // 32, i % 32
            Vt, Pt = buf
            r0, k0 = c0(eg), f"pcA{eg}"
            if eg < NC3:
                r1, k1 = c1(eg), f"pcC{eg}"
            else:
                r1, k1 = Pt, bkey
            for ec in range(4):
                for dc in range(4):
                    for hf in range(2):
                        r, rk = (r0, k0) if hf == 0 else (r1, k1)
                        ph.mm(acc[dc * 2 + hf][:], Vt[:, ec, dc * 128:(dc + 1) * 128], r[:, ec, :],
                              eg == 0 and ec == 0, eg == 31 and ec == 3, [bkey, rk], [f"acc{dc * 2 + hf}"])
            if eg == 31:
                for dc in range(4):
                    for hf in range(2):
                        u = cnt[0]
                        cnt[0] += 1
                        x_, xk = xs[u % 3], f"xs{u % 3}"
                        sl = slice(hf * 512, (hf + 1) * 512)
                        ph.dma("sync", x_[:], x2v[:, db * 4 + dc, sl], writes=[xk])
                        ph.tt(x_[:], acc[dc * 2 + hf][:], x_[:], ALU.add, [f"acc{dc * 2 + hf}", xk], [xk])
                        ph.dma("sync", x3v[:, db * 4 + dc, sl], x_[:], reads=[xk])

        wstream(ph, tiles, bufs, compute)
        ph.emit()

    def ple_phase():
        ph = new_phase()
        consts = load_consts(ph)
        ones = load_cm(ph, CM_ONES, CM_ONES + 128, "ones")
        rstd, ss = norm_to_act(ph, x3T, G_PLE, consts, ones)
        wb = [(ph.sb("wb", [128, 32, 256], BF16), f"wb{i}") for i in range(2)]
        wpb = ph.sb("wpb", [128, 2, 4096], BF16)
        ptb = ph.sb("ptb", [128, 2, T], BF16)
        ph.dma("gpsimd", wpb[:], chunked(wp), writes=["wpb"])
        ph.dma("gpsimd", ptb[:], chunked(pT), writes=["ptb"])
        sg = [ph.sb("sgp", [128, 512], F32) for _ in range(3)]
        xs = [ph.sb("xs", [128, 512], F32) for _ in range(3)]
        pp = [ph.ps("pp") for _ in range(2)]
        x3v, ov = chunked(x3T), chunked(outT)

        def unit(u, chunk, hf, ps, pk):
            s, sk = sg[u % 3], f"sgp{u % 3}"
            ph.tt(s[:], ps[:], rstd[:, hf * 512:(hf + 1) * 512], ALU.mult, [pk, f"rstd{hf}"], [sk])
            ph.act(s[:], s[:], AF.Sigmoid, [sk], [sk])
            x_, xk = xs[u % 3], f"xs{u % 3}"
            sl = slice(hf * 512, (hf + 1) * 512)
            ph.dma("sync", x_[:], x3v[:, chunk, sl], writes=[xk])

            def cont():
                p_, ppk = pp[u % 2], f"pp{u % 2}"
                for kc in range(2):
                    ph.mm(p_[:], wpb[:, kc, chunk * 128:(chunk + 1) * 128], ptb[:, kc, sl], kc == 0, kc == 1, ["wpb", "ptb"], [ppk])
                ph.tt(s[:], p_[:], s[:], ALU.mult, [ppk, sk], [sk])
                ph.tt(x_[:], x_[:], s[:], ALU.add, [xk, sk], [xk])
                ph.dma("sync", ov[:, chunk, sl], x_[:], reads=[xk])
            ph.step([cont])

        gemm_phase_units(ph, chunked(wg), 16, wb, unit)
        ph.emit()

    phases = [lambda: proj_phase(True), lambda: proj_phase(False), attn_phase, conv_phase, outproj_phase,
              peer_score_phase, peer_gate_phase, peer_out_phase, ple_phase]
    for i, f in enumerate(phases[:nph]):
        f()
        if i == 1:
            hes.close()
    if nph < 2:
        hes.close()
    kes.close()
    return nc


def make_inputs(x, p, mix_norm_g, w_in, q_norm_g, k_norm_g, lambda_q, lambda_k, subln_g, glu_b, dw_kernel, dw_b,
                conv_ln_g, conv_ln_b, w_out, ffn_norm_g, peer_w_query, peer_sub_keys, peer_u, peer_v, ple_norm_g,
                ple_gate_w, ple_proj_w):
    f = lambda a: np.ascontiguousarray(np.asarray(a, dtype=np.float32))
    x = f(x)
    p = f(p)

    def pc(v):
        v = f(v).reshape(-1, 128)
        return v.T

    consts = np.zeros((128, NCONST), np.float32)
    consts[:, G_MIX:G_MIX + 32] = pc(mix_norm_g[0])
    consts[:, G_FFN:G_FFN + 32] = pc(ffn_norm_g[0])
    consts[:, G_PLE:G_PLE + 32] = pc(ple_norm_g[0])
    consts[:, QKG:QKG + 2] = f(q_norm_g[0]).T
    consts[:, QKG + 2:QKG + 4] = f(k_norm_g[0]).T
    consts[:, LAM:LAM + 2] = f(lambda_q[0]).T
    consts[:, LAM + 2:LAM + 4] = f(lambda_k[0]).T
    consts[:, SUBG:SUBG + 2] = pc(subln_g[0])
    consts[:, GLUB:GLUB + 32] = pc(glu_b[0])
    consts[:, DWB:DWB + 16] = pc(dw_b[0])
    consts[:, LNG:LNG + 16] = pc(conv_ln_g[0])
    consts[:, LNB:LNB + 16] = pc(conv_ln_b[0])
    dk = f(dw_kernel[0])
    consts[:, DWW:] = dk.reshape(31, 16, 128).transpose(2, 1, 0).reshape(128, 16 * 31)
    cm = np.zeros((128, NCM), np.float32)
    cm[:, CM_ID:CM_ID + 128] = np.eye(128, dtype=np.float32)
    cm[:, CM_ONES:CM_ONES + 128] = 1.0
    kk = np.arange(128)[:, None]
    qq = np.arange(512)[None, :]
    for o in range(4):
        cm[:, CM_MASK + o * 512:CM_MASK + (o + 1) * 512] = (qq >= 128 * o + kk).astype(np.float32)
    shared = {
        "cm": cm,
        "w_in": f(w_in[0]), "w_out": f(w_out[0]), "wq": f(peer_w_query[0]),
        "ksub": np.ascontiguousarray(f(peer_sub_keys[0]).reshape(16, 128, 128).transpose(0, 2, 1)),
        "uT": np.ascontiguousarray(f(peer_u[0]).T), "v": f(peer_v[0]),
        "wg": f(ple_gate_w[0]), "wp": f(ple_proj_w[0]),
    }
    in_maps = []
    for c in range(8):
        b, half = c // 2, c % 2
        cc = consts.copy()
        cc[:, FLAGS] = 1.0 if half == 1 else 0.0
        cc[:, FLAGS + 1] = 0.0 if half == 1 else -30000.0
        d = dict(shared)
        d["consts"] = cc
        d["xo"] = np.ascontiguousarray(x[b, half * T:(half + 1) * T].T)
        d["xc"] = np.ascontiguousarray(x[b, 0:T].T)
        d["pT"] = np.ascontiguousarray(p[0, b, half * T:(half + 1) * T].T)
        in_maps.append(d)
    return in_maps


def kernel(**inputs):
    in_maps = make_inputs(**inputs)
    nc = build_nc()
    res = run_bass_kernel_spmd(nc, in_maps, core_ids=list(range(8)))
    out = np.zeros((4, 2048, 4096), np.float32)
    for c in range(8):
        b, half = c // 2, c % 2
        out[b, half * T:(half + 1) * T] = res.results[c]["outT"].T
    return out
```
